# Optimizing a Trainium2 kernel written in Bass

```python
import numpy as np
import jax
import jax.numpy as jnp
from jax import lax

D_MODEL = 2048
BATCH = 8
SEQ = 2048
DEPTH = 1

MEM_LEN = 256
SSD_EXPAND = 2
SSD_INNER = SSD_EXPAND * D_MODEL
SSD_HEAD_DIM = 64
SSD_HEADS = SSD_INNER // SSD_HEAD_DIM
SSD_GROUPS = 8
SSD_STATE = 128
SSD_CONV = 4
SSD_CHUNK = 256
SSD_XBC = SSD_INNER + 2 * SSD_GROUPS * SSD_STATE
HEAD_DIM = 128
NSA_HEADS = D_MODEL // HEAD_DIM
NSA_KV_HEADS = 4
NSA_WIDTH = NSA_HEADS * HEAD_DIM
NSA_KV_WIDTH = NSA_KV_HEADS * HEAD_DIM
CMP_BLK = 32
CMP_STRIDE = 16
CMP_HID = 256
SEL_BLK = 64
N_SEL = 16
WINDOW = 512
Q_BLK = 128
FORCE_SCORE = 1.0e4
X_HEADS = 4
X_WIDTH = X_HEADS * HEAD_DIM
N_GROUPS = 8
EXPERTS_PER_GROUP = 8
N_EXPERTS = N_GROUPS * EXPERTS_PER_GROUP
TOP_K = 2
EXPERT_HIDDEN = 1408
MOE_BLK = 128
ROPE_THETA = 10000.0
EPS = 1e-6
NEG_INF = -1e30

IN_SPLITS = (SSD_INNER, SSD_XBC, SSD_HEADS,
             NSA_WIDTH,
             NSA_KV_WIDTH, NSA_KV_WIDTH,
             NSA_KV_WIDTH, NSA_KV_WIDTH,
             NSA_KV_WIDTH, NSA_KV_WIDTH,
             3 * NSA_HEADS,
             D_MODEL, D_MODEL)
IN_COLS = sum(IN_SPLITS)

kernel_name = "hybrid_ssd_nsa_memxattn_hmoe_layer"


def rms_norm(x, w):
    xf = x.astype(jnp.float32)
    y = xf * lax.rsqrt(jnp.mean(xf * xf, axis=-1, keepdims=True) + EPS)
    return (y * w.astype(jnp.float32)).astype(x.dtype)


def rope(x, pos):
    half = x.shape[-1] // 2
    inv = ROPE_THETA ** (-jnp.arange(half, dtype=jnp.float32) / half)
    ang = pos.astype(jnp.float32)[:, None] * inv[None, :]
    shape = (1, pos.shape[0]) + (1,) * (x.ndim - 3) + (half,)
    cos = jnp.cos(ang).reshape(shape)
    sin = jnp.sin(ang).reshape(shape)
    x1 = x[..., :half].astype(jnp.float32)
    x2 = x[..., half:].astype(jnp.float32)
    return jnp.concatenate([x1 * cos - x2 * sin, x2 * cos + x1 * sin], axis=-1).astype(x.dtype)


def masked_softmax(s, mask):
    s = jnp.where(mask, s.astype(jnp.float32), NEG_INF)
    p = jax.nn.softmax(s, axis=-1)
    return jnp.where(mask, p, 0.0)


def causal_dwconv(u, w, b):
    c = u.shape[-1]
    y = lax.conv_general_dilated(u, w.reshape(SSD_CONV, 1, c).astype(u.dtype), window_strides=(1,),
                                 padding=[(SSD_CONV - 1, 0)], dimension_numbers=('NWC', 'WIO', 'NWC'),
                                 feature_group_count=c)
    return y + b


def ssd_chunked(x, da, bm, cm):
    b, t, h, p = x.shape
    g, n = bm.shape[2], bm.shape[3]
    r = h // g
    pad = (-t) % SSD_CHUNK
    if pad:
        x = jnp.pad(x, ((0, 0), (0, pad), (0, 0), (0, 0)))
        da = jnp.pad(da, ((0, 0), (0, pad), (0, 0)))
        bm = jnp.pad(bm, ((0, 0), (0, pad), (0, 0), (0, 0)))
        cm = jnp.pad(cm, ((0, 0), (0, pad), (0, 0), (0, 0)))
    tp = t + pad
    nc, L = tp // SSD_CHUNK, SSD_CHUNK
    x = x.reshape(b, nc, L, g, r, p)
    bm = bm.reshape(b, nc, L, g, n)
    cm = cm.reshape(b, nc, L, g, n)
    da = da.astype(jnp.float32).reshape(b, nc, L, g, r).transpose(0, 1, 3, 4, 2)
    acum = jnp.cumsum(da, axis=-1)
    tri = jnp.tril(jnp.ones((L, L), dtype=bool))
    decay = jnp.exp(jnp.where(tri, acum[..., :, None] - acum[..., None, :], -jnp.inf))
    cb = jnp.einsum('bclgn,bcsgn->bcgls', cm, bm)
    y_diag = jnp.einsum('bcgrls,bcsgrp->bclgrp', cb[:, :, :, None] * decay, x)
    decay_to_end = jnp.exp(acum[..., -1:] - acum)
    states = jnp.einsum('bclgn,bcgrl,bclgrp->bcgrpn', bm, decay_to_end, x)
    chunk_decay = jnp.exp(acum[..., -1])

    def step(hstate, inp):
        s_c, d_c = inp
        return hstate * d_c[..., None, None] + s_c, hstate

    h0 = jnp.zeros((b, g, r, p, n), states.dtype)
    _, h_prev = lax.scan(step, h0, (jnp.swapaxes(states, 0, 1), jnp.swapaxes(chunk_decay, 0, 1)))
    h_prev = jnp.swapaxes(h_prev, 0, 1)
    y_off = jnp.einsum('bclgn,bcgrpn,bcgrl->bclgrp', cm, h_prev, jnp.exp(acum))
    return (y_diag + y_off).reshape(b, tp, h, p)[:, :t]


def ssd_branch(z, xbc, dt_raw, conv_w, conv_b, dt_bias, a_log, d_skip, norm_w):
    b, t, _ = z.shape
    xbc = jax.nn.silu(causal_dwconv(xbc, conv_w, conv_b))
    gn = SSD_GROUPS * SSD_STATE
    xs = xbc[..., :SSD_INNER].reshape(b, t, SSD_HEADS, SSD_HEAD_DIM)
    bm = xbc[..., SSD_INNER:SSD_INNER + gn].reshape(b, t, SSD_GROUPS, SSD_STATE)
    cm = xbc[..., SSD_INNER + gn:].reshape(b, t, SSD_GROUPS, SSD_STATE)
    dt = jax.nn.softplus(dt_raw.astype(jnp.float32) + dt_bias.astype(jnp.float32))
    a = -jnp.exp(a_log.astype(jnp.float32))
    y = ssd_chunked(xs * dt[..., None], dt * a, bm, cm)
    y = y + d_skip[:, None] * xs
    yg = (y.reshape(b, t, SSD_INNER) * jax.nn.silu(z)).reshape(b, t, SSD_GROUPS, SSD_INNER // SSD_GROUPS)
    yg = rms_norm(yg, norm_w.reshape(SSD_GROUPS, SSD_INNER // SSD_GROUPS))
    return yg.reshape(b, t, SSD_INNER).astype(z.dtype)


def compress(raw, pe, w1, w2):
    t = raw.shape[1]
    ncmp = (t - CMP_BLK) // CMP_STRIDE + 1
    idx = CMP_STRIDE * jnp.arange(ncmp)[:, None] + jnp.arange(CMP_BLK)[None, :]
    blocks = raw[:, idx] + pe[:, None, :]
    hid = jax.nn.silu(jnp.einsum('bcjgd,jdf->bgcf', blocks, w1))
    return jnp.einsum('bgcf,fd->bgcd', hid, w2)


def nsa_sequence(q, kc, vc, ks, vs, kw_pad, vw_pad, gates):
    g, r, t, hd = q.shape
    ncmp = kc.shape[1]
    nsel_blocks = t // SEL_BLK
    n_sel = min(N_SEL, nsel_blocks)
    scale = hd ** -0.5
    comp_end = jnp.arange(ncmp) * CMP_STRIDE + CMP_BLK - 1
    ci = jnp.arange(ncmp)[:, None]
    sj = jnp.arange(nsel_blocks)[None, :]
    overlap = ((ci * CMP_STRIDE < (sj + 1) * SEL_BLK) &
               (ci * CMP_STRIDE + CMP_BLK > sj * SEL_BLK)).astype(jnp.float32)
    ks_blk = ks.reshape(g, nsel_blocks, SEL_BLK, hd)
    vs_blk = vs.reshape(g, nsel_blocks, SEL_BLK, hd)
    g_idx = jnp.arange(g)[:, None, None]
    sblk = jnp.arange(nsel_blocks)[None, :]

    def block(qb):
        q0 = qb * Q_BLK
        qq = lax.dynamic_slice_in_dim(q, q0, Q_BLK, axis=2)
        gb = lax.dynamic_slice_in_dim(gates, q0, Q_BLK, axis=3)
        tq = q0 + jnp.arange(Q_BLK)
        s_c = jnp.einsum('grqd,gcd->grqc', qq, kc) * scale
        p_c = masked_softmax(s_c, comp_end[None, :] <= tq[:, None])
        o_c = jnp.einsum('grqc,gcd->grqd', p_c.astype(vc.dtype), vc)
        imp = jnp.einsum('grqc,cs->gqs', p_c, overlap)
        blk_t = (tq // SEL_BLK)[:, None]
        valid = sblk <= blk_t
        forced = (sblk == 0) | (sblk == blk_t) | (sblk == blk_t - 1)
        imp = jnp.where(valid, jnp.where(forced, FORCE_SCORE, imp), -jnp.inf)
        _, sel = lax.top_k(imp, n_sel)
        k_sel = ks_blk[g_idx, sel]
        v_sel = vs_blk[g_idx, sel]
        s_s = jnp.einsum('grqd,gqnkd->grqnk', qq, k_sel) * scale
        kpos = sel[..., None] * SEL_BLK + jnp.arange(SEL_BLK)
        m_s = (kpos <= tq[None, :, None, None]).reshape(g, 1, Q_BLK, n_sel * SEL_BLK)
        p_s = masked_softmax(s_s.reshape(g, r, Q_BLK, n_sel * SEL_BLK), m_s)
        o_s = jnp.einsum('grqnk,gqnkd->grqd', p_s.reshape(s_s.shape).astype(v_sel.dtype), v_sel)
        kwin = lax.dynamic_slice_in_dim(kw_pad, q0, WINDOW + Q_BLK, axis=1)
        vwin = lax.dynamic_slice_in_dim(vw_pad, q0, WINDOW + Q_BLK, axis=1)
        s_w = jnp.einsum('grqd,gkd->grqk', qq, kwin) * scale
        kp = q0 - WINDOW + jnp.arange(WINDOW + Q_BLK)
        diff = tq[:, None] - kp[None, :]
        m_w = (kp[None, :] >= 0) & (diff >= 0) & (diff < WINDOW)
        p_w = masked_softmax(s_w, m_w)
        o_w = jnp.einsum('grqk,gkd->grqd', p_w.astype(vwin.dtype), vwin)
        out = gb[0][..., None] * o_c + gb[1][..., None] * o_s + gb[2][..., None] * o_w
        return out.astype(q.dtype)

    out = lax.map(block, jnp.arange(t // Q_BLK))
    return out.transpose(0, 3, 1, 2, 4).reshape(t, g * r * hd)


def nsa_branch(q, kc_raw, vc_raw, ks, vs, kw, vw, gate_logits, pos, q_norm_w, k_norm_w,
               pe_k, w1_k, w2_k, pe_v, w1_v, w2_v):
    b, t, _ = q.shape
    g, r = NSA_KV_HEADS, NSA_HEADS // NSA_KV_HEADS
    qh = rope(rms_norm(q.reshape(b, t, g, r, HEAD_DIM), q_norm_w), pos).transpose(0, 2, 3, 1, 4)

    def keys(k, wn):
        return rope(rms_norm(k.reshape(b, t, g, HEAD_DIM), wn), pos)

    def heads(v):
        return v.reshape(b, t, g, HEAD_DIM)

    kc = compress(keys(kc_raw, k_norm_w[0]), pe_k, w1_k, w2_k)
    vc = compress(heads(vc_raw), pe_v, w1_v, w2_v)
    ks_ = keys(ks, k_norm_w[1]).transpose(0, 2, 1, 3)
    vs_ = heads(vs).transpose(0, 2, 1, 3)
    wpad = ((0, 0), (0, 0), (WINDOW, 0), (0, 0))
    kw_ = jnp.pad(keys(kw, k_norm_w[2]).transpose(0, 2, 1, 3), wpad)
    vw_ = jnp.pad(heads(vw).transpose(0, 2, 1, 3), wpad)
    gates = jax.nn.sigmoid(gate_logits.astype(jnp.float32)).reshape(b, t, 3, g, r).transpose(0, 2, 3, 4, 1)
    return lax.map(lambda a: nsa_sequence(*a), (qh, kc, vc, ks_, vs_, kw_, vw_, gates))


def hybrid_mixer(h, norm_w, w_in, conv_w, conv_b, dt_bias, a_log, d_skip, ssd_norm_w,
                 q_norm_w, k_norm_w, pe_k, w1_k, w2_k, pe_v, w1_v, w2_v, w_up_ssd, w_up_nsa, w_out):
    t = h.shape[1]
    pos = jnp.arange(t)
    hn = rms_norm(h, norm_w)
    proj = hn @ w_in
    offs = np.cumsum(IN_SPLITS)[:-1].tolist()
    (z, xbc, dt_raw, q, kc_raw, vc_raw, ks, vs, kw, vw, nsa_g, g_ssd, g_nsa) = jnp.split(proj, offs, axis=-1)
    y_ssd = ssd_branch(z, xbc, dt_raw, conv_w, conv_b, dt_bias, a_log, d_skip, ssd_norm_w)
    y_nsa = nsa_branch(q, kc_raw, vc_raw, ks, vs, kw, vw, nsa_g, pos, q_norm_w, k_norm_w,
                       pe_k, w1_k, w2_k, pe_v, w1_v, w2_v)
    merged = jax.nn.sigmoid(g_ssd) * (y_ssd @ w_up_ssd) + jax.nn.sigmoid(g_nsa) * (y_nsa @ w_up_nsa)
    return h + merged @ w_out


def memory_xattn(h, mem, norm_w, mem_norm_w, wq, wkv, q_norm_w, k_norm_w, wo):
    b, t, _ = h.shape
    m = mem.shape[1]
    hn = rms_norm(h, norm_w)
    mn = rms_norm(mem, mem_norm_w)
    q = rms_norm((hn @ wq).reshape(b, t, X_HEADS, HEAD_DIM), q_norm_w)
    kv = (mn @ wkv).reshape(b, m, 2, X_HEADS, HEAD_DIM)
    k = rms_norm(kv[:, :, 0], k_norm_w)
    v = kv[:, :, 1]
    s = jnp.einsum('bthd,bmhd->bhtm', q, k).astype(jnp.float32) * HEAD_DIM ** -0.5
    p = jax.nn.softmax(s, axis=-1).astype(v.dtype)
    o = jnp.einsum('bhtm,bmhd->bthd', p, v).reshape(b, t, X_WIDTH)
    return h + o @ wo


def hier_moe(h, norm_w, rg_w, rg_b, re_w, re_b, w_gate, w_up, w_down):
    b, t, d = h.shape
    n = b * t
    hf = rms_norm(h, norm_w).reshape(n, d)
    pg = jax.nn.softmax((hf @ rg_w + rg_b).astype(jnp.float32), axis=-1)
    grp = jnp.argmax(pg, axis=-1)
    pg_top = jnp.take_along_axis(pg, grp[:, None], axis=-1)
    le = (hf @ re_w + re_b).astype(jnp.float32).reshape(n, N_GROUPS, EXPERTS_PER_GROUP)
    le = jnp.take_along_axis(le, grp[:, None, None], axis=1)[:, 0]
    top_p, top_i = lax.top_k(jax.nn.softmax(le, axis=-1), TOP_K)
    wts = (pg_top * top_p / jnp.sum(top_p, axis=-1, keepdims=True)).reshape(-1)
    eid = (grp[:, None] * EXPERTS_PER_GROUP + top_i).reshape(-1).astype(jnp.int32)
    tok = jnp.repeat(jnp.arange(n, dtype=jnp.int32), TOP_K)
    n_assign = n * TOP_K
    order = jnp.argsort(eid)
    e_sorted = eid[order]
    counts = jnp.bincount(eid, length=N_EXPERTS)
    start = jnp.cumsum(counts) - counts
    padded = (counts + MOE_BLK - 1) // MOE_BLK * MOE_BLK
    pend = jnp.cumsum(padded)
    pstart = pend - padded
    dest = pstart[e_sorted] + jnp.arange(n_assign) - start[e_sorted]
    n_blk = (n_assign + N_EXPERTS * (MOE_BLK - 1) + MOE_BLK - 1) // MOE_BLK
    rows = n_blk * MOE_BLK
    tok_buf = jnp.full((rows,), n, jnp.int32).at[dest].set(tok[order])
    w_buf = jnp.zeros((rows,), jnp.float32).at[dest].set(wts[order])
    blk_e = jnp.minimum(jnp.searchsorted(pend, jnp.arange(n_blk) * MOE_BLK, side='right'), N_EXPERTS - 1)
    x_buf = jnp.concatenate([hf, jnp.zeros((1, d), hf.dtype)], axis=0)[tok_buf].reshape(n_blk, MOE_BLK, d)

    def expert_block(args):
        xb, e = args
        return (jax.nn.silu(xb @ w_gate[e]) * (xb @ w_up[e])) @ w_down[e]

    y_buf = lax.map(expert_block, (x_buf, blk_e)).reshape(rows, d)
    out = jnp.zeros((n + 1, d), y_buf.dtype).at[tok_buf].add(y_buf * w_buf[:, None].astype(y_buf.dtype))
    return h + out[:n].reshape(b, t, d)


def setup_inputs(seed: int = 0) -> dict:
    key = jax.random.key(seed)
    ks = jax.random.split(key, 40)
    f32 = jnp.float32

    def nrm(k, shape, scale):
        return jax.random.normal(k, (DEPTH,) + shape, f32) * scale

    def gain(k, shape):
        return 1.0 + 0.02 * jax.random.normal(k, (DEPTH,) + shape, f32)

    dt0 = jnp.exp(jax.random.uniform(ks[6], (DEPTH, SSD_HEADS), f32, np.log(1e-3), np.log(1e-1)))
    return {
        "x": jax.random.normal(ks[0], (BATCH, SEQ, D_MODEL), f32),
        "mem": jax.random.normal(ks[1], (BATCH, MEM_LEN, D_MODEL), f32),
        "norm1_w": gain(ks[2], (D_MODEL,)),
        "w_in": nrm(ks[3], (D_MODEL, IN_COLS), D_MODEL ** -0.5),
        "ssd_conv_w": nrm(ks[4], (SSD_CONV, SSD_XBC), SSD_CONV ** -0.5),
        "ssd_conv_b": nrm(ks[5], (SSD_XBC,), 0.02),
        "ssd_dt_bias": dt0 + jnp.log(-jnp.expm1(-dt0)),
        "ssd_a_log": jnp.log(jax.random.uniform(ks[7], (DEPTH, SSD_HEADS), f32, 1.0, 16.0)),
        "ssd_d": gain(ks[8], (SSD_HEADS,)),
        "ssd_norm_w": gain(ks[9], (SSD_INNER,)),
        "nsa_q_norm_w": gain(ks[10], (HEAD_DIM,)),
        "nsa_k_norm_w": gain(ks[11], (3, HEAD_DIM)),
        "cmp_pe_k": nrm(ks[12], (CMP_BLK, HEAD_DIM), 0.02),
        "cmp_w1_k": nrm(ks[13], (CMP_BLK, HEAD_DIM, CMP_HID), (CMP_BLK * HEAD_DIM) ** -0.5),
        "cmp_w2_k": nrm(ks[14], (CMP_HID, HEAD_DIM), CMP_HID ** -0.5),
        "cmp_pe_v": nrm(ks[15], (CMP_BLK, HEAD_DIM), 0.02),
        "cmp_w1_v": nrm(ks[16], (CMP_BLK, HEAD_DIM, CMP_HID), (CMP_BLK * HEAD_DIM) ** -0.5),
        "cmp_w2_v": nrm(ks[17], (CMP_HID, HEAD_DIM), CMP_HID ** -0.5),
        "w_up_ssd": nrm(ks[18], (SSD_INNER, D_MODEL), SSD_INNER ** -0.5),
        "w_up_nsa": nrm(ks[19], (NSA_WIDTH, D_MODEL), NSA_WIDTH ** -0.5),
        "w_out": nrm(ks[20], (D_MODEL, D_MODEL), D_MODEL ** -0.5),
        "norm2_w": gain(ks[21], (D_MODEL,)),
        "mem_norm_w": gain(ks[22], (D_MODEL,)),
        "xq_w": nrm(ks[23], (D_MODEL, X_WIDTH), D_MODEL ** -0.5),
        "xkv_w": nrm(ks[24], (D_MODEL, 2 * X_WIDTH), D_MODEL ** -0.5),
        "x_q_norm_w": gain(ks[25], (HEAD_DIM,)),
        "x_k_norm_w": gain(ks[26], (HEAD_DIM,)),
        "xo_w": nrm(ks[27], (X_WIDTH, D_MODEL), X_WIDTH ** -0.5),
        "norm3_w": gain(ks[28], (D_MODEL,)),
        "router_g_w": nrm(ks[29], (D_MODEL, N_GROUPS), D_MODEL ** -0.5),
        "router_g_b": nrm(ks[30], (N_GROUPS,), 0.01),
        "router_e_w": nrm(ks[31], (D_MODEL, N_EXPERTS), D_MODEL ** -0.5),
        "router_e_b": nrm(ks[32], (N_EXPERTS,), 0.01),
        "moe_w_gate": nrm(ks[33], (N_EXPERTS, D_MODEL, EXPERT_HIDDEN), D_MODEL ** -0.5),
        "moe_w_up": nrm(ks[34], (N_EXPERTS, D_MODEL, EXPERT_HIDDEN), D_MODEL ** -0.5),
        "moe_w_down": nrm(ks[35], (N_EXPERTS, EXPERT_HIDDEN, D_MODEL), EXPERT_HIDDEN ** -0.5),
    }


def reference(x, mem, norm1_w, w_in, ssd_conv_w, ssd_conv_b, ssd_dt_bias, ssd_a_log, ssd_d, ssd_norm_w,
              nsa_q_norm_w, nsa_k_norm_w, cmp_pe_k, cmp_w1_k, cmp_w2_k, cmp_pe_v, cmp_w1_v, cmp_w2_v,
              w_up_ssd, w_up_nsa, w_out, norm2_w, mem_norm_w, xq_w, xkv_w, x_q_norm_w, x_k_norm_w, xo_w,
              norm3_w, router_g_w, router_g_b, router_e_w, router_e_b, moe_w_gate, moe_w_up, moe_w_down):
    h = x
    for l in range(DEPTH):
        h = hybrid_mixer(h, norm1_w[l], w_in[l], ssd_conv_w[l], ssd_conv_b[l], ssd_dt_bias[l], ssd_a_log[l],
                         ssd_d[l], ssd_norm_w[l], nsa_q_norm_w[l], nsa_k_norm_w[l], cmp_pe_k[l], cmp_w1_k[l],
                         cmp_w2_k[l], cmp_pe_v[l], cmp_w1_v[l], cmp_w2_v[l], w_up_ssd[l], w_up_nsa[l], w_out[l])
        h = memory_xattn(h, mem, norm2_w[l], mem_norm_w[l], xq_w[l], xkv_w[l], x_q_norm_w[l], x_k_norm_w[l], xo_w[l])
        h = hier_moe(h, norm3_w[l], router_g_w[l], router_g_b[l], router_e_w[l], router_e_b[l],
                     moe_w_gate[l], moe_w_up[l], moe_w_down[l])
    return h.astype(x.dtype)
```

```python
import numpy as np
import ml_dtypes
from contextlib import ExitStack
import concourse.bass as bass
import concourse.mybir as mybir
from concourse.bass_utils import run_bass_kernel_spmd

F32 = mybir.dt.float32
BF16 = mybir.dt.bfloat16
I32 = mybir.dt.int32
ALU = mybir.AluOpType
AF = mybir.ActivationFunctionType
AX = mybir.AxisListType

T = 2048
D = 2048
NT = 16
EPS = 1e-6
NEG = -30000.0
IN_SPLITS = (4096, 6144, 64, 2048, 512, 512, 512, 512, 512, 512, 48, 2048, 2048)
IN_COLS = sum(IN_SPLITS)
SEG_NAMES = ("z", "xbc", "dt", "q", "kc", "vc", "ks", "vs", "kw", "vw", "nsag", "gssd", "gnsa")
SEG_OFF = dict(zip(SEG_NAMES, np.cumsum((0,) + IN_SPLITS[:-1]).tolist()))
SEG_LEN = dict(zip(SEG_NAMES, IN_SPLITS))
CG = 384
NGRP = 8
EH = 1408

ENGS = ("pe", "dve", "act", "pool", "sp")


class Buf:
    __slots__ = ("name", "w", "rs")

    def __init__(self, name):
        self.name = name
        self.w = None
        self.rs = []


class Tl:
    def __init__(self, t, name):
        self.t = t
        self.b = Buf(name)

    def __getitem__(self, k):
        return self.t[k]


class Op:
    __slots__ = ("eng", "fn", "deps", "marked", "idx", "is_dma", "dsem", "dval")

    def __init__(self, eng, fn):
        self.eng = eng
        self.fn = fn
        self.deps = []
        self.marked = False
        self.idx = -1
        self.is_dma = False
        self.dsem = None
        self.dval = 0


class Prog:
    def __init__(self, nc, es):
        self.nc = nc
        self.es = es
        self.ops = {e: [] for e in ENGS}
        self.sems = {e: es.enter_context(nc.semaphore("s_" + e)) for e in ENGS}
        self.waited = {e: {f: -1 for f in ENGS} for e in ENGS}
        self.waited_d = {e: {} for e in ENGS}
        self.keymap = {}
        self.sem_pool = []
        self.sem_count = []
        self.sem_free = []
        self.last_dma = {}
        self.scopes = [es]

    def sb(self, name, shape, dtype):
        t = self.scopes[-1].enter_context(self.nc.sbuf_tensor("sb_" + name, list(shape), dtype))
        return Tl(t, name)

    def push(self):
        self.scopes.append(ExitStack())

    def pop(self):
        self.barrier()
        self.scopes.pop().close()

    def ps(self, name, shape, dtype):
        t = self.es.enter_context(self.nc.psum_tensor("ps_" + name, list(shape), dtype))
        return Tl(t, name)

    def dram(self, name, shape, dtype, kind="Internal"):
        t = self.nc.dram_tensor(name, list(shape), dtype, kind=kind)
        return Tl(t, name)

    def _dsem(self, key):
        if key not in self.keymap:
            if self.sem_free:
                idx = self.sem_free.pop()
            else:
                idx = len(self.sem_pool)
                self.sem_pool.append(self.es.enter_context(self.nc.semaphore("d_%d" % idx)))
                self.sem_count.append(0)
            self.keymap[key] = idx
        return self.keymap[key]

    def op(self, eng, fn, r=(), w=(), selfsync=False):
        o = Op(eng, fn)
        o.idx = len(self.ops[eng])
        raw = []
        war = []
        for t in r:
            if t.b.w is not None:
                raw.append(t.b.w)
        for t in w:
            if t.b.w is not None:
                raw.append(t.b.w)
            war.extend(t.b.rs)
        for d, is_raw in [(d, True) for d in raw] + [(d, False) for d in war]:
            if d is o:
                continue
            if d.is_dma:
                cur = self.waited_d[eng].get(d.dsem, 0)
                if d.dval > cur:
                    self.waited_d[eng][d.dsem] = d.dval
                    o.deps.append(d)
            else:
                if d.eng == eng:
                    if eng != "pe" and is_raw and d.fn is not None and d.idx > self.waited[eng][eng]:
                        self.waited[eng][eng] = d.idx
                        d.marked = True
                        o.deps.append(d)
                    continue
                if d.idx > self.waited[eng][d.eng]:
                    self.waited[eng][d.eng] = d.idx
                    d.marked = True
                    o.deps.append(d)
        for t in r:
            t.b.rs.append(o)
        for t in w:
            t.b.w = o
            t.b.rs = []
        self.ops[eng].append(o)
        return o

    def dma(self, q, out, in_, r=(), w=(), key=None, **kw):
        if key is None:
            key = r[0].b.name if r else w[0].b.name

        def fn(e):
            return e.dma_start(out=out, in_=in_, **kw)

        o = Op(q, fn)
        o.is_dma = True
        o.idx = len(self.ops[q])
        deps = []
        for t in r:
            if t.b.w is not None:
                deps.append(t.b.w)
        for t in w:
            if t.b.w is not None:
                deps.append(t.b.w)
            deps.extend(t.b.rs)
        for d in deps:
            if d.is_dma:
                cur = self.waited_d[q].get(d.dsem, 0)
                if d.dval > cur:
                    self.waited_d[q][d.dsem] = d.dval
                    o.deps.append(d)
            else:
                if d.eng == q:
                    d.marked = True
                    o.deps.append(d)
                elif d.idx > self.waited[q][d.eng]:
                    self.waited[q][d.eng] = d.idx
                    d.marked = True
                    o.deps.append(d)
        sidx = self._dsem(key)
        o.dsem = self.sem_pool[sidx]
        self.sem_count[sidx] += 16
        o.dval = self.sem_count[sidx]
        self.last_dma[key] = o
        for t in r:
            t.b.rs.append(o)
        for t in w:
            t.b.w = o
            t.b.rs = []
        self.ops[q].append(o)
        return o

    def dma_fn(self, q, fn, r=(), w=(), key=None):
        o = self.dma(q, None, None, r=r, w=w, key=key)
        o.fn = fn
        return o

    def store(self, q, dram_tl, out, src_tl, in_, **kw):
        return self.dma(q, out, in_, r=[src_tl], w=[dram_tl], key=src_tl.b.name, **kw)

    def load(self, q, dst_tl, out, in_, src=None, **kw):
        return self.dma(q, out, in_, r=([src] if src is not None else []), w=[dst_tl], key=dst_tl.b.name, **kw)

    def barrier(self):
        lasts = {}
        for f in ENGS:
            for o in reversed(self.ops[f]):
                if o.fn is not None and not o.is_dma:
                    lasts[f] = o
                    break
        for e in ENGS:
            o = Op(e, None)
            o.idx = len(self.ops[e])
            for f, lo in lasts.items():
                if f == e:
                    continue
                if lo.idx > self.waited[e][f]:
                    self.waited[e][f] = lo.idx
                    lo.marked = True
                    o.deps.append(lo)
            for key, d in self.last_dma.items():
                cur = self.waited_d[e].get(d.dsem, 0)
                if d.dval > cur:
                    self.waited_d[e][d.dsem] = d.dval
                    o.deps.append(d)
            self.ops[e].append(o)
        self.last_dma = {}
        self.keymap = {}
        self.sem_free = list(range(len(self.sem_pool)))

    def emit(self):
        nc = self.nc
        val = {}
        for e in ENGS:
            c = 0
            for o in self.ops[e]:
                if o.marked and not o.is_dma:
                    c += 1
                    val[id(o)] = c
        final_waits = [(self.sem_pool[i], self.sem_count[i]) for i in range(len(self.sem_pool))]

        def run(ename, eng):
            for o in self.ops[ename]:
                for d in o.deps:
                    if d.is_dma:
                        eng.wait_ge(d.dsem, d.dval)
                    else:
                        eng.wait_ge(self.sems[d.eng], val[id(d)])
                if o.fn is None:
                    continue
                ins = o.fn(eng)
                if o.is_dma:
                    ins.then_inc(o.dsem, 16)
                elif o.marked:
                    ins.then_inc(self.sems[ename], 1)
            if ename == "sp":
                for s, v in final_waits:
                    if v > 0:
                        eng.wait_ge(s, v)

        with nc.Block() as block:
            @block.tensor
            def _(pe):
                run("pe", pe)

            @block.vector
            def _(dve):
                run("dve", dve)

            @block.scalar
            def _(act):
                run("act", act)

            @block.gpsimd
            def _(pool):
                run("pool", pool)

            @block.sync
            def _(sp):
                run("sp", sp)


class Ring:
    def __init__(self, tiles):
        self.tiles = tiles
        self.i = 0

    def next(self):
        t = self.tiles[self.i % len(self.tiles)]
        self.i += 1
        return t


def seg_blocks():
    out = []
    for s in SEG_NAMES:
        off, ln = SEG_OFF[s], SEG_LEN[s]
        c = 0
        while c < ln:
            n = min(512, ln - c)
            out.append((s, off + c, n, c))
            c += n
    return out


def build(dbg=(), stop=None, ext=()):
    nc = bass.Bass("TRN2", target_bir_lowering=False)
    es = ExitStack()
    P = Prog(nc, es)
    dbg = set(dbg)

    def dten(name, shape, dtype):
        kind = "ExternalOutput" if name in dbg else ("ExternalInput" if name in ext else "Internal")
        return P.dram(name, shape, dtype, kind=kind)

    def inp(name, shape, dtype=F32):
        return P.dram(name, shape, dtype, kind="ExternalInput")

    x_in = inp("x", [T, D])
    w_in = inp("w_in", [D, IN_COLS])
    norm1_bc = inp("norm1_w", [1, D])
    convw = inp("conv_w", [128, 48, 4])
    convb = inp("conv_b", [128, 48])
    qnw = inp("qnw", [1, 128])
    knw = inp("knw", [3, 128])
    ropec = inp("rope_cos", [128, NT, 64])
    ropes = inp("rope_sin", [128, NT, 64])
    ident_in = inp("ident", [128, 128], BF16)
    out_t = P.dram("out", [T, D], F32, kind="ExternalOutput")

    zs = dten("zs", [T, 4096], BF16)
    xs = dten("xs", [T, 4096], BF16)
    BTs = dten("BTs", [1024, T], BF16)
    CTs = dten("CTs", [1024, T], BF16)
    Btok = dten("Btok", [T, 1024], BF16)
    qT = dten("qT", [2048, T], BF16)
    kTc = dten("kTc", [512, T], BF16)
    kTs = dten("kTs", [512, T], BF16)
    kTw = dten("kTw", [512, T], BF16)
    vcT = dten("vcT", [512, T], BF16)
    vstok = dten("vstok", [T, 512], BF16)
    vwtok = dten("vwtok", [T, 512], BF16)
    gsT = dten("gsT", [2048, T], BF16)
    gnT = dten("gnT", [2048, T], BF16)
    dtraw_d = dten("dtraw", [T, 64], F32)

    ident = P.sb("ident", [128, 128], BF16)
    P.dma("sp", ident[:, :], ident_in[:, :], w=[ident])
    gT = P.sb("gT", [48, T], BF16)
    junk = P.sb("junk", [128, D], BF16)
    ss_r = Ring([P.sb("ss%d" % i, [128, 1], F32) for i in range(2)])
    hn_r = Ring([P.sb("hn%d" % i, [128, D], BF16) for i in range(2)])
    stg_r = Ring([P.sb("stg%d" % i, [128, 512], BF16) for i in range(3)])
    f32a = P.sb("f32a", [128, 512], F32)
    f32b = P.sb("f32b", [128, 512], F32)
    P.push()
    wb1 = P.sb("wb1", [128, D], F32)
    P.dma("sp", wb1[:, :], norm1_bc[0:1, :].partition_broadcast(128), w=[wb1])
    hnT = P.sb("hnT", [128, 16, T], BF16)
    cw = P.sb("cw", [128, 48, 4], F32)
    P.dma("sp", cw[:, :, :], convw[:, :, :], w=[cw])
    cbias = P.sb("cb", [128, 48], F32)
    P.dma("sp", cbias[:, :], convb[:, :], w=[cbias])
    rc = P.sb("rc", [128, NT, 64], F32)
    rs_ = P.sb("rs", [128, NT, 64], F32)
    P.dma("sp", rc[:, :, :], ropec[:, :, :], w=[rc])
    P.dma("sp", rs_[:, :, :], ropes[:, :, :], w=[rs_])
    nwq = P.sb("nwq", [128, 4, 128], F32)
    nwk = [P.sb("nwk%d" % i, [128, 4, 128], F32) for i in range(3)]
    for hh in range(4):
        P.dma("sp", nwq[:, hh, :], qnw[0:1, :].partition_broadcast(128), w=[nwq])
        for i in range(3):
            P.dma("sp", nwk[i][:, hh, :], knw[i:i + 1, :].partition_broadcast(128), w=[nwk[i]])

    psf = Ring([P.ps("psf%d" % i, [128, 512], F32) for i in range(6)])
    psb = Ring([P.ps("psb%d" % i, [128, 1024], BF16) for i in range(2)])

    xt_r = Ring([P.sb("xt%d" % i, [128, D], F32) for i in range(2)])

    def rmsnorm_tile(src_tl, wb_tl, dst_tl, width=D, woff=0):
        ss = ss_r.next()
        P.op("dve", lambda e, ss=ss: e.memset(ss[:, 0:1], 0.0), w=[ss])
        P.op("act", lambda e, ss=ss: e.activation(out=junk[:, :width], in_=src_tl[:, :width], func=AF.Square,
                                                   accum_out=ss[:, 0:1]), r=[src_tl], w=[junk, ss])
        P.op("dve", lambda e, ss=ss: e.tensor_scalar(out=ss[:, 0:1], in0=ss[:, 0:1], scalar1=1.0 / width,
                                                      scalar2=EPS, op0=ALU.mult, op1=ALU.add), r=[ss], w=[ss])
        P.op("act", lambda e, ss=ss: e.activation(out=ss[:, 0:1], in_=ss[:, 0:1], func=AF.Sqrt), r=[ss], w=[ss])
        P.op("dve", lambda e, ss=ss: e.reciprocal(out=ss[:, 0:1], in_=ss[:, 0:1]), r=[ss], w=[ss])
        P.op("dve", lambda e, ss=ss: e.scalar_tensor_tensor(out=dst_tl[:, :width], in0=src_tl[:, :width],
                                                             scalar=ss[:, 0:1], in1=wb_tl[:, woff:woff + width],
                                                             op0=ALU.mult, op1=ALU.mult),
             r=[src_tl, ss, wb_tl], w=[dst_tl], selfsync=True)

    def transpose_into(src_tl, nblk, dst_fn, dst_tl):
        for half in range((nblk + 7) // 8):
            pb = psb.next()
            nb = min(8, nblk - half * 8)
            for j in range(nb):
                kc = half * 8 + j
                P.op("pe", lambda e, pb=pb, j=j, kc=kc: e.transpose(out=pb[:, j * 128:(j + 1) * 128],
                                                                     in_=src_tl[:, kc * 128:(kc + 1) * 128],
                                                                     identity=ident[:, :]),
                     r=[src_tl, ident], w=[pb])
            P.op("act", lambda e, pb=pb, half=half, nb=nb: e.copy(
                out=dst_fn(half, nb), in_=pb[:, :nb * 128].rearrange("p (k t) -> p k t", t=128)),
                r=[pb], w=[dst_tl])

    for i in range(NT):
        xt = xt_r.next()
        hn = hn_r.next()
        P.dma("sp", xt[:, :], x_in[i * 128:(i + 1) * 128, :], w=[xt])
        rmsnorm_tile(xt, wb1, hn)
        transpose_into(hn, 16, lambda half, nb, i=i: hnT[:, half * 8:half * 8 + nb, i * 128:(i + 1) * 128], hnT)

    wt_r = Ring([P.sb("wt%d" % i, [128, 16, 512], BF16) for i in range(2)])
    rstd4 = P.sb("rstd4", [128, 4], F32)
    xpad = P.sb("xpad", [128, 3 + T], F32)
    acc = P.sb("acc", [128, T], F32)
    xc = P.sb("xc", [128, T], BF16)
    tstg = P.sb("tstg", [128, 16, 128], BF16)
    P.op("dve", lambda e: e.memset(xpad[:, 0:3], 0.0), w=[xpad])
    w_view = w_in.t.ap().rearrange("(kc p) n -> p kc n", p=128)

    def mm_tok(wt, n, i, pt):
        for kc in range(16):
            P.op("pe", lambda e, kc=kc: e.matmul(pt[:, :n], lhsT=hnT[:, kc, i * 128:(i + 1) * 128],
                                                 rhs=wt[:, kc, :n], start=(kc == 0), stop=(kc == 15)),
                 r=[hnT, wt], w=[pt])

    def mm_feat(wt, j, m, tb, pt):
        for kc in range(16):
            P.op("pe", lambda e, kc=kc: e.matmul(pt[:m, :], lhsT=wt[:, kc, j * 128:j * 128 + m],
                                                 rhs=hnT[:, kc, tb * 512:(tb + 1) * 512],
                                                 start=(kc == 0), stop=(kc == 15)),
                 r=[hnT, wt], w=[pt])

    def qk_epilogue(pt, i, nw_tl, dstT, c0r):
        P.op("dve", lambda e: e.memset(rstd4[:, :], 0.0), w=[rstd4])
        for hh in range(4):
            P.op("act", lambda e, hh=hh: e.activation(out=f32a[:, hh * 128:(hh + 1) * 128],
                                                      in_=pt[:, hh * 128:(hh + 1) * 128], func=AF.Square,
                                                      accum_out=rstd4[:, hh:hh + 1]), r=[pt], w=[f32a, rstd4])
        P.op("dve", lambda e: e.tensor_scalar(out=rstd4[:, :], in0=rstd4[:, :], scalar1=1.0 / 128, scalar2=EPS,
                                              op0=ALU.mult, op1=ALU.add), r=[rstd4], w=[rstd4])
        P.op("act", lambda e: e.activation(out=rstd4[:, :], in_=rstd4[:, :], func=AF.Sqrt), r=[rstd4], w=[rstd4])
        P.op("dve", lambda e: e.reciprocal(out=rstd4[:, :], in_=rstd4[:, :]), r=[rstd4], w=[rstd4])
        P.op("dve", lambda e: e.tensor_tensor(out=f32a[:, :].rearrange("p (h d) -> p h d", d=128),
                                              in0=pt[:, :].rearrange("p (h d) -> p h d", d=128),
                                              in1=rstd4[:, :].unsqueeze(2).to_broadcast([128, 4, 128]),
                                              op=ALU.mult), r=[pt, rstd4], w=[f32a], selfsync=True)
        P.op("dve", lambda e: e.tensor_tensor(out=f32a[:, :], in0=f32a[:, :],
                                              in1=nw_tl[:, :, :].rearrange("p h d -> p (h d)"),
                                              op=ALU.mult), r=[f32a, nw_tl], w=[f32a])
        v = f32a[:, :].rearrange("p (h t d) -> p h t d", h=4, t=2)
        o = f32b[:, :].rearrange("p (h t d) -> p h t d", h=4, t=2)
        cosb = rc[:, i, :].unsqueeze(1).to_broadcast([128, 4, 64])
        sinb = rs_[:, i, :].unsqueeze(1).to_broadcast([128, 4, 64])
        hnq = hn_r.next()
        hv = hnq[:, 0:512].rearrange("p (h t d) -> p h t d", h=4, t=2)
        P.op("dve", lambda e: e.tensor_tensor(out=o[:, :, 0, :], in0=v[:, :, 0, :], in1=cosb, op=ALU.mult),
             r=[f32a, rc], w=[f32b])
        P.op("dve", lambda e: e.tensor_tensor(out=o[:, :, 1, :], in0=v[:, :, 1, :], in1=cosb, op=ALU.mult),
             r=[f32a, rc], w=[f32b])
        P.op("dve", lambda e: e.tensor_tensor(out=v[:, :, 0, :], in0=v[:, :, 0, :], in1=sinb, op=ALU.mult),
             r=[f32a, rs_, f32b], w=[f32a])
        P.op("dve", lambda e: e.tensor_tensor(out=v[:, :, 1, :], in0=v[:, :, 1, :], in1=sinb, op=ALU.mult),
             r=[f32a, rs_], w=[f32a])
        P.op("dve", lambda e: e.tensor_tensor(out=hv[:, :, 0, :], in0=o[:, :, 0, :], in1=v[:, :, 1, :],
                                              op=ALU.subtract), r=[f32a, f32b], w=[hnq])
        P.op("dve", lambda e: e.tensor_tensor(out=hv[:, :, 1, :], in0=o[:, :, 1, :], in1=v[:, :, 0, :],
                                              op=ALU.add), r=[f32a, f32b], w=[hnq])
        st = stg_r.next()
        transpose_into(hnq, 4, lambda half, nb: st[:, :].rearrange("p (k t) -> p k t", t=128), st)
        P.dma("sp", dstT.t.ap()[c0r:c0r + 512, i * 128:(i + 1) * 128].rearrange("(h d) t -> d h t", d=128),
              st[:, :].rearrange("p (k t) -> p k t", t=128), r=[st], w=[dstT])

    blocks = seg_blocks()
    for (seg, c0, n, c0r) in blocks:
        wt = wt_r.next()
        P.dma("pool", wt[:, :, :n], w_view[:, :, c0:c0 + n], w=[wt])
        if seg in ("z", "vs", "vw", "dt", "q", "kc", "ks", "kw"):
            for i in range(NT):
                pt = psf.next()
                mm_tok(wt, n, i, pt)
                if seg == "z":
                    st = stg_r.next()
                    P.op("act", lambda e, pt=pt, st=st: e.activation(out=st[:, :], in_=pt[:, :], func=AF.Silu),
                         r=[pt], w=[st])
                    P.dma("sp", zs[i * 128:(i + 1) * 128, c0r:c0r + 512], st[:, :], r=[st], w=[zs])
                elif seg in ("vs", "vw"):
                    st = stg_r.next()
                    dst = vstok if seg == "vs" else vwtok
                    P.op("act", lambda e, pt=pt, st=st: e.copy(out=st[:, :], in_=pt[:, :]), r=[pt], w=[st])
                    P.dma("sp", dst[i * 128:(i + 1) * 128, :], st[:, :], r=[st], w=[dst])
                elif seg == "dt":
                    P.op("act", lambda e, pt=pt: e.copy(out=f32a[:, :64], in_=pt[:, :64]), r=[pt], w=[f32a])
                    P.dma("sp", dtraw_d[i * 128:(i + 1) * 128, :], f32a[:, :64], r=[f32a], w=[dtraw_d])
                elif seg == "q":
                    qk_epilogue(pt, i, nwq, qT, c0r)
                else:
                    bi = ("kc", "ks", "kw").index(seg)
                    qk_epilogue(pt, i, nwk[bi], (kTc, kTs, kTw)[bi], 0)
        else:
            nj = (n + 127) // 128
            for j in range(nj):
                m = min(128, n - j * 128)
                if seg == "xbc":
                    ct = (c0r + j * 128) // 128
                    for tb in range(4):
                        pt = psf.next()
                        mm_feat(wt, j, m, tb, pt)
                        P.op("act", lambda e, pt=pt, tb=tb: e.copy(out=xpad[:, 3 + tb * 512:3 + (tb + 1) * 512],
                                                                   in_=pt[:, :]), r=[pt], w=[xpad])
                    P.op("dve", lambda e, ct=ct: e.tensor_scalar(out=acc[:, :], in0=xpad[:, 0:T],
                                                                 scalar1=cw[:, ct, 0:1], scalar2=None,
                                                                 op0=ALU.mult), r=[xpad, cw], w=[acc])
                    for k in range(1, 4):
                        P.op("dve", lambda e, ct=ct, k=k: e.scalar_tensor_tensor(
                            out=acc[:, :], in0=xpad[:, k:k + T], scalar=cw[:, ct, k:k + 1], in1=acc[:, :],
                            op0=ALU.mult, op1=ALU.add), r=[xpad, cw, acc], w=[acc])
                    P.op("act", lambda e, ct=ct: e.activation(out=xc[:, :], in_=acc[:, :], func=AF.Silu,
                                                              bias=cbias[:, ct:ct + 1]), r=[acc, cbias], w=[xc])
                    if ct < 32 or ct < 40:
                        transpose_into(xc, 16, lambda half, nb: tstg[:, half * 8:half * 8 + nb, :], tstg)
                        if ct < 32:
                            dst_ap = xs.t.ap()[:, ct * 128:(ct + 1) * 128].rearrange("(i p) c -> p i c", p=128)
                            P.dma("sp", dst_ap, tstg[:, :, :], r=[tstg], w=[xs])
                        else:
                            cc = ct - 32
                            dst_ap = Btok.t.ap()[:, cc * 128:(cc + 1) * 128].rearrange("(i p) c -> p i c", p=128)
                            P.dma("sp", dst_ap, tstg[:, :, :], r=[tstg], w=[Btok])
                    if 32 <= ct < 40:
                        P.dma("sp", BTs[(ct - 32) * 128:(ct - 31) * 128, :], xc[:, :], r=[xc], w=[BTs])
                    elif ct >= 40:
                        P.dma("sp", CTs[(ct - 40) * 128:(ct - 39) * 128, :], xc[:, :], r=[xc], w=[CTs])
                else:
                    for tb in range(4):
                        pt = psf.next()
                        mm_feat(wt, j, m, tb, pt)
                        if seg == "nsag":
                            P.op("act", lambda e, pt=pt, tb=tb, m=m: e.activation(
                                out=gT[:m, tb * 512:(tb + 1) * 512], in_=pt[:m, :], func=AF.Sigmoid),
                                r=[pt], w=[gT])
                        else:
                            st = stg_r.next()
                            if seg == "vc":
                                P.op("act", lambda e, pt=pt, st=st: e.copy(out=st[:, :], in_=pt[:, :]),
                                     r=[pt], w=[st])
                                dst = vcT
                            else:
                                P.op("act", lambda e, pt=pt, st=st: e.activation(out=st[:, :], in_=pt[:, :],
                                                                                 func=AF.Sigmoid),
                                     r=[pt], w=[st])
                                dst = gsT if seg == "gssd" else gnT
                            r0 = c0r + j * 128
                            P.dma("sp", dst[r0:r0 + 128, tb * 512:(tb + 1) * 512], st[:, :], r=[st], w=[dst])

    if stop == "B":
        P.dma("sp", out_t[0:128, 0:512], f32a[:, :], r=[f32a], w=[out_t])
        P.emit()
        return nc, es

    P.pop()
    P.push()
    identf_in = inp("identf", [128, 128])
    triu_in = inp("triu", [128, 128])
    dtb_in = inp("dt_bias", [1, 64])
    alog_in = inp("a_log", [1, 64])
    dsk_in = inp("d_skip", [1, 64])
    snw_in = inp("ssd_norm_w", [1, 4096])
    acumT_d = dten("acumT_d", [64, T], F32)
    yT = dten("yT", [4096, T], BF16)

    identf = P.sb("identf", [128, 128], F32)
    triu = P.sb("triu", [128, 128], F32)
    onesf = P.sb("onesf", [128, 128], F32)
    P.load("sp", identf, identf[:, :], identf_in[:, :])
    P.load("sp", triu, triu[:, :], triu_in[:, :])
    P.op("dve", lambda e: e.memset(onesf[:, :], 1.0), w=[onesf])
    dtb = P.sb("dtb", [128, 64], F32)
    aneg = P.sb("aneg", [128, 64], F32)
    dskb = P.sb("dskb", [128, 64], F32)
    snw = P.sb("snw", [128, 4096], F32)
    P.load("sp", dtb, dtb[:, :], dtb_in[0:1, :].partition_broadcast(128))
    P.load("sp", aneg, aneg[:, :], alog_in[0:1, :].partition_broadcast(128))
    P.load("sp", dskb, dskb[:, :], dsk_in[0:1, :].partition_broadcast(128))
    P.load("sp", snw, snw[:, :], snw_in[0:1, :].partition_broadcast(128))
    P.op("act", lambda e: e.activation(out=aneg[:, :], in_=aneg[:, :], func=AF.Exp), r=[aneg], w=[aneg])
    P.op("dve", lambda e: e.tensor_scalar(out=aneg[:, :], in0=aneg[:, :], scalar1=-1.0, scalar2=None,
                                          op0=ALU.mult), r=[aneg], w=[aneg])
    dt_sb = P.sb("dt_sb", [128, NT, 64], F32)
    da_sb = P.sb("da_sb", [128, NT, 64], F32)
    acum = P.sb("acum", [128, NT, 64], F32)
    nacum = P.sb("nacum", [128, NT, 64], F32)
    eacum = P.sb("eacum", [128, NT, 64], F32)
    dte = P.sb("dte", [128, NT, 64], F32)
    cdec = P.sb("cdec", [128, 8, 64], F32)
    P.load("sp", dt_sb, dt_sb[:, :, :], dtraw_d.t.ap().rearrange("(i p) h -> p i h", p=128), src=dtraw_d)
    P.op("dve", lambda e: e.tensor_tensor(out=dt_sb[:, :, :], in0=dt_sb[:, :, :],
                                          in1=dtb[:, :].unsqueeze(1).to_broadcast([128, NT, 64]), op=ALU.add),
         r=[dt_sb, dtb], w=[dt_sb])
    P.op("act", lambda e: e.activation(out=dt_sb[:, :, :], in_=dt_sb[:, :, :], func=AF.Exp), r=[dt_sb], w=[dt_sb])
    P.op("dve", lambda e: e.tensor_scalar(out=dt_sb[:, :, :], in0=dt_sb[:, :, :], scalar1=1.0, scalar2=None,
                                          op0=ALU.add), r=[dt_sb], w=[dt_sb])
    P.op("act", lambda e: e.activation(out=dt_sb[:, :, :], in_=dt_sb[:, :, :], func=AF.Ln), r=[dt_sb], w=[dt_sb])
    P.op("dve", lambda e: e.tensor_tensor(out=da_sb[:, :, :], in0=dt_sb[:, :, :],
                                          in1=aneg[:, :].unsqueeze(1).to_broadcast([128, NT, 64]), op=ALU.mult),
         r=[dt_sb, aneg], w=[da_sb])
    for c in range(8):
        i0, i1 = 2 * c, 2 * c + 1
        p0 = psf.next()
        P.op("pe", lambda e, p0=p0, i0=i0: e.matmul(p0[:, :64], lhsT=triu[:, :], rhs=da_sb[:, i0, :],
                                                    start=True, stop=True), r=[triu, da_sb], w=[p0])
        P.op("act", lambda e, p0=p0, i0=i0: e.copy(out=acum[:, i0, :], in_=p0[:, :64]), r=[p0], w=[acum])
        p1 = psf.next()
        P.op("pe", lambda e, p1=p1, i0=i0: e.matmul(p1[:, :64], lhsT=onesf[:, :], rhs=da_sb[:, i0, :],
                                                    start=True, stop=False), r=[onesf, da_sb], w=[p1])
        P.op("pe", lambda e, p1=p1, i1=i1: e.matmul(p1[:, :64], lhsT=triu[:, :], rhs=da_sb[:, i1, :],
                                                    start=False, stop=True), r=[triu, da_sb], w=[p1])
        P.op("act", lambda e, p1=p1, i1=i1: e.copy(out=acum[:, i1, :], in_=p1[:, :64]), r=[p1], w=[acum])
        p2 = psf.next()
        P.op("pe", lambda e, p2=p2, i0=i0: e.matmul(p2[:, :64], lhsT=onesf[:, :], rhs=da_sb[:, i0, :],
                                                    start=True, stop=False), r=[onesf, da_sb], w=[p2])
        P.op("pe", lambda e, p2=p2, i1=i1: e.matmul(p2[:, :64], lhsT=onesf[:, :], rhs=da_sb[:, i1, :],
                                                    start=False, stop=True), r=[onesf, da_sb], w=[p2])
        for ii in (i0, i1):
            P.op("dve", lambda e, p2=p2, ii=ii: e.tensor_tensor(out=dte[:, ii, :], in0=p2[:, :64],
                                                                in1=acum[:, ii, :], op=ALU.subtract),
                 r=[p2, acum], w=[dte])
        P.op("act", lambda e, p2=p2, c=c: e.activation(out=cdec[:, c, :], in_=p2[:, :64], func=AF.Exp),
             r=[p2], w=[cdec])
    P.op("act", lambda e: e.activation(out=dte[:, :, :], in_=dte[:, :, :], func=AF.Exp), r=[dte], w=[dte])
    P.op("dve", lambda e: e.tensor_tensor(out=dte[:, :, :], in0=dte[:, :, :], in1=dt_sb[:, :, :], op=ALU.mult),
         r=[dte, dt_sb], w=[dte])
    P.op("act", lambda e: e.activation(out=eacum[:, :, :], in_=acum[:, :, :], func=AF.Exp), r=[acum], w=[eacum])
    P.op("dve", lambda e: e.tensor_scalar(out=nacum[:, :, :], in0=acum[:, :, :], scalar1=-1.0, scalar2=None,
                                          op0=ALU.mult), r=[acum], w=[nacum])
    acT = P.sb("acT", [64, T], F32)
    for i in range(NT):
        pt = psf.next()
        P.op("pe", lambda e, pt=pt, i=i: e.transpose(out=pt[:64, :128], in_=acum[:, i, :], identity=identf[:, :]),
             r=[acum, identf], w=[pt])
        P.op("act", lambda e, pt=pt, i=i: e.copy(out=acT[:, i * 128:(i + 1) * 128], in_=pt[:64, :128]),
             r=[pt], w=[acT])
    P.store("sp", acumT_d, acumT_d[:, :], acT, acT[:, :])
    P.barrier()

    BTc = P.sb("BTc", [128, 8, 256], BF16)
    CTc = P.sb("CTc", [128, 8, 256], BF16)
    Btc = P.sb("Btc", [128, 2, 1024], BF16)
    xsc = P.sb("xsc", [128, 2, 4096], BF16)
    zsc = P.sb("zsc", [128, 2, 4096], BF16)
    xsd = P.sb("xsd", [128, 2, 4096], BF16)
    Hs = P.sb("Hs", [128, 8, 512], F32)
    Hb = P.sb("Hb", [128, 8, 512], BF16)
    P.op("dve", lambda e: e.memset(Hs[:, :, :], 0.0), w=[Hs])
    P.op("dve", lambda e: e.memset(Hb[:, :, :], 0.0), w=[Hb])
    bc_r = Ring([P.sb("bc%d" % i, [128, 8, 256], F32) for i in range(2)])
    cbTm = P.sb("cbTm", [128, 2, 256], F32)
    dif_r = Ring([P.sb("dif%d" % i, [128, 256], F32) for i in range(2)])
    MT_r = Ring([P.sb("MT%d" % i, [128, 2, 256], BF16) for i in range(3)])
    yA = P.sb("yA", [128, 512], F32)
    yB = P.sb("yB", [128, 512], F32)
    ynb = P.sb("ynb", [128, 512], BF16)
    BT_v = BTs.t.ap().rearrange("(g n) t -> n g t", n=128)
    CT_v = CTs.t.ap().rearrange("(g n) t -> n g t", n=128)
    for c in range(8):
        t0 = c * 256
        P.load("sp", BTc, BTc[:, :, :], BT_v[:, :, t0:t0 + 256], src=BTs)
        P.load("sp", CTc, CTc[:, :, :], CT_v[:, :, t0:t0 + 256], src=CTs)
        P.load("sp", Btc, Btc[:, :, :], Btok.t.ap()[t0:t0 + 256, :].rearrange("(i p) n -> p i n", p=128), src=Btok)
        P.load("sp", xsc, xsc[:, :, :], xs.t.ap()[t0:t0 + 256, :].rearrange("(i p) n -> p i n", p=128), src=xs)
        P.load("sp", zsc, zsc[:, :, :], zs.t.ap()[t0:t0 + 256, :].rearrange("(i p) n -> p i n", p=128), src=zs)
        for lt in range(2):
            ii = 2 * c + lt
            P.op("dve", lambda e, lt=lt, ii=ii: e.tensor_tensor(
                out=xsd[:, lt, :].rearrange("p (h d) -> p h d", d=64),
                in0=xsc[:, lt, :].rearrange("p (h d) -> p h d", d=64),
                in1=dte[:, ii, :].unsqueeze(2).to_broadcast([128, 64, 64]), op=ALU.mult),
                r=[xsc, dte], w=[xsd])
        for g in range(8):
            bc = bc_r.next()
            P.load("pool", bc, bc[:, :, :],
                   acumT_d.t.ap()[g * 8:(g + 1) * 8, t0:t0 + 256].partition_broadcast(128),
                   src=acumT_d)
            for st in range(2):
                pc = psf.next()
                P.op("pe", lambda e, pc=pc, st=st, g=g: e.matmul(pc[:, :256], lhsT=BTc[:, g, st * 128:(st + 1) * 128],
                                                                 rhs=CTc[:, g, :], start=True, stop=True),
                     r=[BTc, CTc], w=[pc])
                if st == 0:
                    P.op("dve", lambda e, pc=pc: e.tensor_tensor(out=cbTm[:, 0, 0:128], in0=pc[:, 0:128],
                                                                 in1=triu[:, :], op=ALU.mult),
                         r=[pc, triu], w=[cbTm])
                    P.op("act", lambda e, pc=pc: e.copy(out=cbTm[:, 0, 128:256], in_=pc[:, 128:256]),
                         r=[pc], w=[cbTm])
                else:
                    P.op("dve", lambda e, pc=pc: e.tensor_tensor(out=cbTm[:, 1, 128:256], in0=pc[:, 128:256],
                                                                 in1=triu[:, :], op=ALU.mult),
                         r=[pc, triu], w=[cbTm])
            pst = psf.next()
            for lt in range(2):
                P.op("pe", lambda e, lt=lt, g=g, pst=pst: e.matmul(pst[:, :], lhsT=Btc[:, lt, g * 128:(g + 1) * 128],
                                                                   rhs=xsd[:, lt, g * 512:(g + 1) * 512],
                                                                   start=(lt == 0), stop=(lt == 1)),
                     r=[Btc, xsd], w=[pst])
            poff = [psf.next(), psf.next()]
            for lt in range(2):
                P.op("pe", lambda e, lt=lt, g=g, po=poff[lt]: e.matmul(po[:, :], lhsT=CTc[:, g, lt * 128:(lt + 1) * 128],
                                                                       rhs=Hb[:, g, :], start=True, stop=True),
                     r=[CTc, Hb], w=[poff[lt]])
            P.op("dve", lambda e, g=g, c=c: e.tensor_tensor(
                out=Hs[:, g, :].rearrange("p (h d) -> p h d", d=64),
                in0=Hs[:, g, :].rearrange("p (h d) -> p h d", d=64),
                in1=cdec[:, c, g * 8:(g + 1) * 8].unsqueeze(2).to_broadcast([128, 8, 64]), op=ALU.mult),
                r=[Hs, cdec], w=[Hs])
            P.op("dve", lambda e, g=g, pst=pst: e.tensor_tensor(out=Hs[:, g, :], in0=Hs[:, g, :], in1=pst[:, :],
                                                                op=ALU.add), r=[Hs, pst], w=[Hs])
            P.op("act", lambda e, g=g: e.copy(out=Hb[:, g, :], in_=Hs[:, g, :]), r=[Hs, poff[0], poff[1]], w=[Hb])
            yoff = [yA, yB]
            for lt in range(2):
                ii = 2 * c + lt
                P.op("dve", lambda e, lt=lt, ii=ii, g=g, po=poff[lt]: e.tensor_tensor(
                    out=yoff[lt][:, :].rearrange("p (h d) -> p h d", d=64),
                    in0=po[:, :].rearrange("p (h d) -> p h d", d=64),
                    in1=eacum[:, ii, g * 8:(g + 1) * 8].unsqueeze(2).to_broadcast([128, 8, 64]), op=ALU.mult),
                    r=[poff[lt], eacum], w=[yoff[lt]])
            pd = [psf.next(), psf.next()]
            for hh in range(8):
                h = g * 8 + hh
                MT = MT_r.next()
                for st in range(2):
                    ii = 2 * c + st
                    l0 = 0 if st == 0 else 128
                    dif = dif_r.next()
                    P.op("dve", lambda e, dif=dif, bc=bc, hh=hh, ii=ii, h=h, l0=l0: e.tensor_scalar(
                        out=dif[:, l0:256], in0=bc[:, hh, l0:256], scalar1=nacum[:, ii, h:h + 1], scalar2=0.0,
                        op0=ALU.add, op1=ALU.min), r=[bc, nacum], w=[dif])
                    P.op("act", lambda e, dif=dif, l0=l0: e.activation(out=dif[:, l0:256], in_=dif[:, l0:256],
                                                                       func=AF.Exp), r=[dif], w=[dif])
                    P.op("dve", lambda e, dif=dif, MT=MT, st=st, ii=ii, h=h, l0=l0: e.scalar_tensor_tensor(
                        out=MT[:, st, l0:256], in0=dif[:, l0:256], scalar=dt_sb[:, ii, h:h + 1],
                        in1=cbTm[:, st, l0:256], op0=ALU.mult, op1=ALU.mult),
                        r=[dif, dt_sb, cbTm], w=[MT])
                for lt in range(2):
                    for st in range(lt + 1):
                        P.op("pe", lambda e, lt=lt, st=st, MT=MT, hh=hh, h=h, pdl=pd[lt]: e.matmul(
                            pdl[:, hh * 64:(hh + 1) * 64], lhsT=MT[:, st, lt * 128:(lt + 1) * 128],
                            rhs=xsc[:, st, h * 64:(h + 1) * 64], start=(st == 0), stop=(st == lt)),
                            r=[MT, xsc], w=[pd[lt]])
            for lt in range(2):
                ii = 2 * c + lt
                yt = yoff[lt]
                P.op("dve", lambda e, yt=yt, lt=lt, pdl=pd[lt]: e.tensor_tensor(out=yt[:, :], in0=yt[:, :], in1=pdl[:, :],
                                                                    op=ALU.add), r=[yt, pd[lt]], w=[yt])
                P.op("dve", lambda e, lt=lt, g=g: e.tensor_tensor(
                    out=f32a[:, :].rearrange("p (h d) -> p h d", d=64),
                    in0=xsc[:, lt, g * 512:(g + 1) * 512].rearrange("p (h d) -> p h d", d=64),
                    in1=dskb[:, g * 8:(g + 1) * 8].unsqueeze(2).to_broadcast([128, 8, 64]), op=ALU.mult),
                    r=[xsc, dskb], w=[f32a])
                P.op("dve", lambda e, yt=yt: e.tensor_tensor(out=yt[:, :], in0=yt[:, :], in1=f32a[:, :], op=ALU.add),
                     r=[yt, f32a], w=[yt])
                P.op("dve", lambda e, yt=yt, lt=lt, g=g: e.tensor_tensor(out=yt[:, :], in0=yt[:, :],
                                                                         in1=zsc[:, lt, g * 512:(g + 1) * 512],
                                                                         op=ALU.mult), r=[yt, zsc], w=[yt])
                rmsnorm_tile(yt, snw, ynb, width=512, woff=g * 512)
                st_ = stg_r.next()
                transpose_into(ynb, 4, lambda half, nb, st_=st_: st_[:, :].rearrange("p (k t) -> p k t", t=128), st_)
                P.store("sp", yT, yT.t.ap()[g * 512:(g + 1) * 512, ii * 128:(ii + 1) * 128].rearrange(
                    "(k d) t -> d k t", d=128), st_, st_[:, :].rearrange("p (k t) -> p k t", t=128))
    P.pop()
    if stop == "C":
        P.dma("sp", out_t[0:128, 0:512], f32a[:, :], r=[f32a], w=[out_t])
        P.emit()
        return nc, es

    SCALE = float(128 ** -0.5)
    ynT = dten("ynT", [2048, T], BF16)
    if "dbg_sel" in dbg:
        dbg_sel = dten("dbg_sel", [4, 16, 128, 32], BF16)
        dbg_imp = dten("dbg_imp", [4, 16, 128, 32], F32)
    if "ynT" not in ext:
        pek_in = inp("cmp_pe_kT", [128, 32])
        pev_in = inp("cmp_pe_vT", [128, 32])
        w1k_in = inp("cmp_w1_k", [32, 128, 256])
        w1v_in = inp("cmp_w1_v", [32, 128, 256])
        w2k_in = inp("cmp_w2_k", [256, 128])
        w2v_in = inp("cmp_w2_v", [256, 128])
        cmask_in = inp("cmask", [128, T], BF16)
        wmask_in = inp("wmask", [128, 8, 512], BF16)
        E_in = inp("Esel", [32, 16, 128], BF16)
        sel48_in = inp("sel48", [48, 48, 128], BF16)
        ovl_in = inp("ovl", [128, 32])
        keep_in = inp("keepc", [128, 16, 32])
        addc_in = inp("addc", [128, 16, 32])
        P.push()
        kcT_sb = P.sb("kcT_sb", [128, 4, 127], BF16)
        vc_sb = P.sb("vc_sb", [128, 4, 128], BF16)
        P.push()
        raw_sb = P.sb("raw_sb", [128, 4, T], BF16)
        w1_sb = P.sb("w1_sb", [128, 32, 256], BF16)
        w2_sb = P.sb("w2_sb", [128, 2, 128], BF16)
        peT = P.sb("peT", [128, 32], F32)
        blk_all = P.sb("blk_all", [128, 32, 4, 127], BF16)
        hidT = P.sb("hidT", [128, 2, 508], BF16)
        for which in range(2):
            srcT = kTc if which == 0 else vcT
            P.load("sp", raw_sb, raw_sb[:, :, :], srcT.t.ap().rearrange("(g d) t -> d g t", d=128), src=srcT)
            P.load("pool", w1_sb, w1_sb[:, :, :], (w1k_in if which == 0 else w1v_in).t.ap().rearrange("j d f -> d j f"))
            P.load("pool", w2_sb, w2_sb[:, :, :], (w2k_in if which == 0 else w2v_in).t.ap().rearrange("(c f) d -> f c d", f=128))
            P.load("sp", peT, peT[:, :], (pek_in if which == 0 else pev_in)[:, :])
            pk = [psf.tiles[0], psf.tiles[1]]
            rv = raw_sb[:, :, :].rearrange("p g (c s) -> p g c s", s=16)
            for j in range(32):
                c0, jj = j // 16, j % 16
                P.op("dve", lambda e, c0=c0, jj=jj, j=j: e.tensor_scalar(
                    out=blk_all[:, j, :, :], in0=rv[:, :, c0:c0 + 127, jj], scalar1=peT[:, j:j + 1], scalar2=None,
                    op0=ALU.add), r=[raw_sb, peT], w=[blk_all])
            for fc in range(2):
                for g in range(4):
                    for j in range(32):
                        P.op("pe", lambda e, fc=fc, g=g, j=j: e.matmul(
                            pk[fc][:, g * 127:(g + 1) * 127], lhsT=w1_sb[:, j, fc * 128:(fc + 1) * 128],
                            rhs=blk_all[:, j, g, :], start=(j == 0), stop=(j == 31)), r=[w1_sb, blk_all], w=[pk[fc]])
            for fc in range(2):
                P.op("act", lambda e, fc=fc: e.activation(out=hidT[:, fc, :], in_=pk[fc][:, :508], func=AF.Silu),
                     r=[pk[fc]], w=[hidT])
            po_ = psf.tiles[2]
            if which == 0:
                for g in range(4):
                    for fc in range(2):
                        P.op("pe", lambda e, g=g, fc=fc: e.matmul(po_[:, g * 127:(g + 1) * 127], lhsT=w2_sb[:, fc, :],
                                                                  rhs=hidT[:, fc, g * 127:(g + 1) * 127],
                                                                  start=(fc == 0), stop=(fc == 1)),
                             r=[w2_sb, hidT], w=[po_])
                P.op("act", lambda e: e.copy(out=kcT_sb[:, :, :], in_=po_[:, :508].rearrange("p (g c) -> p g c", c=127)),
                     r=[po_], w=[kcT_sb])
            else:
                for g in range(4):
                    for fc in range(2):
                        P.op("pe", lambda e, g=g, fc=fc: e.matmul(po_[:127, g * 128:(g + 1) * 128],
                                                                  lhsT=hidT[:, fc, g * 127:(g + 1) * 127],
                                                                  rhs=w2_sb[:, fc, :], start=(fc == 0), stop=(fc == 1)),
                             r=[w2_sb, hidT], w=[po_])
                P.op("act", lambda e: e.copy(out=vc_sb[:127, :, :], in_=po_[:127, :].rearrange("p (g d) -> p g d", d=128)),
                     r=[po_], w=[vc_sb])
        P.pop()
        kTs_sb = P.sb("kTs_sb", [128, 4, T], BF16)
        kTw_sb = P.sb("kTw_sb", [128, 4, T], BF16)
        vs_sb = P.sb("vs_sb", [128, NT, 512], BF16)
        vw_sb = P.sb("vw_sb", [128, NT, 512], BF16)
        P.load("sp", kTs_sb, kTs_sb[:, :, :], kTs.t.ap().rearrange("(g d) t -> d g t", d=128), src=kTs)
        P.load("sp", kTw_sb, kTw_sb[:, :, :], kTw.t.ap().rearrange("(g d) t -> d g t", d=128), src=kTw)
        P.load("sp", vs_sb, vs_sb[:, :, :], vstok.t.ap().rearrange("(i p) c -> p i c", p=128), src=vstok)
        P.load("sp", vw_sb, vw_sb[:, :, :], vwtok.t.ap().rearrange("(i p) c -> p i c", p=128), src=vwtok)
        cmask = P.sb("cmask", [128, T], BF16)
        wmask = P.sb("wmask", [128, 8, 512], BF16)
        Esel = P.sb("Esel", [32, 16, 128], BF16)
        sel48 = P.sb("sel48", [48, 48, 128], BF16)
        ovl = P.sb("ovl", [128, 32], F32)
        keepc = P.sb("keepc", [128, 16, 32], F32)
        addc = P.sb("addc", [128, 16, 32], F32)
        P.load("sp", cmask, cmask[:, :], cmask_in[:, :])
        P.load("sp", wmask, wmask[:, :, :], wmask_in[:, :, :])
        P.load("sp", Esel, Esel[:, :, :], E_in[:, :, :])
        P.load("sp", sel48, sel48[:, :, :], sel48_in[:, :, :])
        P.load("sp", ovl, ovl[:, :], ovl_in[:, :])
        P.load("sp", keepc, keepc[:, :, :], keep_in[:, :, :])
        P.load("sp", addc, addc[:, :, :], addc_in[:, :, :])
        onesn = P.sb("onesn", [128, 128], BF16)
        P.op("dve", lambda e: e.memset(onesn[:, :], 1.0), w=[onesn])
        qg_r = Ring([P.sb("qg%d" % i, [128, 4, T], BF16) for i in range(2)])
        pT_r = Ring([P.sb("pTn%d" % i, [128, 512], BF16) for i in range(3)])
        yacc = P.sb("yacc", [128, 4, 512], F32)
        rz = P.sb("rz", [128, 512], F32)
        tmpn = P.sb("tmpn", [128, 512], F32)
        Pn = P.sb("Pn", [128, 512], F32)
        PnS = P.sb("PnS", [128, 512], F32)
        impp = P.sb("impp", [128, 32], F32)
        imp2 = P.sb("imp2", [128, 32], F32)
        m8a = P.sb("m8a", [128, 8], F32)
        m8b = P.sb("m8b", [128, 8], F32)
        selb = P.sb("selb", [128, 128], BF16)
        selbT = P.sb("selbT", [32, 512], BF16)
        ybf_r = Ring([P.sb("ybf%d" % i, [128, 512], BF16) for i in range(2)])
        Sb = [psf.tiles[0], psf.tiles[1]]
        n_po, n_pz, n_pg, n_pimp = psf.tiles[2], psf.tiles[3], psf.tiles[4], psf.tiles[5]
        qT_v = qT.t.ap().rearrange("(g r d) t -> g d r t", r=4, d=128)

        def attn_branch(qg, r, tb, units):
            n = len(units)
            qs = qg[:, r, tb * 512:(tb + 1) * 512]

            def issue_S(idx):
                kp, kl, ktl, biases, vl, vtl = units[idx]
                pS = Sb[idx % 2]
                mms = [(kl, qs, [ktl, qg])] + [(b[0], b[2], [b[1], b[3]]) for b in biases]
                for mi, (l_, r_, tls) in enumerate(mms):
                    P.op("pe", lambda e, l_=l_, r_=r_, pS=pS, mi=mi, last=(mi == len(mms) - 1), kp=kp: e.matmul(
                        pS[:kp, :], lhsT=l_, rhs=r_, start=(mi == 0), stop=last), r=tls, w=[pS])

            issue_S(0)
            pT = None
            for idx in range(n):
                if idx + 1 < n:
                    issue_S(idx + 1)
                kp, kl, ktl, biases, vl, vtl = units[idx]
                pS = Sb[idx % 2]
                pT = pT_r.next()
                P.op("act", lambda e, pS=pS, pT=pT, kp=kp: e.activation(out=pT[:kp, :], in_=pS[:kp, :], func=AF.Exp,
                                                                       scale=SCALE), r=[pS], w=[pT])
                P.op("pe", lambda e, pT=pT, vl=vl, kp=kp, idx=idx: e.matmul(n_po[:, :], lhsT=vl, rhs=pT[:kp, :],
                                                                          start=(idx == 0), stop=(idx == n - 1)),
                     r=[vtl, pT], w=[n_po])
                P.op("pe", lambda e, pT=pT, kp=kp, idx=idx: e.matmul(n_pz[:, :], lhsT=onesn[:kp, :], rhs=pT[:kp, :],
                                                                   start=(idx == 0), stop=(idx == n - 1)),
                     r=[onesn, pT], w=[n_pz])
            return pT

        def combine(r, tb, gate_row, first):
            P.op("pe", lambda e: e.matmul(n_pg[:, :], lhsT=sel48[:, gate_row, :], rhs=gT[:, tb * 512:(tb + 1) * 512],
                                          start=True, stop=True), r=[sel48, gT], w=[n_pg])
            P.op("dve", lambda e: e.tensor_scalar(out=rz[:, :], in0=n_pz[:, :], scalar1=1e-20, scalar2=None, op0=ALU.max),
                 r=[n_pz], w=[rz])
            P.op("dve", lambda e: e.reciprocal(out=rz[:, :], in_=rz[:, :]), r=[rz], w=[rz])
            P.op("dve", lambda e: e.tensor_tensor(out=tmpn[:, :], in0=n_pg[:, :], in1=rz[:, :], op=ALU.mult),
                 r=[n_pg, rz], w=[tmpn])
            if first:
                P.op("dve", lambda e: e.tensor_tensor(out=yacc[:, r, :], in0=n_po[:, :], in1=tmpn[:, :], op=ALU.mult),
                     r=[n_po, tmpn], w=[yacc])
            else:
                P.op("dve", lambda e: e.tensor_tensor(out=tmpn[:, :], in0=n_po[:, :], in1=tmpn[:, :], op=ALU.mult),
                     r=[n_po, tmpn], w=[tmpn])
                P.op("dve", lambda e: e.tensor_tensor(out=yacc[:, r, :], in0=yacc[:, r, :], in1=tmpn[:, :], op=ALU.add),
                     r=[yacc, tmpn], w=[yacc])

        def topk_tile(g, tb, a):
            qt = 4 * tb + a
            P.op("dve", lambda e: e.tensor_tensor(out=impp[:, :], in0=n_pimp[:, a * 32:(a + 1) * 32], in1=keepc[:, qt, :],
                                                  op=ALU.mult), r=[n_pimp, keepc], w=[impp])
            P.op("dve", lambda e: e.tensor_tensor(out=impp[:, :], in0=impp[:, :], in1=addc[:, qt, :], op=ALU.add),
                 r=[impp, addc], w=[impp])
            P.op("dve", lambda e: e.max(out=m8a[:, :], in_=impp[:, :]), r=[impp], w=[m8a], selfsync=True)
            P.op("dve", lambda e: e.match_replace(out=imp2[:, :], in_to_replace=m8a[:, :], in_values=impp[:, :],
                                                  imm_value=-1e30), r=[m8a, impp], w=[imp2], selfsync=True)
            P.op("dve", lambda e: e.max(out=m8b[:, :], in_=imp2[:, :]), r=[imp2], w=[m8b], selfsync=True)
            P.op("dve", lambda e: e.tensor_scalar(out=imp2[:, :], in0=impp[:, :], scalar1=m8b[:, 7:8], scalar2=30000.0,
                                                  op0=ALU.is_ge, op1=ALU.mult), r=[impp, m8b], w=[imp2], selfsync=True)
            P.op("dve", lambda e: e.tensor_scalar(out=selb[:, 0:32], in0=imp2[:, :], scalar1=-30000.0, scalar2=None,
                                                  op0=ALU.add), r=[imp2], w=[selb])
            if "dbg_sel" in dbg:
                P.store("sp", dbg_sel, dbg_sel[g, qt, :, :], selb, selb[:, 0:32])
                P.store("sp", dbg_imp, dbg_imp[g, qt, :, :], impp, impp[:, :])
            pb = psb.next()
            P.op("pe", lambda e, pb=pb: e.transpose(out=pb[:32, 0:128], in_=selb[:, 0:32], identity=ident[:, :]),
                 r=[selb, ident], w=[pb])
            P.op("act", lambda e, pb=pb: e.copy(out=selbT[:, a * 128:(a + 1) * 128], in_=pb[:32, 0:128]),
                 r=[pb], w=[selbT])

        for g in range(4):
            qg = qg_r.next()
            P.load("sp", qg, qg[:, :, :], qT_v[g], src=qT)
            for tb in range(4):
                for r in range(4):
                    head = g * 4 + r
                    units = [(127, kcT_sb[:, g, :], kcT_sb,
                              [(ident[:127, :127], ident, cmask[:127, tb * 512:(tb + 1) * 512], cmask)],
                              vc_sb[:127, g, :], vc_sb)]
                    pT = attn_branch(qg, r, tb, units)
                    combine(r, tb, 0 * 16 + head, True)
                    if tb >= 2:
                        if r == 0:
                            P.op("dve", lambda e, pT=pT: e.tensor_tensor(out=PnS[:127, :], in0=pT[:127, :],
                                                                         in1=rz[:127, :], op=ALU.mult),
                                 r=[pT, rz], w=[PnS])
                        else:
                            P.op("dve", lambda e, pT=pT: e.tensor_tensor(out=Pn[:127, :], in0=pT[:127, :],
                                                                         in1=rz[:127, :], op=ALU.mult),
                                 r=[pT, rz], w=[Pn])
                            P.op("dve", lambda e: e.tensor_tensor(out=PnS[:127, :], in0=PnS[:127, :], in1=Pn[:127, :],
                                                                  op=ALU.add), r=[PnS, Pn], w=[PnS])
                if tb >= 2:
                    for a in range(4):
                        P.op("pe", lambda e, a=a: e.matmul(n_pimp[:, a * 32:(a + 1) * 32],
                                                           lhsT=PnS[:127, a * 128:(a + 1) * 128], rhs=ovl[:127, :],
                                                           start=True, stop=True), r=[PnS, ovl], w=[n_pimp])
                if tb >= 2:
                    for a in range(4):
                        topk_tile(g, tb, a)
                for r in range(4):
                    head = g * 4 + r
                    units = []
                    for kt in range(4 * tb + 4):
                        biases = []
                        if tb >= 2:
                            biases.append((Esel[:, kt, :], Esel, selbT[:, :], selbT))
                        if kt >= 4 * tb:
                            biases.append((ident[:, :], ident, wmask[:, 4 + kt - 4 * tb, :], wmask))
                        units.append((128, kTs_sb[:, g, kt * 128:(kt + 1) * 128], kTs_sb, biases,
                                      vs_sb[:, kt, g * 128:(g + 1) * 128], vs_sb))
                    attn_branch(qg, r, tb, units)
                    combine(r, tb, 1 * 16 + head, False)
                    units = []
                    for kt in range(max(0, 4 * tb - 4), 4 * tb + 4):
                        biases = [(ident[:, :], ident, wmask[:, 4 + kt - 4 * tb, :], wmask)]
                        units.append((128, kTw_sb[:, g, kt * 128:(kt + 1) * 128], kTw_sb, biases,
                                      vw_sb[:, kt, g * 128:(g + 1) * 128], vw_sb))
                    attn_branch(qg, r, tb, units)
                    combine(r, tb, 2 * 16 + head, False)
                    ybf = ybf_r.next()
                    P.op("act", lambda e, ybf=ybf, r=r: e.copy(out=ybf[:, :], in_=yacc[:, r, :]), r=[yacc], w=[ybf])
                    P.store("sp", ynT, ynT[head * 128:(head + 1) * 128, tb * 512:(tb + 1) * 512], ybf, ybf[:, :])
        P.pop()
    if stop == "N":
        P.dma("sp", out_t[0:128, 0:512], f32a[:, :], r=[f32a], w=[out_t])
        P.emit()
        return nc, es

    P.push()
    wus_in = inp("w_up_ssd", [4096, D])
    wun_in = inp("w_up_nsa", [2048, D])
    wo_in = inp("w_out", [D, D])
    mT_d = dten("mT_d", [2048, T], BF16)
    h1_d = dten("h1_d", [T, D], F32)
    wus_v = wus_in.t.ap().rearrange("(kc p) n -> p kc n", p=128)
    wun_v = wun_in.t.ap().rearrange("(kc p) n -> p kc n", p=128)
    yT_v = yT.t.ap().rearrange("(kc p) t -> p kc t", p=128)
    ynT_v = ynT.t.ap().rearrange("(kc p) t -> p kc t", p=128)
    wA_r = Ring([P.sb("wA%d" % i, [128, 32, 128], BF16) for i in range(2)])
    wB_r = Ring([P.sb("wB%d" % i, [128, 16, 128], BF16) for i in range(2)])
    yTb_r = Ring([P.sb("yTb%d" % i, [128, 32, 512], BF16) for i in range(2)])
    nTb_r = Ring([P.sb("nTb%d" % i, [128, 16, 512], BF16) for i in range(2)])
    gs_r = Ring([P.sb("gsb%d" % i, [128, 512], BF16) for i in range(2)])
    gn_r = Ring([P.sb("gnb%d" % i, [128, 512], BF16) for i in range(2)])
    for f in range(16):
        wA = wA_r.next()
        wB = wB_r.next()
        P.load("pool", wA, wA[:, :, :], wus_v[:, :, f * 128:(f + 1) * 128])
        P.load("pool", wB, wB[:, :, :], wun_v[:, :, f * 128:(f + 1) * 128])
        for tb in range(4):
            yTb = yTb_r.next()
            nTb = nTb_r.next()
            gsb = gs_r.next()
            gnb = gn_r.next()
            P.load("sp", yTb, yTb[:, :, :], yT_v[:, :, tb * 512:(tb + 1) * 512], src=yT)
            P.load("sp", nTb, nTb[:, :, :], ynT_v[:, :, tb * 512:(tb + 1) * 512], src=ynT)
            P.load("sp", gsb, gsb[:, :], gsT[f * 128:(f + 1) * 128, tb * 512:(tb + 1) * 512], src=gsT)
            P.load("sp", gnb, gnb[:, :], gnT[f * 128:(f + 1) * 128, tb * 512:(tb + 1) * 512], src=gnT)
            pA = psf.next()
            for kc in range(32):
                P.op("pe", lambda e, kc=kc, pA=pA, wA=wA, yTb=yTb: e.matmul(pA[:, :], lhsT=wA[:, kc, :], rhs=yTb[:, kc, :],
                                                                          start=(kc == 0), stop=(kc == 31)),
                     r=[wA, yTb], w=[pA])
            pB = psf.next()
            for kc in range(16):
                P.op("pe", lambda e, kc=kc, pB=pB, wB=wB, nTb=nTb: e.matmul(pB[:, :], lhsT=wB[:, kc, :], rhs=nTb[:, kc, :],
                                                                          start=(kc == 0), stop=(kc == 15)),
                     r=[wB, nTb], w=[pB])
            st = stg_r.next()
            P.op("dve", lambda e, pA=pA, gsb=gsb: e.tensor_tensor(out=f32a[:, :], in0=pA[:, :], in1=gsb[:, :], op=ALU.mult),
                 r=[pA, gsb], w=[f32a])
            P.op("dve", lambda e, pB=pB, gnb=gnb: e.tensor_tensor(out=f32b[:, :], in0=pB[:, :], in1=gnb[:, :], op=ALU.mult),
                 r=[pB, gnb], w=[f32b])
            P.op("dve", lambda e, st=st: e.tensor_tensor(out=st[:, :], in0=f32a[:, :], in1=f32b[:, :], op=ALU.add),
                 r=[f32a, f32b], w=[st])
            P.store("sp", mT_d, mT_d[f * 128:(f + 1) * 128, tb * 512:(tb + 1) * 512], st, st[:, :])
    P.pop()
    P.push()
    wo_sb = P.sb("wo_sb", [128, 16, D], BF16)
    wo_v = wo_in.t.ap().rearrange("(kc p) n -> p kc n", p=128)
    for cb in range(4):
        P.load("pool", wo_sb, wo_sb[:, :, cb * 512:(cb + 1) * 512], wo_v[:, :, cb * 512:(cb + 1) * 512])
    mT_v = mT_d.t.ap().rearrange("(kc p) t -> p kc t", p=128)
    mti_r = Ring([P.sb("mti%d" % i, [128, 16, 128], BF16) for i in range(2)])
    xr_r = Ring([P.sb("xr%d" % i, [128, D], F32) for i in range(2)])
    for i in range(NT):
        mti = mti_r.next()
        xr = xr_r.next()
        P.load("sp", mti, mti[:, :, :], mT_v[:, :, i * 128:(i + 1) * 128], src=mT_d)
        P.load("sp", xr, xr[:, :], x_in[i * 128:(i + 1) * 128, :])
        for cb in range(4):
            pt = psf.next()
            for kc in range(16):
                P.op("pe", lambda e, kc=kc, pt=pt, mti=mti, cb=cb: e.matmul(pt[:, :], lhsT=mti[:, kc, :],
                                                                          rhs=wo_sb[:, kc, cb * 512:(cb + 1) * 512],
                                                                          start=(kc == 0), stop=(kc == 15)),
                     r=[mti, wo_sb], w=[pt])
            P.op("dve", lambda e, pt=pt, xr=xr, cb=cb: e.tensor_tensor(out=xr[:, cb * 512:(cb + 1) * 512], in0=pt[:, :],
                                                                     in1=xr[:, cb * 512:(cb + 1) * 512], op=ALU.add),
                 r=[pt, xr], w=[xr])
        P.store("sp", h1_d, h1_d[i * 128:(i + 1) * 128, :], xr, xr[:, :])
    P.pop()
    if stop == "D":
        P.dma("sp", out_t[0:128, 0:512], f32a[:, :], r=[f32a], w=[out_t])
        P.emit()
        return nc, es

    P.push()
    mem_in = inp("mem", [256, D])
    n2_in = inp("norm2_w", [1, D])
    mn_in = inp("mem_norm_w", [1, D])
    xq_in = inp("xq_w", [D, 512])
    xkv_in = inp("xkv_w", [D, 1024])
    xqn_in = inp("x_q_norm_w", [1, 128])
    xkn_in = inp("x_k_norm_w", [1, 128])
    xo_in = inp("xo_w", [512, D])
    h2_d = dten("h2_d", [T, D], F32)
    wb2 = P.sb("wb2", [128, D], F32)
    wbm = P.sb("wbm", [128, D], F32)
    P.load("sp", wb2, wb2[:, :], n2_in[0:1, :].partition_broadcast(128))
    P.load("sp", wbm, wbm[:, :], mn_in[0:1, :].partition_broadcast(128))
    xqnw = P.sb("xqnw", [128, 4, 128], F32)
    xknw = P.sb("xknw", [128, 4, 128], F32)
    for hh in range(4):
        P.load("sp", xqnw, xqnw[:, hh, :], xqn_in[0:1, :].partition_broadcast(128))
        P.load("sp", xknw, xknw[:, hh, :], xkn_in[0:1, :].partition_broadcast(128))
    xq_sb = P.sb("xq_sb", [128, 16, 512], BF16)
    xkv_sb = P.sb("xkv_sb", [128, 16, 1024], BF16)
    xo_sb = P.sb("xo_sb", [128, 4, D], BF16)
    P.load("pool", xq_sb, xq_sb[:, :, :], xq_in.t.ap().rearrange("(kc p) n -> p kc n", p=128))
    P.load("pool", xkv_sb, xkv_sb[:, :, 0:512], xkv_in.t.ap().rearrange("(kc p) n -> p kc n", p=128)[:, :, 0:512])
    P.load("pool", xkv_sb, xkv_sb[:, :, 512:1024], xkv_in.t.ap().rearrange("(kc p) n -> p kc n", p=128)[:, :, 512:1024])
    for cb in range(4):
        P.load("pool", xo_sb, xo_sb[:, :, cb * 512:(cb + 1) * 512],
               xo_in.t.ap().rearrange("(kc p) n -> p kc n", p=128)[:, :, cb * 512:(cb + 1) * 512])
    onesb = P.sb("onesb", [128, 128], BF16)
    P.op("dve", lambda e: e.memset(onesb[:, :], 1.0), w=[onesb])
    rstd4e = P.sb("rstd4e", [128, 4], F32)
    hT_r = Ring([P.sb("hTe%d" % i, [128, 16, 128], BF16) for i in range(2)])
    h1t_r = Ring([P.sb("h1t%d" % i, [128, D], F32) for i in range(2)])
    kxT = P.sb("kxT", [128, 4, 256], BF16)
    vx = P.sb("vx", [128, 2, 512], BF16)
    qxT = P.sb("qxT", [128, 4, 128], BF16)
    pT_r = Ring([P.sb("pTe%d" % i, [128, 2, 128], BF16) for i in range(2)])
    oT = P.sb("oT", [128, 4, 128], BF16)
    rsum = P.sb("rsum", [128, 128], F32)

    def head_norm(pt, nw_tl, dst_hn):
        P.op("dve", lambda e: e.memset(rstd4e[:, :], 0.0), w=[rstd4e])
        for hh in range(4):
            P.op("act", lambda e, hh=hh: e.activation(out=f32a[:, hh * 128:(hh + 1) * 128],
                                                      in_=pt[:, hh * 128:(hh + 1) * 128], func=AF.Square,
                                                      accum_out=rstd4e[:, hh:hh + 1]), r=[pt], w=[f32a, rstd4e])
        P.op("dve", lambda e: e.tensor_scalar(out=rstd4e[:, :], in0=rstd4e[:, :], scalar1=1.0 / 128, scalar2=EPS,
                                              op0=ALU.mult, op1=ALU.add), r=[rstd4e], w=[rstd4e])
        P.op("act", lambda e: e.activation(out=rstd4e[:, :], in_=rstd4e[:, :], func=AF.Sqrt), r=[rstd4e], w=[rstd4e])
        P.op("dve", lambda e: e.reciprocal(out=rstd4e[:, :], in_=rstd4e[:, :]), r=[rstd4e], w=[rstd4e])
        P.op("dve", lambda e: e.tensor_tensor(out=f32a[:, :].rearrange("p (h d) -> p h d", d=128),
                                              in0=pt[:, :].rearrange("p (h d) -> p h d", d=128),
                                              in1=rstd4e[:, :].unsqueeze(2).to_broadcast([128, 4, 128]),
                                              op=ALU.mult), r=[pt, rstd4e], w=[f32a], selfsync=True)
        P.op("dve", lambda e: e.tensor_tensor(out=dst_hn[:, 0:512], in0=f32a[:, :],
                                              in1=nw_tl[:, :, :].rearrange("p h d -> p (h d)"),
                                              op=ALU.mult), r=[f32a, nw_tl], w=[dst_hn])

    for mt in range(2):
        h1t = h1t_r.next()
        hn = hn_r.next()
        hT = hT_r.next()
        P.load("sp", h1t, h1t[:, :], mem_in[mt * 128:(mt + 1) * 128, :])
        rmsnorm_tile(h1t, wbm, hn)
        transpose_into(hn, 16, lambda half, nb, hT=hT: hT[:, half * 8:half * 8 + nb, :], hT)
        for part in range(2):
            pt = psf.next()
            for kc in range(16):
                P.op("pe", lambda e, kc=kc, pt=pt, hT=hT, part=part: e.matmul(
                    pt[:, :], lhsT=hT[:, kc, :], rhs=xkv_sb[:, kc, part * 512:(part + 1) * 512],
                    start=(kc == 0), stop=(kc == 15)), r=[hT, xkv_sb], w=[pt])
            if part == 0:
                hk = hn_r.next()
                head_norm(pt, xknw, hk)
                transpose_into(hk, 4, lambda half, nb, mt=mt: kxT[:, :, mt * 128:(mt + 1) * 128], kxT)
            else:
                P.op("act", lambda e, pt=pt, mt=mt: e.copy(out=vx[:, mt, :], in_=pt[:, :]), r=[pt], w=[vx])
    for i in range(NT):
        h1t = h1t_r.next()
        hn = hn_r.next()
        hT = hT_r.next()
        P.load("sp", h1t, h1t[:, :], h1_d[i * 128:(i + 1) * 128, :], src=h1_d)
        rmsnorm_tile(h1t, wb2, hn)
        transpose_into(hn, 16, lambda half, nb, hT=hT: hT[:, half * 8:half * 8 + nb, :], hT)
        pt = psf.next()
        for kc in range(16):
            P.op("pe", lambda e, kc=kc, pt=pt, hT=hT: e.matmul(pt[:, :], lhsT=hT[:, kc, :], rhs=xq_sb[:, kc, :],
                                                             start=(kc == 0), stop=(kc == 15)),
                 r=[hT, xq_sb], w=[pt])
        hq = hn_r.next()
        head_norm(pt, xqnw, hq)
        transpose_into(hq, 4, lambda half, nb: qxT[:, :, :], qxT)
        for hh in range(4):
            pT = pT_r.next()
            for mt in range(2):
                psc = psf.next()
                P.op("pe", lambda e, psc=psc, hh=hh, mt=mt: e.matmul(psc[:, :128], lhsT=kxT[:, hh, mt * 128:(mt + 1) * 128],
                                                                     rhs=qxT[:, hh, :], start=True, stop=True),
                     r=[kxT, qxT], w=[psc])
                P.op("act", lambda e, psc=psc, pT=pT, mt=mt: e.activation(out=pT[:, mt, :], in_=psc[:, :128], func=AF.Exp,
                                                                         scale=float(128 ** -0.5)), r=[psc], w=[pT])
            po = psf.next()
            pz = psf.next()
            for mt in range(2):
                P.op("pe", lambda e, po=po, pT=pT, hh=hh, mt=mt: e.matmul(po[:, :128], lhsT=vx[:, mt, hh * 128:(hh + 1) * 128],
                                                                         rhs=pT[:, mt, :], start=(mt == 0), stop=(mt == 1)),
                     r=[vx, pT], w=[po])
            for mt in range(2):
                P.op("pe", lambda e, pz=pz, pT=pT, mt=mt: e.matmul(pz[:, :128], lhsT=onesb[:, :], rhs=pT[:, mt, :],
                                                                  start=(mt == 0), stop=(mt == 1)),
                     r=[onesb, pT], w=[pz])
            P.op("dve", lambda e, pz=pz: e.reciprocal(out=rsum[:, :], in_=pz[:, :128]), r=[pz], w=[rsum])
            P.op("dve", lambda e, po=po, hh=hh: e.tensor_tensor(out=oT[:, hh, :], in0=po[:, :128], in1=rsum[:, :],
                                                               op=ALU.mult), r=[po, rsum], w=[oT])
        for cb in range(4):
            pt = psf.next()
            for kc in range(4):
                P.op("pe", lambda e, kc=kc, pt=pt, cb=cb: e.matmul(pt[:, :], lhsT=oT[:, kc, :],
                                                                  rhs=xo_sb[:, kc, cb * 512:(cb + 1) * 512],
                                                                  start=(kc == 0), stop=(kc == 3)),
                     r=[oT, xo_sb], w=[pt])
            P.op("dve", lambda e, pt=pt, h1t=h1t, cb=cb: e.tensor_tensor(out=h1t[:, cb * 512:(cb + 1) * 512], in0=pt[:, :],
                                                                       in1=h1t[:, cb * 512:(cb + 1) * 512], op=ALU.add),
                 r=[pt, h1t], w=[h1t])
        P.store("sp", h2_d, h2_d[i * 128:(i + 1) * 128, :], h1t, h1t[:, :])
    P.pop()
    if stop == "E":
        P.dma("sp", out_t[0:128, 0:512], f32a[:, :], r=[f32a], w=[out_t])
        P.emit()
        return nc, es

    n3_in = inp("norm3_w", [1, D])
    rgw_in = inp("router_g_w", [D, 8])
    rew_in = inp("router_e_w", [D, 64])
    rb_in = inp("router_b", [1, 72])
    wg_in = inp("moe_w_gate", [64, D, EH])
    wu_in = inp("moe_w_up", [64, D, EH])
    wd_in = inp("moe_w_down", [64, EH, D])
    iota_in = inp("iota_row", [128, CG])
    gbase_in = inp("gbase", [128, 8])
    ustr_in = inp("ustrict", [128, 128], BF16)
    XgT_d = dten("XgT_d", [8, 128, 16, CG], BF16)
    Y_d = dten("Y_d", [8 * CG, D], F32)
    P.push()
    posm_all = P.sb("posm_all", [128, NT, 8], F32)
    wdh = P.sb("wdh", [128, NT, 16], BF16)
    ridx = P.sb("ridx", [128, NT], I32)
    wslot = P.sb("wslot", [128, 8, 3, 8], F32)
    onesb2 = P.sb("onesb2", [128, 128], BF16)
    ustr = P.sb("ustr", [128, 128], BF16)
    iota_row = P.sb("iota_row", [128, CG], F32)
    gbase = P.sb("gbase", [128, 8], F32)
    P.op("dve", lambda e: e.memset(onesb2[:, :], 1.0), w=[onesb2])
    P.load("sp", ustr, ustr[:, :], ustr_in[:, :])
    P.load("sp", iota_row, iota_row[:, :], iota_in[:, :])
    P.load("sp", gbase, gbase[:, :], gbase_in[:, :])
    P.push()
    hf_all = P.sb("hf_all", [128, NT, D], BF16)
    wb3 = P.sb("wb3", [128, D], F32)
    P.load("sp", wb3, wb3[:, :], n3_in[0:1, :].partition_broadcast(128))
    identf2 = P.sb("identf2", [128, 128], F32)
    P.load("sp", identf2, identf2[:, :], identf_in[:, :])
    wr_sb = P.sb("wr_sb", [128, 16, 72], F32)
    P.load("sp", wr_sb, wr_sb[:, :, 0:8], rgw_in.t.ap().rearrange("(kc p) n -> p kc n", p=128))
    P.load("sp", wr_sb, wr_sb[:, :, 8:72], rew_in.t.ap().rearrange("(kc p) n -> p kc n", p=128))
    rbias = P.sb("rbias", [128, 72], F32)
    P.load("sp", rbias, rbias[:, :], rb_in[0:1, :].partition_broadcast(128))
    h2t_r = Ring([P.sb("h2t%d" % i, [128, D], F32) for i in range(2)])
    hf32 = P.sb("hf32", [128, D], F32)
    hfT32 = P.sb("hfT32", [128, 16, 128], F32)
    cnt = P.sb("cnt", [128, 8], F32)
    P.op("dve", lambda e: e.memset(cnt[:, :], 0.0), w=[cnt])
    lg = P.sb("lg", [128, 72], F32)
    m8 = P.sb("m8", [128, 8], F32)
    m8e = P.sb("m8e", [128, 8], F32)
    oh = P.sb("oh", [128, 8], F32)
    ohb = P.sb("ohb", [128, 8], BF16)
    sc1 = P.sb("sc1", [128, 8], F32)
    le = P.sb("le", [128, 8], F32)
    e2 = P.sb("e2", [128, 8], F32)
    msk2 = P.sb("msk2", [128, 8], F32)
    wd32 = P.sb("wd32", [128, 8], F32)
    pos = P.sb("pos", [128, 8], F32)
    tmp8 = P.sb("tmp8", [128, 8], F32)
    junk8 = P.sb("junk8", [128, 8], F32)
    rid32 = P.sb("rid32", [128, 1], F32)

    def rms_f32(src_tl, wb_tl, dst_tl):
        ss = ss_r.next()
        P.op("dve", lambda e: e.memset(ss[:, 0:1], 0.0), w=[ss])
        P.op("act", lambda e: e.activation(out=junk[:, :], in_=src_tl[:, :], func=AF.Square, accum_out=ss[:, 0:1]),
             r=[src_tl], w=[junk, ss])
        P.op("dve", lambda e: e.tensor_scalar(out=ss[:, 0:1], in0=ss[:, 0:1], scalar1=1.0 / D, scalar2=EPS,
                                              op0=ALU.mult, op1=ALU.add), r=[ss], w=[ss])
        P.op("act", lambda e: e.activation(out=ss[:, 0:1], in_=ss[:, 0:1], func=AF.Sqrt), r=[ss], w=[ss])
        P.op("dve", lambda e: e.reciprocal(out=ss[:, 0:1], in_=ss[:, 0:1]), r=[ss], w=[ss])
        P.op("dve", lambda e: e.scalar_tensor_tensor(out=dst_tl[:, :], in0=src_tl[:, :], scalar=ss[:, 0:1],
                                                     in1=wb_tl[:, :], op0=ALU.mult, op1=ALU.mult),
             r=[src_tl, ss, wb_tl], w=[dst_tl])

    def route_tile(i):
        h2t = h2t_r.next()
        P.load("sp", h2t, h2t[:, :], h2_d[i * 128:(i + 1) * 128, :], src=h2_d)
        rms_f32(h2t, wb3, hf32)
        P.op("act", lambda e: e.copy(out=hf_all[:, i, :], in_=hf32[:, :]), r=[hf32], w=[hf_all])
        for q4 in range(4):
            pt = psf.next()
            for j in range(4):
                kc = q4 * 4 + j
                P.op("pe", lambda e, pt=pt, j=j, kc=kc: e.transpose(out=pt[:, j * 128:(j + 1) * 128],
                                                                   in_=hf32[:, kc * 128:(kc + 1) * 128],
                                                                   identity=identf2[:, :]),
                     r=[hf32, identf2], w=[pt])
            P.op("act", lambda e, pt=pt, q4=q4: e.copy(out=hfT32[:, q4 * 4:(q4 + 1) * 4, :],
                                                       in_=pt[:, :].rearrange("p (k t) -> p k t", t=128)),
                 r=[pt], w=[hfT32])
        pl = psf.next()
        for kc in range(16):
            P.op("pe", lambda e, kc=kc: e.matmul(pl[:, :72], lhsT=hfT32[:, kc, :], rhs=wr_sb[:, kc, :],
                                                 start=(kc == 0), stop=(kc == 15)), r=[hfT32, wr_sb], w=[pl])
        P.op("dve", lambda e: e.tensor_tensor(out=lg[:, :], in0=pl[:, :72], in1=rbias[:, :], op=ALU.add),
             r=[pl, rbias], w=[lg])
        P.op("dve", lambda e: e.max(out=m8[:, :], in_=lg[:, 0:8]), r=[lg], w=[m8])
        P.op("dve", lambda e: e.tensor_scalar(out=oh[:, :], in0=lg[:, 0:8], scalar1=m8[:, 0:1], scalar2=None,
                                              op0=ALU.is_ge), r=[lg, m8], w=[oh])
        P.op("act", lambda e: e.copy(out=ohb[:, :], in_=oh[:, :]), r=[oh], w=[ohb])
        P.op("dve", lambda e: e.tensor_scalar(out=sc1[:, 0:1], in0=m8[:, 0:1], scalar1=-1.0, scalar2=None,
                                              op0=ALU.mult), r=[m8], w=[sc1])
        P.op("dve", lambda e: e.memset(sc1[:, 1:2], 0.0), r=[sc1], w=[sc1])
        P.op("act", lambda e: e.activation(out=junk8[:, :], in_=lg[:, 0:8], func=AF.Exp, bias=sc1[:, 0:1],
                                           accum_out=sc1[:, 1:2]), r=[lg, sc1], w=[junk8, sc1])
        P.op("dve", lambda e: e.reciprocal(out=sc1[:, 2:3], in_=sc1[:, 1:2]), r=[sc1], w=[sc1])
        for g in range(8):
            if g == 0:
                P.op("dve", lambda e: e.tensor_scalar(out=le[:, :], in0=lg[:, 8:16], scalar1=oh[:, 0:1], scalar2=None,
                                                      op0=ALU.mult), r=[lg, oh], w=[le])
            else:
                P.op("dve", lambda e, g=g: e.scalar_tensor_tensor(out=le[:, :], in0=lg[:, 8 + g * 8:16 + g * 8],
                                                                  scalar=oh[:, g:g + 1], in1=le[:, :],
                                                                  op0=ALU.mult, op1=ALU.add), r=[lg, oh, le], w=[le])
        P.op("dve", lambda e: e.max(out=m8e[:, :], in_=le[:, :]), r=[le], w=[m8e])
        P.op("dve", lambda e: e.tensor_scalar(out=msk2[:, :], in0=le[:, :], scalar1=m8e[:, 1:2], scalar2=None,
                                              op0=ALU.is_ge), r=[le, m8e], w=[msk2])
        P.op("dve", lambda e: e.tensor_scalar(out=sc1[:, 3:4], in0=m8e[:, 0:1], scalar1=-1.0, scalar2=None,
                                              op0=ALU.mult), r=[m8e], w=[sc1])
        P.op("act", lambda e: e.activation(out=e2[:, :], in_=le[:, :], func=AF.Exp, bias=sc1[:, 3:4]),
             r=[le, sc1], w=[e2])
        P.op("act", lambda e: e.activation(out=sc1[:, 4:5], in_=m8e[:, 1:2], func=AF.Exp, bias=sc1[:, 3:4]),
             r=[m8e, sc1], w=[sc1])
        P.op("dve", lambda e: e.tensor_scalar(out=sc1[:, 4:5], in0=sc1[:, 4:5], scalar1=1.0, scalar2=None,
                                              op0=ALU.add), r=[sc1], w=[sc1])
        P.op("dve", lambda e: e.reciprocal(out=sc1[:, 4:5], in_=sc1[:, 4:5]), r=[sc1], w=[sc1])
        P.op("dve", lambda e: e.tensor_tensor(out=sc1[:, 5:6], in0=sc1[:, 4:5], in1=sc1[:, 2:3], op=ALU.mult),
             r=[sc1], w=[sc1])
        P.op("dve", lambda e: e.scalar_tensor_tensor(out=wd32[:, :], in0=e2[:, :], scalar=sc1[:, 5:6], in1=msk2[:, :],
                                                     op0=ALU.mult, op1=ALU.mult), r=[e2, sc1, msk2], w=[wd32])
        P.op("act", lambda e: e.copy(out=wdh[:, i, 0:8], in_=wd32[:, :]), r=[wd32], w=[wdh])
        P.op("dve", lambda e: e.tensor_tensor(out=tmp8[:, :], in0=wd32[:, :], in1=wdh[:, i, 0:8], op=ALU.subtract),
             r=[wd32, wdh], w=[tmp8])
        P.op("act", lambda e: e.copy(out=wdh[:, i, 8:16], in_=tmp8[:, :]), r=[tmp8], w=[wdh])
        pp = psf.next()
        P.op("pe", lambda e: e.matmul(pp[:, 0:8], lhsT=ustr[:, :], rhs=ohb[:, :], start=True, stop=True),
             r=[ustr, ohb], w=[pp])
        P.op("dve", lambda e: e.tensor_tensor(out=pos[:, :], in0=pp[:, 0:8], in1=cnt[:, :], op=ALU.add),
             r=[pp, cnt], w=[pos])
        pc2 = psf.next()
        P.op("pe", lambda e: e.matmul(pc2[:, 0:8], lhsT=onesb2[:, :], rhs=ohb[:, :], start=True, stop=True),
             r=[onesb2, ohb], w=[pc2])
        P.op("dve", lambda e: e.tensor_tensor(out=cnt[:, :], in0=cnt[:, :], in1=pc2[:, 0:8], op=ALU.add),
             r=[cnt, pc2], w=[cnt])
        P.op("dve", lambda e: e.scalar_tensor_tensor(out=tmp8[:, :], in0=pos[:, :], scalar=1.0, in1=oh[:, :],
                                                     op0=ALU.add, op1=ALU.mult), r=[pos, oh], w=[tmp8])
        P.op("dve", lambda e: e.tensor_scalar(out=posm_all[:, i, :], in0=tmp8[:, :], scalar1=-1.0, scalar2=None,
                                              op0=ALU.add), r=[tmp8], w=[posm_all])
        P.op("dve", lambda e: e.tensor_tensor(out=tmp8[:, :], in0=pos[:, :], in1=gbase[:, :], op=ALU.add),
             r=[pos, gbase], w=[tmp8])
        P.op("dve", lambda e: e.tensor_tensor(out=tmp8[:, :], in0=tmp8[:, :], in1=oh[:, :], op=ALU.mult),
             r=[tmp8, oh], w=[tmp8])
        P.op("dve", lambda e: e.memset(rid32[:, :], 0.0), w=[rid32])
        P.op("act", lambda e: e.activation(out=junk8[:, :], in_=tmp8[:, :], func=AF.Identity, accum_out=rid32[:, 0:1]),
             r=[tmp8, rid32], w=[junk8, rid32])
        P.op("dve", lambda e: e.tensor_copy(out=ridx[:, i:i + 1], in_=rid32[:, 0:1]), r=[rid32], w=[ridx])

    for i in range(NT):
        route_tile(i)

    Sg = P.sb("Sg", [128, NT, CG], BF16)
    xstg = P.sb("xstg", [128, 16, CG], BF16)

    def gather_group(g):
        for i in range(NT):
            P.op("dve", lambda e, i=i: e.tensor_scalar(out=Sg[:, i, :], in0=iota_row[:, :],
                                                       scalar1=posm_all[:, i, g:g + 1], scalar2=None,
                                                       op0=ALU.is_equal), r=[iota_row, posm_all], w=[Sg])
        for kc in range(16):
            pt = psf.next()
            for i in range(NT):
                P.op("pe", lambda e, pt=pt, kc=kc, i=i: e.matmul(pt[:, :CG], lhsT=hf_all[:, i, kc * 128:(kc + 1) * 128],
                                                                rhs=Sg[:, i, :], start=(i == 0), stop=(i == NT - 1)),
                     r=[hf_all, Sg], w=[pt])
            P.op("act", lambda e, pt=pt, kc=kc: e.copy(out=xstg[:, kc, :], in_=pt[:, :CG]), r=[pt], w=[xstg])
        P.store("sp", XgT_d, XgT_d[g, :, :, :], xstg, xstg[:, :, :])
        for ct in range(3):
            pw = psf.next()
            n = 0
            for i in range(NT):
                for hl in range(2):
                    P.op("pe", lambda e, pw=pw, i=i, hl=hl, ct=ct, n=n: e.matmul(
                        pw[:, 0:8], lhsT=Sg[:, i, ct * 128:(ct + 1) * 128], rhs=wdh[:, i, hl * 8:(hl + 1) * 8],
                        start=(n == 0), stop=(n == 2 * NT - 1)), r=[Sg, wdh], w=[pw])
                    n += 1
            P.op("act", lambda e, pw=pw, ct=ct: e.copy(out=wslot[:, g, ct, :], in_=pw[:, 0:8]), r=[pw], w=[wslot])

    for g in range(8):
        gather_group(g)
    P.pop()

    P.push()
    xg = P.sb("xg", [128, 16, CG], BF16)
    yg = P.sb("yg", [128, 3, D], F32)
    wgb_r = Ring([P.sb("wgb%d" % i, [128, 16, 512], BF16) for i in range(2)])
    wub_r = Ring([P.sb("wub%d" % i, [128, 16, 512], BF16) for i in range(2)])
    wdb_r = Ring([P.sb("wdb%d" % i, [128, 4, D], BF16) for i in range(2)])
    hact_r = Ring([P.sb("hact%d" % i, [128, 512], BF16) for i in range(2)])
    hactT_r = Ring([P.sb("hactT%d" % i, [128, 4, 128], BF16) for i in range(2)])
    sg32 = P.sb("sg32", [128, 512], F32)
    FB = [(0, 512), (512, 512), (1024, 384)]

    def expert(g, j, first):
        e_id = g * 8 + j
        for (f0, nf) in FB:
            wgb = wgb_r.next()
            wub = wub_r.next()
            wdb = wdb_r.next()
            nch = nf // 128
            P.load("pool", wgb, wgb[:, :, :nf], wg_in.t.ap()[e_id].rearrange("(kc p) n -> p kc n", p=128)[:, :, f0:f0 + nf])
            P.load("pool", wub, wub[:, :, :nf], wu_in.t.ap()[e_id].rearrange("(kc p) n -> p kc n", p=128)[:, :, f0:f0 + nf])
            P.load("pool", wdb, wdb[:, :nch, :], wd_in.t.ap()[e_id, f0:f0 + nf, :].rearrange("(c p) n -> p c n", p=128))
            for ct in range(3):
                pga = psf.next()
                for kc in range(16):
                    P.op("pe", lambda e, pga=pga, kc=kc, ct=ct, wgb=wgb, nf=nf: e.matmul(
                        pga[:, :nf], lhsT=xg[:, kc, ct * 128:(ct + 1) * 128], rhs=wgb[:, kc, :nf],
                        start=(kc == 0), stop=(kc == 15)), r=[xg, wgb], w=[pga])
                pup = psf.next()
                for kc in range(16):
                    P.op("pe", lambda e, pup=pup, kc=kc, ct=ct, wub=wub, nf=nf: e.matmul(
                        pup[:, :nf], lhsT=xg[:, kc, ct * 128:(ct + 1) * 128], rhs=wub[:, kc, :nf],
                        start=(kc == 0), stop=(kc == 15)), r=[xg, wub], w=[pup])
                hact = hact_r.next()
                P.op("act", lambda e, pga=pga, nf=nf: e.activation(out=sg32[:, :nf], in_=pga[:, :nf], func=AF.Silu),
                     r=[pga], w=[sg32])
                P.op("dve", lambda e, pup=pup, hact=hact, nf=nf, ct=ct: e.scalar_tensor_tensor(
                    out=hact[:, :nf], in0=sg32[:, :nf], scalar=wslot[:, g, ct, j:j + 1], in1=pup[:, :nf],
                    op0=ALU.mult, op1=ALU.mult), r=[sg32, wslot, pup], w=[hact])
                hactT = hactT_r.next()
                pb = psb.next()
                for c4 in range(nch):
                    P.op("pe", lambda e, pb=pb, c4=c4, hact=hact: e.transpose(out=pb[:, c4 * 128:(c4 + 1) * 128],
                                                                             in_=hact[:, c4 * 128:(c4 + 1) * 128],
                                                                             identity=ident[:, :]),
                         r=[hact, ident], w=[pb])
                P.op("act", lambda e, pb=pb, hactT=hactT, nch=nch: e.copy(
                    out=hactT[:, :nch, :], in_=pb[:, :nch * 128].rearrange("p (k t) -> p k t", t=128)),
                    r=[pb], w=[hactT])
                for db in range(4):
                    pdn = psf.next()
                    for c4 in range(nch):
                        P.op("pe", lambda e, pdn=pdn, c4=c4, db=db, hactT=hactT, wdb=wdb, nch=nch: e.matmul(
                            pdn[:, :], lhsT=hactT[:, c4, :], rhs=wdb[:, c4, db * 512:(db + 1) * 512],
                            start=(c4 == 0), stop=(c4 == nch - 1)), r=[hactT, wdb], w=[pdn])
                    if first and f0 == 0:
                        P.op("act", lambda e, pdn=pdn, ct=ct, db=db: e.copy(out=yg[:, ct, db * 512:(db + 1) * 512],
                                                                           in_=pdn[:, :]), r=[pdn], w=[yg])
                    else:
                        P.op("dve", lambda e, pdn=pdn, ct=ct, db=db: e.tensor_tensor(
                            out=yg[:, ct, db * 512:(db + 1) * 512], in0=yg[:, ct, db * 512:(db + 1) * 512],
                            in1=pdn[:, :], op=ALU.add), r=[yg, pdn], w=[yg])

    for g in range(8):
        P.load("sp", xg, xg[:, :, :], XgT_d[g, :, :, :], src=XgT_d)
        for j in range(8):
            expert(g, j, j == 0)
        P.store("sp", Y_d, Y_d.t.ap()[g * CG:(g + 1) * CG, :].rearrange("(c p) n -> p c n", p=128), yg, yg[:, :, :])
    P.pop()

    P.push()
    yr_r = Ring([P.sb("yr%d" % i, [128, D], F32) for i in range(2)])
    h2o_r = Ring([P.sb("h2o%d" % i, [128, D], F32) for i in range(2)])
    for i in range(NT):
        yr = yr_r.next()
        h2o = h2o_r.next()
        P.load("sp", h2o, h2o[:, :], h2_d[i * 128:(i + 1) * 128, :], src=h2_d)
        P.dma_fn("pool", lambda e, yr=yr, i=i: e.indirect_dma_start(
            out=yr[:, :], out_offset=None, in_=Y_d[:, :],
            in_offset=bass.IndirectOffsetOnAxis(ap=ridx[:, i:i + 1], axis=0)),
            r=[Y_d, ridx], w=[yr], key=yr.b.name)
        P.op("dve", lambda e, yr=yr, h2o=h2o: e.tensor_tensor(out=h2o[:, :], in0=h2o[:, :], in1=yr[:, :], op=ALU.add),
             r=[h2o, yr], w=[h2o])
        P.store("sp", out_t, out_t[i * 128:(i + 1) * 128, :], h2o, h2o[:, :])
    P.pop()
    P.pop()

    P.emit()
    return nc, es


def rope_tables():
    half = 64
    inv = (10000.0 ** (-np.arange(half, dtype=np.float32) / half)).astype(np.float32)
    pos = np.arange(T, dtype=np.float32)
    ang = pos[:, None] * inv[None, :]
    cos = np.cos(ang).astype(np.float32).reshape(NT, 128, 64).transpose(1, 0, 2)
    sin = np.sin(ang).astype(np.float32).reshape(NT, 128, 64).transpose(1, 0, 2)
    return np.ascontiguousarray(cos), np.ascontiguousarray(sin)


_NSA_CONST = None


def nsa_constants():
    global _NSA_CONST
    if _NSA_CONST is not None:
        return _NSA_CONST
    bf = ml_dtypes.bfloat16
    c = np.arange(128)[:, None]
    q = np.arange(T)[None, :]
    cmask = np.where((16 * c + 31 <= q) & (c < 127), 0.0, NEG).astype(np.float32)
    wmask = np.zeros((128, 8, 512), np.float32)
    k = np.arange(128)[:, None]
    qq = np.arange(512)[None, :]
    for idx in range(8):
        rel = idx - 4
        diff = qq - (rel * 128 + k)
        wmask[:, idx, :] = np.where((diff >= 0) & (diff < 512), 0.0, NEG)
    E = np.zeros((32, 16, 128), np.float32)
    for kt in range(16):
        for kk in range(128):
            E[2 * kt + kk // 64, kt, kk] = 1.0
    sel48 = np.zeros((48, 48, 128), np.float32)
    for j in range(48):
        sel48[j, j, :] = 1.0
    ci = np.arange(128)[:, None]
    sj = np.arange(32)[None, :]
    ovl = ((ci * 16 < (sj + 1) * 64) & (ci * 16 + 32 > sj * 64) & (ci < 127)).astype(np.float32)
    keep = np.zeros((128, 16, 32), np.float32)
    addc = np.zeros((128, 16, 32), np.float32)
    for qt in range(16):
        tq = qt * 128 + np.arange(128)[:, None]
        blk_t = tq // 64
        sb_ = np.arange(32)[None, :]
        valid = sb_ <= blk_t
        forced = (sb_ == 0) | (sb_ == blk_t) | (sb_ == blk_t - 1)
        keep[:, qt, :] = (valid & ~forced).astype(np.float32)
        addc[:, qt, :] = np.where(~valid, -1e30, np.where(forced, 1e4 + sb_, 0.0))
    _NSA_CONST = {"cmask": cmask.astype(bf), "wmask": wmask.astype(bf), "Esel": E.astype(bf), "sel48": sel48.astype(bf),
                  "ovl": ovl, "keepc": keep, "addc": addc}
    return _NSA_CONST


def make_in_map(inputs, b):
    f = np.float32
    cos, sin = rope_tables()
    m = {}
    m["x"] = np.ascontiguousarray(inputs["x"][b], dtype=f)
    m["w_in"] = np.ascontiguousarray(inputs["w_in"][0], dtype=f)
    m["norm1_w"] = np.ascontiguousarray(inputs["norm1_w"][0].reshape(1, D), dtype=f)
    m["conv_w"] = np.ascontiguousarray(inputs["ssd_conv_w"][0].reshape(4, 48, 128).transpose(2, 1, 0), dtype=f)
    m["conv_b"] = np.ascontiguousarray(inputs["ssd_conv_b"][0].reshape(48, 128).T, dtype=f)
    m["qnw"] = np.ascontiguousarray(inputs["nsa_q_norm_w"][0].reshape(1, 128), dtype=f)
    m["knw"] = np.ascontiguousarray(inputs["nsa_k_norm_w"][0].reshape(3, 128), dtype=f)
    m["rope_cos"] = cos
    m["rope_sin"] = sin
    m["ident"] = np.eye(128, dtype=np.float32).astype(ml_dtypes.bfloat16)
    m["identf"] = np.eye(128, dtype=np.float32)
    m["triu"] = np.triu(np.ones((128, 128), dtype=np.float32))
    m["dt_bias"] = np.ascontiguousarray(inputs["ssd_dt_bias"][0].reshape(1, 64), dtype=f)
    m["a_log"] = np.ascontiguousarray(inputs["ssd_a_log"][0].reshape(1, 64), dtype=f)
    m["d_skip"] = np.ascontiguousarray(inputs["ssd_d"][0].reshape(1, 64), dtype=f)
    m["ssd_norm_w"] = np.ascontiguousarray(inputs["ssd_norm_w"][0].reshape(1, 4096), dtype=f)
    m.update(nsa_constants())
    m["cmp_pe_kT"] = np.ascontiguousarray(inputs["cmp_pe_k"][0].T, dtype=f)
    m["cmp_pe_vT"] = np.ascontiguousarray(inputs["cmp_pe_v"][0].T, dtype=f)
    m["cmp_w1_k"] = np.ascontiguousarray(inputs["cmp_w1_k"][0], dtype=f)
    m["cmp_w1_v"] = np.ascontiguousarray(inputs["cmp_w1_v"][0], dtype=f)
    m["cmp_w2_k"] = np.ascontiguousarray(inputs["cmp_w2_k"][0], dtype=f)
    m["cmp_w2_v"] = np.ascontiguousarray(inputs["cmp_w2_v"][0], dtype=f)
    m["w_up_ssd"] = np.ascontiguousarray(inputs["w_up_ssd"][0], dtype=f)
    m["w_up_nsa"] = np.ascontiguousarray(inputs["w_up_nsa"][0], dtype=f)
    m["w_out"] = np.ascontiguousarray(inputs["w_out"][0], dtype=f)
    m["mem"] = np.ascontiguousarray(inputs["mem"][b], dtype=f)
    m["norm2_w"] = np.ascontiguousarray(inputs["norm2_w"][0].reshape(1, D), dtype=f)
    m["mem_norm_w"] = np.ascontiguousarray(inputs["mem_norm_w"][0].reshape(1, D), dtype=f)
    m["xq_w"] = np.ascontiguousarray(inputs["xq_w"][0], dtype=f)
    m["xkv_w"] = np.ascontiguousarray(inputs["xkv_w"][0], dtype=f)
    m["x_q_norm_w"] = np.ascontiguousarray(inputs["x_q_norm_w"][0].reshape(1, 128), dtype=f)
    m["x_k_norm_w"] = np.ascontiguousarray(inputs["x_k_norm_w"][0].reshape(1, 128), dtype=f)
    m["xo_w"] = np.ascontiguousarray(inputs["xo_w"][0], dtype=f)
    if "moe_w_gate" in inputs:
        m["norm3_w"] = np.ascontiguousarray(inputs["norm3_w"][0].reshape(1, D), dtype=f)
        m["router_g_w"] = np.ascontiguousarray(inputs["router_g_w"][0], dtype=f)
        m["router_e_w"] = np.ascontiguousarray(inputs["router_e_w"][0], dtype=f)
        m["router_b"] = np.ascontiguousarray(
            np.concatenate([inputs["router_g_b"][0].reshape(-1), inputs["router_e_b"][0].reshape(-1)]).reshape(1, 72), dtype=f)
        m["moe_w_gate"] = np.ascontiguousarray(inputs["moe_w_gate"][0], dtype=f)
        m["moe_w_up"] = np.ascontiguousarray(inputs["moe_w_up"][0], dtype=f)
        m["moe_w_down"] = np.ascontiguousarray(inputs["moe_w_down"][0], dtype=f)
        m["iota_row"] = np.ascontiguousarray(np.broadcast_to(np.arange(CG, dtype=f)[None, :], (128, CG)))
        m["gbase"] = np.ascontiguousarray(np.broadcast_to((np.arange(8, dtype=f) * CG)[None, :], (128, 8)))
        m["ustrict"] = np.triu(np.ones((128, 128), dtype=f), k=1).astype(ml_dtypes.bfloat16)
    return m


def kernel(**inputs):
    nc, es = build()
    in_maps = [make_in_map(inputs, b) for b in range(8)]
    res = run_bass_kernel_spmd(nc, in_maps, core_ids=list(range(8)))
    out = np.stack([np.asarray(r["out"], dtype=np.float32) for r in res.results], axis=0)
    return out
```

```python
import numpy as np
import ml_dtypes
from contextlib import ExitStack
import concourse.bass as bass
import concourse.mybir as mybir
from concourse.bass_utils import run_bass_kernel_spmd

F32 = mybir.dt.float32
BF16 = mybir.dt.bfloat16
I32 = mybir.dt.int32
ALU = mybir.AluOpType
AF = mybir.ActivationFunctionType
AX = mybir.AxisListType

T = 2048
D = 2048
NT = 16
EPS = 1e-6
NEG = -30000.0
IN_SPLITS = (4096, 6144, 64, 2048, 512, 512, 512, 512, 512, 512, 48, 2048, 2048)
IN_COLS = sum(IN_SPLITS)
SEG_NAMES = ("z", "xbc", "dt", "q", "kc", "vc", "ks", "vs", "kw", "vw", "nsag", "gssd", "gnsa")
SEG_OFF = dict(zip(SEG_NAMES, np.cumsum((0,) + IN_SPLITS[:-1]).tolist()))
SEG_LEN = dict(zip(SEG_NAMES, IN_SPLITS))
CG = 384
NGRP = 8
EH = 1408

ENGS = ("pe", "dve", "act", "pool", "sp")


class Buf:
    __slots__ = ("name", "w", "rs")

    def __init__(self, name):
        self.name = name
        self.w = None
        self.rs = []


class Tl:
    def __init__(self, t, name):
        self.t = t
        self.b = Buf(name)

    def __getitem__(self, k):
        return self.t[k]


class Op:
    __slots__ = ("eng", "fn", "deps", "marked", "idx", "is_dma", "dsem", "dval")

    def __init__(self, eng, fn):
        self.eng = eng
        self.fn = fn
        self.deps = []
        self.marked = False
        self.idx = -1
        self.is_dma = False
        self.dsem = None
        self.dval = 0


class Prog:
    def __init__(self, nc, es):
        self.nc = nc
        self.es = es
        self.ops = {e: [] for e in ENGS}
        self.sems = {e: es.enter_context(nc.semaphore("s_" + e)) for e in ENGS}
        self.waited = {e: {f: -1 for f in ENGS} for e in ENGS}
        self.waited_d = {e: {} for e in ENGS}
        self.keymap = {}
        self.sem_pool = []
        self.sem_count = []
        self.sem_free = []
        self.last_dma = {}
        self.scopes = [es]

    def sb(self, name, shape, dtype):
        t = self.scopes[-1].enter_context(self.nc.sbuf_tensor("sb_" + name, list(shape), dtype))
        return Tl(t, name)

    def push(self):
        self.scopes.append(ExitStack())

    def pop(self):
        self.barrier()
        self.scopes.pop().close()

    def ps(self, name, shape, dtype):
        t = self.es.enter_context(self.nc.psum_tensor("ps_" + name, list(shape), dtype))
        return Tl(t, name)

    def dram(self, name, shape, dtype, kind="Internal"):
        t = self.nc.dram_tensor(name, list(shape), dtype, kind=kind)
        return Tl(t, name)

    def _dsem(self, key):
        if key not in self.keymap:
            if self.sem_free:
                idx = self.sem_free.pop()
            else:
                idx = len(self.sem_pool)
                self.sem_pool.append(self.es.enter_context(self.nc.semaphore("d_%d" % idx)))
                self.sem_count.append(0)
            self.keymap[key] = idx
        return self.keymap[key]

    def op(self, eng, fn, r=(), w=(), selfsync=False):
        o = Op(eng, fn)
        o.idx = len(self.ops[eng])
        raw = []
        war = []
        for t in r:
            if t.b.w is not None:
                raw.append(t.b.w)
        for t in w:
            if t.b.w is not None:
                raw.append(t.b.w)
            war.extend(t.b.rs)
        for d, is_raw in [(d, True) for d in raw] + [(d, False) for d in war]:
            if d is o:
                continue
            if d.is_dma:
                cur = self.waited_d[eng].get(d.dsem, 0)
                if d.dval > cur:
                    self.waited_d[eng][d.dsem] = d.dval
                    o.deps.append(d)
            else:
                if d.eng == eng:
                    if eng != "pe" and is_raw and d.fn is not None and d.idx > self.waited[eng][eng]:
                        self.waited[eng][eng] = d.idx
                        d.marked = True
                        o.deps.append(d)
                    continue
                if d.idx > self.waited[eng][d.eng]:
                    self.waited[eng][d.eng] = d.idx
                    d.marked = True
                    o.deps.append(d)
        for t in r:
            t.b.rs.append(o)
        for t in w:
            t.b.w = o
            t.b.rs = []
        self.ops[eng].append(o)
        return o

    def dma(self, q, out, in_, r=(), w=(), key=None, **kw):
        if key is None:
            key = r[0].b.name if r else w[0].b.name

        def fn(e):
            return e.dma_start(out=out, in_=in_, **kw)

        o = Op(q, fn)
        o.is_dma = True
        o.idx = len(self.ops[q])
        deps = []
        for t in r:
            if t.b.w is not None:
                deps.append(t.b.w)
        for t in w:
            if t.b.w is not None:
                deps.append(t.b.w)
            deps.extend(t.b.rs)
        for d in deps:
            if d.is_dma:
                cur = self.waited_d[q].get(d.dsem, 0)
                if d.dval > cur:
                    self.waited_d[q][d.dsem] = d.dval
                    o.deps.append(d)
            else:
                if d.eng == q:
                    d.marked = True
                    o.deps.append(d)
                elif d.idx > self.waited[q][d.eng]:
                    self.waited[q][d.eng] = d.idx
                    d.marked = True
                    o.deps.append(d)
        sidx = self._dsem(key)
        o.dsem = self.sem_pool[sidx]
        self.sem_count[sidx] += 16
        o.dval = self.sem_count[sidx]
        self.last_dma[key] = o
        for t in r:
            t.b.rs.append(o)
        for t in w:
            t.b.w = o
            t.b.rs = []
        self.ops[q].append(o)
        return o

    def dma_fn(self, q, fn, r=(), w=(), key=None):
        o = self.dma(q, None, None, r=r, w=w, key=key)
        o.fn = fn
        return o

    def store(self, q, dram_tl, out, src_tl, in_, **kw):
        return self.dma(q, out, in_, r=[src_tl], w=[dram_tl], key=src_tl.b.name, **kw)

    def load(self, q, dst_tl, out, in_, src=None, **kw):
        return self.dma(q, out, in_, r=([src] if src is not None else []), w=[dst_tl], key=dst_tl.b.name, **kw)

    def barrier(self):
        lasts = {}
        for f in ENGS:
            for o in reversed(self.ops[f]):
                if o.fn is not None and not o.is_dma:
                    lasts[f] = o
                    break
        for e in ENGS:
            o = Op(e, None)
            o.idx = len(self.ops[e])
            for f, lo in lasts.items():
                if f == e:
                    continue
                if lo.idx > self.waited[e][f]:
                    self.waited[e][f] = lo.idx
                    lo.marked = True
                    o.deps.append(lo)
            for key, d in self.last_dma.items():
                cur = self.waited_d[e].get(d.dsem, 0)
                if d.dval > cur:
                    self.waited_d[e][d.dsem] = d.dval
                    o.deps.append(d)
            self.ops[e].append(o)
        self.last_dma = {}
        self.keymap = {}
        self.sem_free = list(range(len(self.sem_pool)))

    def emit(self):
        nc = self.nc
        val = {}
        for e in ENGS:
            c = 0
            for o in self.ops[e]:
                if o.marked and not o.is_dma:
                    c += 1
                    val[id(o)] = c
        final_waits = [(self.sem_pool[i], self.sem_count[i]) for i in range(len(self.sem_pool))]

        def run(ename, eng):
            for o in self.ops[ename]:
                for d in o.deps:
                    if d.is_dma:
                        eng.wait_ge(d.dsem, d.dval)
                    else:
                        eng.wait_ge(self.sems[d.eng], val[id(d)])
                if o.fn is None:
                    continue
                ins = o.fn(eng)
                if o.is_dma:
                    ins.then_inc(o.dsem, 16)
                elif o.marked:
                    ins.then_inc(self.sems[ename], 1)
            if ename == "sp":
                for s, v in final_waits:
                    if v > 0:
                        eng.wait_ge(s, v)

        with nc.Block() as block:
            @block.tensor
            def _(pe):
                run("pe", pe)

            @block.vector
            def _(dve):
                run("dve", dve)

            @block.scalar
            def _(act):
                run("act", act)

            @block.gpsimd
            def _(pool):
                run("pool", pool)

            @block.sync
            def _(sp):
                run("sp", sp)


class Ring:
    def __init__(self, tiles):
        self.tiles = tiles
        self.i = 0

    def next(self):
        t = self.tiles[self.i % len(self.tiles)]
        self.i += 1
        return t


def seg_blocks():
    out = []
    for s in SEG_NAMES:
        off, ln = SEG_OFF[s], SEG_LEN[s]
        c = 0
        while c < ln:
            n = min(512, ln - c)
            out.append((s, off + c, n, c))
            c += n
    return out


def build(dbg=(), stop=None, ext=()):
    nc = bass.Bass("TRN2", target_bir_lowering=False)
    es = ExitStack()
    P = Prog(nc, es)
    dbg = set(dbg)

    def dten(name, shape, dtype):
        kind = "ExternalOutput" if name in dbg else ("ExternalInput" if name in ext else "Internal")
        return P.dram(name, shape, dtype, kind=kind)

    def inp(name, shape, dtype=F32):
        return P.dram(name, shape, dtype, kind="ExternalInput")

    x_in = inp("x", [T, D])
    w_in = inp("w_in", [D, IN_COLS])
    norm1_bc = inp("norm1_w", [1, D])
    convw = inp("conv_w", [128, 48, 4])
    convb = inp("conv_b", [128, 48])
    qnw = inp("qnw", [1, 128])
    knw = inp("knw", [3, 128])
    ropec = inp("rope_cos", [128, NT, 64])
    ropes = inp("rope_sin", [128, NT, 64])
    ident_in = inp("ident", [128, 128], BF16)
    out_t = P.dram("out", [T, D], F32, kind="ExternalOutput")

    zs = dten("zs", [T, 4096], BF16)
    xs = dten("xs", [T, 4096], BF16)
    BTs = dten("BTs", [1024, T], BF16)
    CTs = dten("CTs", [1024, T], BF16)
    Btok = dten("Btok", [T, 1024], BF16)
    qT = dten("qT", [2048, T], BF16)
    kTc = dten("kTc", [512, T], BF16)
    kTs = dten("kTs", [512, T], BF16)
    kTw = dten("kTw", [512, T], BF16)
    vcT = dten("vcT", [512, T], BF16)
    vstok = dten("vstok", [T, 512], BF16)
    vwtok = dten("vwtok", [T, 512], BF16)
    gsT = dten("gsT", [2048, T], BF16)
    gnT = dten("gnT", [2048, T], BF16)
    dtraw_d = dten("dtraw", [T, 64], F32)

    ident = P.sb("ident", [128, 128], BF16)
    P.dma("sp", ident[:, :], ident_in[:, :], w=[ident])
    gT = P.sb("gT", [48, T], BF16)
    junk = P.sb("junk", [128, D], BF16)
    ss_r = Ring([P.sb("ss%d" % i, [128, 1], F32) for i in range(2)])
    hn_r = Ring([P.sb("hn%d" % i, [128, D], BF16) for i in range(2)])
    stg_r = Ring([P.sb("stg%d" % i, [128, 512], BF16) for i in range(3)])
    f32a = P.sb("f32a", [128, 512], F32)
    f32b = P.sb("f32b", [128, 512], F32)
    P.push()
    wb1 = P.sb("wb1", [128, D], F32)
    P.dma("sp", wb1[:, :], norm1_bc[0:1, :].partition_broadcast(128), w=[wb1])
    hnT = P.sb("hnT", [128, 16, T], BF16)
    cw = P.sb("cw", [128, 48, 4], F32)
    P.dma("sp", cw[:, :, :], convw[:, :, :], w=[cw])
    cbias = P.sb("cb", [128, 48], F32)
    P.dma("sp", cbias[:, :], convb[:, :], w=[cbias])
    rc = P.sb("rc", [128, NT, 64], F32)
    rs_ = P.sb("rs", [128, NT, 64], F32)
    P.dma("sp", rc[:, :, :], ropec[:, :, :], w=[rc])
    P.dma("sp", rs_[:, :, :], ropes[:, :, :], w=[rs_])
    nwq = P.sb("nwq", [128, 4, 128], F32)
    nwk = [P.sb("nwk%d" % i, [128, 4, 128], F32) for i in range(3)]
    for hh in range(4):
        P.dma("sp", nwq[:, hh, :], qnw[0:1, :].partition_broadcast(128), w=[nwq])
        for i in range(3):
            P.dma("sp", nwk[i][:, hh, :], knw[i:i + 1, :].partition_broadcast(128), w=[nwk[i]])

    psf = Ring([P.ps("psf%d" % i, [128, 512], F32) for i in range(6)])
    psb = Ring([P.ps("psb%d" % i, [128, 1024], BF16) for i in range(2)])

    xt_r = Ring([P.sb("xt%d" % i, [128, D], F32) for i in range(2)])

    def rmsnorm_tile(src_tl, wb_tl, dst_tl, width=D, woff=0):
        ss = ss_r.next()
        P.op("dve", lambda e, ss=ss: e.memset(ss[:, 0:1], 0.0), w=[ss])
        P.op("act", lambda e, ss=ss: e.activation(out=junk[:, :width], in_=src_tl[:, :width], func=AF.Square,
                                                   accum_out=ss[:, 0:1]), r=[src_tl], w=[junk, ss])
        P.op("dve", lambda e, ss=ss: e.tensor_scalar(out=ss[:, 0:1], in0=ss[:, 0:1], scalar1=1.0 / width,
                                                      scalar2=EPS, op0=ALU.mult, op1=ALU.add), r=[ss], w=[ss])
        P.op("act", lambda e, ss=ss: e.activation(out=ss[:, 0:1], in_=ss[:, 0:1], func=AF.Sqrt), r=[ss], w=[ss])
        P.op("dve", lambda e, ss=ss: e.reciprocal(out=ss[:, 0:1], in_=ss[:, 0:1]), r=[ss], w=[ss])
        P.op("dve", lambda e, ss=ss: e.scalar_tensor_tensor(out=dst_tl[:, :width], in0=src_tl[:, :width],
                                                             scalar=ss[:, 0:1], in1=wb_tl[:, woff:woff + width],
                                                             op0=ALU.mult, op1=ALU.mult),
             r=[src_tl, ss, wb_tl], w=[dst_tl], selfsync=True)

    def transpose_into(src_tl, nblk, dst_fn, dst_tl):
        for half in range((nblk + 7) // 8):
            pb = psb.next()
            nb = min(8, nblk - half * 8)
            for j in range(nb):
                kc = half * 8 + j
                P.op("pe", lambda e, pb=pb, j=j, kc=kc: e.transpose(out=pb[:, j * 128:(j + 1) * 128],
                                                                     in_=src_tl[:, kc * 128:(kc + 1) * 128],
                                                                     identity=ident[:, :]),
                     r=[src_tl, ident], w=[pb])
            P.op("act", lambda e, pb=pb, half=half, nb=nb: e.copy(
                out=dst_fn(half, nb), in_=pb[:, :nb * 128].rearrange("p (k t) -> p k t", t=128)),
                r=[pb], w=[dst_tl])

    for i in range(NT):
        xt = xt_r.next()
        hn = hn_r.next()
        P.dma("sp", xt[:, :], x_in[i * 128:(i + 1) * 128, :], w=[xt])
        rmsnorm_tile(xt, wb1, hn)
        transpose_into(hn, 16, lambda half, nb, i=i: hnT[:, half * 8:half * 8 + nb, i * 128:(i + 1) * 128], hnT)

    wt_r = Ring([P.sb("wt%d" % i, [128, 16, 512], BF16) for i in range(2)])
    rstd4 = P.sb("rstd4", [128, 4], F32)
    xpad = P.sb("xpad", [128, 3 + T], F32)
    acc = P.sb("acc", [128, T], F32)
    xc = P.sb("xc", [128, T], BF16)
    tstg = P.sb("tstg", [128, 16, 128], BF16)
    P.op("dve", lambda e: e.memset(xpad[:, 0:3], 0.0), w=[xpad])
    w_view = w_in.t.ap().rearrange("(kc p) n -> p kc n", p=128)

    def mm_tok(wt, n, i, pt):
        for kc in range(16):
            P.op("pe", lambda e, kc=kc: e.matmul(pt[:, :n], lhsT=hnT[:, kc, i * 128:(i + 1) * 128],
                                                 rhs=wt[:, kc, :n], start=(kc == 0), stop=(kc == 15)),
                 r=[hnT, wt], w=[pt])

    def mm_feat(wt, j, m, tb, pt):
        for kc in range(16):
            P.op("pe", lambda e, kc=kc: e.matmul(pt[:m, :], lhsT=wt[:, kc, j * 128:j * 128 + m],
                                                 rhs=hnT[:, kc, tb * 512:(tb + 1) * 512],
                                                 start=(kc == 0), stop=(kc == 15)),
                 r=[hnT, wt], w=[pt])

    def qk_epilogue(pt, i, nw_tl, dstT, c0r):
        P.op("dve", lambda e: e.memset(rstd4[:, :], 0.0), w=[rstd4])
        for hh in range(4):
            P.op("act", lambda e, hh=hh: e.activation(out=f32a[:, hh * 128:(hh + 1) * 128],
                                                      in_=pt[:, hh * 128:(hh + 1) * 128], func=AF.Square,
                                                      accum_out=rstd4[:, hh:hh + 1]), r=[pt], w=[f32a, rstd4])
        P.op("dve", lambda e: e.tensor_scalar(out=rstd4[:, :], in0=rstd4[:, :], scalar1=1.0 / 128, scalar2=EPS,
                                              op0=ALU.mult, op1=ALU.add), r=[rstd4], w=[rstd4])
        P.op("act", lambda e: e.activation(out=rstd4[:, :], in_=rstd4[:, :], func=AF.Sqrt), r=[rstd4], w=[rstd4])
        P.op("dve", lambda e: e.reciprocal(out=rstd4[:, :], in_=rstd4[:, :]), r=[rstd4], w=[rstd4])
        P.op("dve", lambda e: e.tensor_tensor(out=f32a[:, :].rearrange("p (h d) -> p h d", d=128),
                                              in0=pt[:, :].rearrange("p (h d) -> p h d", d=128),
                                              in1=rstd4[:, :].unsqueeze(2).to_broadcast([128, 4, 128]),
                                              op=ALU.mult), r=[pt, rstd4], w=[f32a], selfsync=True)
        P.op("dve", lambda e: e.tensor_tensor(out=f32a[:, :], in0=f32a[:, :],
                                              in1=nw_tl[:, :, :].rearrange("p h d -> p (h d)"),
                                              op=ALU.mult), r=[f32a, nw_tl], w=[f32a])
        v = f32a[:, :].rearrange("p (h t d) -> p h t d", h=4, t=2)
        o = f32b[:, :].rearrange("p (h t d) -> p h t d", h=4, t=2)
        cosb = rc[:, i, :].unsqueeze(1).to_broadcast([128, 4, 64])
        sinb = rs_[:, i, :].unsqueeze(1).to_broadcast([128, 4, 64])
        hnq = hn_r.next()
        hv = hnq[:, 0:512].rearrange("p (h t d) -> p h t d", h=4, t=2)
        P.op("dve", lambda e: e.tensor_tensor(out=o[:, :, 0, :], in0=v[:, :, 0, :], in1=cosb, op=ALU.mult),
             r=[f32a, rc], w=[f32b])
        P.op("dve", lambda e: e.tensor_tensor(out=o[:, :, 1, :], in0=v[:, :, 1, :], in1=cosb, op=ALU.mult),
             r=[f32a, rc], w=[f32b])
        P.op("dve", lambda e: e.tensor_tensor(out=v[:, :, 0, :], in0=v[:, :, 0, :], in1=sinb, op=ALU.mult),
             r=[f32a, rs_, f32b], w=[f32a])
        P.op("dve", lambda e: e.tensor_tensor(out=v[:, :, 1, :], in0=v[:, :, 1, :], in1=sinb, op=ALU.mult),
             r=[f32a, rs_], w=[f32a])
        P.op("dve", lambda e: e.tensor_tensor(out=hv[:, :, 0, :], in0=o[:, :, 0, :], in1=v[:, :, 1, :],
                                              op=ALU.subtract), r=[f32a, f32b], w=[hnq])
        P.op("dve", lambda e: e.tensor_tensor(out=hv[:, :, 1, :], in0=o[:, :, 1, :], in1=v[:, :, 0, :],
                                              op=ALU.add), r=[f32a, f32b], w=[hnq])
        st = stg_r.next()
        transpose_into(hnq, 4, lambda half, nb: st[:, :].rearrange("p (k t) -> p k t", t=128), st)
        P.dma("sp", dstT.t.ap()[c0r:c0r + 512, i * 128:(i + 1) * 128].rearrange("(h d) t -> d h t", d=128),
              st[:, :].rearrange("p (k t) -> p k t", t=128), r=[st], w=[dstT])

    blocks = seg_blocks()
    for (seg, c0, n, c0r) in blocks:
        wt = wt_r.next()
        P.dma("pool", wt[:, :, :n], w_view[:, :, c0:c0 + n], w=[wt])
        if seg in ("z", "vs", "vw", "dt", "q", "kc", "ks", "kw"):
            for i in range(NT):
                pt = psf.next()
                mm_tok(wt, n, i, pt)
                if seg == "z":
                    st = stg_r.next()
                    P.op("act", lambda e, pt=pt, st=st: e.activation(out=st[:, :], in_=pt[:, :], func=AF.Silu),
                         r=[pt], w=[st])
                    P.dma("sp", zs[i * 128:(i + 1) * 128, c0r:c0r + 512], st[:, :], r=[st], w=[zs])
                elif seg in ("vs", "vw"):
                    st = stg_r.next()
                    dst = vstok if seg == "vs" else vwtok
                    P.op("act", lambda e, pt=pt, st=st: e.copy(out=st[:, :], in_=pt[:, :]), r=[pt], w=[st])
                    P.dma("sp", dst[i * 128:(i + 1) * 128, :], st[:, :], r=[st], w=[dst])
                elif seg == "dt":
                    P.op("act", lambda e, pt=pt: e.copy(out=f32a[:, :64], in_=pt[:, :64]), r=[pt], w=[f32a])
                    P.dma("sp", dtraw_d[i * 128:(i + 1) * 128, :], f32a[:, :64], r=[f32a], w=[dtraw_d])
                elif seg == "q":
                    qk_epilogue(pt, i, nwq, qT, c0r)
                else:
                    bi = ("kc", "ks", "kw").index(seg)
                    qk_epilogue(pt, i, nwk[bi], (kTc, kTs, kTw)[bi], 0)
        else:
            nj = (n + 127) // 128
            for j in range(nj):
                m = min(128, n - j * 128)
                if seg == "xbc":
                    ct = (c0r + j * 128) // 128
                    for tb in range(4):
                        pt = psf.next()
                        mm_feat(wt, j, m, tb, pt)
                        P.op("act", lambda e, pt=pt, tb=tb: e.copy(out=xpad[:, 3 + tb * 512:3 + (tb + 1) * 512],
                                                                   in_=pt[:, :]), r=[pt], w=[xpad])
                    P.op("dve", lambda e, ct=ct: e.tensor_scalar(out=acc[:, :], in0=xpad[:, 0:T],
                                                                 scalar1=cw[:, ct, 0:1], scalar2=None,
                                                                 op0=ALU.mult), r=[xpad, cw], w=[acc])
                    for k in range(1, 4):
                        P.op("dve", lambda e, ct=ct, k=k: e.scalar_tensor_tensor(
                            out=acc[:, :], in0=xpad[:, k:k + T], scalar=cw[:, ct, k:k + 1], in1=acc[:, :],
                            op0=ALU.mult, op1=ALU.add), r=[xpad, cw, acc], w=[acc])
                    P.op("act", lambda e, ct=ct: e.activation(out=xc[:, :], in_=acc[:, :], func=AF.Silu,
                                                              bias=cbias[:, ct:ct + 1]), r=[acc, cbias], w=[xc])
                    if ct < 32 or ct < 40:
                        transpose_into(xc, 16, lambda half, nb: tstg[:, half * 8:half * 8 + nb, :], tstg)
                        if ct < 32:
                            dst_ap = xs.t.ap()[:, ct * 128:(ct + 1) * 128].rearrange("(i p) c -> p i c", p=128)
                            P.dma("sp", dst_ap, tstg[:, :, :], r=[tstg], w=[xs])
                        else:
                            cc = ct - 32
                            dst_ap = Btok.t.ap()[:, cc * 128:(cc + 1) * 128].rearrange("(i p) c -> p i c", p=128)
                            P.dma("sp", dst_ap, tstg[:, :, :], r=[tstg], w=[Btok])
                    if 32 <= ct < 40:
                        P.dma("sp", BTs[(ct - 32) * 128:(ct - 31) * 128, :], xc[:, :], r=[xc], w=[BTs])
                    elif ct >= 40:
                        P.dma("sp", CTs[(ct - 40) * 128:(ct - 39) * 128, :], xc[:, :], r=[xc], w=[CTs])
                else:
                    for tb in range(4):
                        pt = psf.next()
                        mm_feat(wt, j, m, tb, pt)
                        if seg == "nsag":
                            P.op("act", lambda e, pt=pt, tb=tb, m=m: e.activation(
                                out=gT[:m, tb * 512:(tb + 1) * 512], in_=pt[:m, :], func=AF.Sigmoid),
                                r=[pt], w=[gT])
                        else:
                            st = stg_r.next()
                            if seg == "vc":
                                P.op("act", lambda e, pt=pt, st=st: e.copy(out=st[:, :], in_=pt[:, :]),
                                     r=[pt], w=[st])
                                dst = vcT
                            else:
                                P.op("act", lambda e, pt=pt, st=st: e.activation(out=st[:, :], in_=pt[:, :],
                                                                                 func=AF.Sigmoid),
                                     r=[pt], w=[st])
                                dst = gsT if seg == "gssd" else gnT
                            r0 = c0r + j * 128
                            P.dma("sp", dst[r0:r0 + 128, tb * 512:(tb + 1) * 512], st[:, :], r=[st], w=[dst])

    if stop == "B":
        P.dma("sp", out_t[0:128, 0:512], f32a[:, :], r=[f32a], w=[out_t])
        P.emit()
        return nc, es

    P.pop()
    P.push()
    identf_in = inp("identf", [128, 128])
    triu_in = inp("triu", [128, 128])
    dtb_in = inp("dt_bias", [1, 64])
    alog_in = inp("a_log", [1, 64])
    dsk_in = inp("d_skip", [1, 64])
    snw_in = inp("ssd_norm_w", [1, 4096])
    acumT_d = dten("acumT_d", [64, T], F32)
    yT = dten("yT", [4096, T], BF16)

    identf = P.sb("identf", [128, 128], F32)
    triu = P.sb("triu", [128, 128], F32)
    onesf = P.sb("onesf", [128, 128], F32)
    P.load("sp", identf, identf[:, :], identf_in[:, :])
    P.load("sp", triu, triu[:, :], triu_in[:, :])
    P.op("dve", lambda e: e.memset(onesf[:, :], 1.0), w=[onesf])
    dtb = P.sb("dtb", [128, 64], F32)
    aneg = P.sb("aneg", [128, 64], F32)
    dskb = P.sb("dskb", [128, 64], F32)
    snw = P.sb("snw", [128, 4096], F32)
    P.load("sp", dtb, dtb[:, :], dtb_in[0:1, :].partition_broadcast(128))
    P.load("sp", aneg, aneg[:, :], alog_in[0:1, :].partition_broadcast(128))
    P.load("sp", dskb, dskb[:, :], dsk_in[0:1, :].partition_broadcast(128))
    P.load("sp", snw, snw[:, :], snw_in[0:1, :].partition_broadcast(128))
    P.op("act", lambda e: e.activation(out=aneg[:, :], in_=aneg[:, :], func=AF.Exp), r=[aneg], w=[aneg])
    P.op("dve", lambda e: e.tensor_scalar(out=aneg[:, :], in0=aneg[:, :], scalar1=-1.0, scalar2=None,
                                          op0=ALU.mult), r=[aneg], w=[aneg])
    dt_sb = P.sb("dt_sb", [128, NT, 64], F32)
    da_sb = P.sb("da_sb", [128, NT, 64], F32)
    acum = P.sb("acum", [128, NT, 64], F32)
    nacum = P.sb("nacum", [128, NT, 64], F32)
    eacum = P.sb("eacum", [128, NT, 64], F32)
    dte = P.sb("dte", [128, NT, 64], F32)
    cdec = P.sb("cdec", [128, 8, 64], F32)
    P.load("sp", dt_sb, dt_sb[:, :, :], dtraw_d.t.ap().rearrange("(i p) h -> p i h", p=128), src=dtraw_d)
    P.op("dve", lambda e: e.tensor_tensor(out=dt_sb[:, :, :], in0=dt_sb[:, :, :],
                                          in1=dtb[:, :].unsqueeze(1).to_broadcast([128, NT, 64]), op=ALU.add),
         r=[dt_sb, dtb], w=[dt_sb])
    P.op("act", lambda e: e.activation(out=dt_sb[:, :, :], in_=dt_sb[:, :, :], func=AF.Exp), r=[dt_sb], w=[dt_sb])
    P.op("dve", lambda e: e.tensor_scalar(out=dt_sb[:, :, :], in0=dt_sb[:, :, :], scalar1=1.0, scalar2=None,
                                          op0=ALU.add), r=[dt_sb], w=[dt_sb])
    P.op("act", lambda e: e.activation(out=dt_sb[:, :, :], in_=dt_sb[:, :, :], func=AF.Ln), r=[dt_sb], w=[dt_sb])
    P.op("dve", lambda e: e.tensor_tensor(out=da_sb[:, :, :], in0=dt_sb[:, :, :],
                                          in1=aneg[:, :].unsqueeze(1).to_broadcast([128, NT, 64]), op=ALU.mult),
         r=[dt_sb, aneg], w=[da_sb])
    for c in range(8):
        i0, i1 = 2 * c, 2 * c + 1
        p0 = psf.next()
        P.op("pe", lambda e, p0=p0, i0=i0: e.matmul(p0[:, :64], lhsT=triu[:, :], rhs=da_sb[:, i0, :],
                                                    start=True, stop=True), r=[triu, da_sb], w=[p0])
        P.op("act", lambda e, p0=p0, i0=i0: e.copy(out=acum[:, i0, :], in_=p0[:, :64]), r=[p0], w=[acum])
        p1 = psf.next()
        P.op("pe", lambda e, p1=p1, i0=i0: e.matmul(p1[:, :64], lhsT=onesf[:, :], rhs=da_sb[:, i0, :],
                                                    start=True, stop=False), r=[onesf, da_sb], w=[p1])
        P.op("pe", lambda e, p1=p1, i1=i1: e.matmul(p1[:, :64], lhsT=triu[:, :], rhs=da_sb[:, i1, :],
                                                    start=False, stop=True), r=[triu, da_sb], w=[p1])
        P.op("act", lambda e, p1=p1, i1=i1: e.copy(out=acum[:, i1, :], in_=p1[:, :64]), r=[p1], w=[acum])
        p2 = psf.next()
        P.op("pe", lambda e, p2=p2, i0=i0: e.matmul(p2[:, :64], lhsT=onesf[:, :], rhs=da_sb[:, i0, :],
                                                    start=True, stop=False), r=[onesf, da_sb], w=[p2])
        P.op("pe", lambda e, p2=p2, i1=i1: e.matmul(p2[:, :64], lhsT=onesf[:, :], rhs=da_sb[:, i1, :],
                                                    start=False, stop=True), r=[onesf, da_sb], w=[p2])
        for ii in (i0, i1):
            P.op("dve", lambda e, p2=p2, ii=ii: e.tensor_tensor(out=dte[:, ii, :], in0=p2[:, :64],
                                                                in1=acum[:, ii, :], op=ALU.subtract),
                 r=[p2, acum], w=[dte])
        P.op("act", lambda e, p2=p2, c=c: e.activation(out=cdec[:, c, :], in_=p2[:, :64], func=AF.Exp),
             r=[p2], w=[cdec])
    P.op("act", lambda e: e.activation(out=dte[:, :, :], in_=dte[:, :, :], func=AF.Exp), r=[dte], w=[dte])
    P.op("dve", lambda e: e.tensor_tensor(out=dte[:, :, :], in0=dte[:, :, :], in1=dt_sb[:, :, :], op=ALU.mult),
         r=[dte, dt_sb], w=[dte])
    P.op("act", lambda e: e.activation(out=eacum[:, :, :], in_=acum[:, :, :], func=AF.Exp), r=[acum], w=[eacum])
    P.op("dve", lambda e: e.tensor_scalar(out=nacum[:, :, :], in0=acum[:, :, :], scalar1=-1.0, scalar2=None,
                                          op0=ALU.mult), r=[acum], w=[nacum])
    acT = P.sb("acT", [64, T], F32)
    for i in range(NT):
        pt = psf.next()
        P.op("pe", lambda e, pt=pt, i=i: e.transpose(out=pt[:64, :128], in_=acum[:, i, :], identity=identf[:, :]),
             r=[acum, identf], w=[pt])
        P.op("act", lambda e, pt=pt, i=i: e.copy(out=acT[:, i * 128:(i + 1) * 128], in_=pt[:64, :128]),
             r=[pt], w=[acT])
    P.store("sp", acumT_d, acumT_d[:, :], acT, acT[:, :])
    P.barrier()

    BTc = P.sb("BTc", [128, 8, 256], BF16)
    CTc = P.sb("CTc", [128, 8, 256], BF16)
    Btc = P.sb("Btc", [128, 2, 1024], BF16)
    xsc = P.sb("xsc", [128, 2, 4096], BF16)
    zsc = P.sb("zsc", [128, 2, 4096], BF16)
    xsd = P.sb("xsd", [128, 2, 4096], BF16)
    Hs = P.sb("Hs", [128, 8, 512], F32)
    Hb = P.sb("Hb", [128, 8, 512], BF16)
    P.op("dve", lambda e: e.memset(Hs[:, :, :], 0.0), w=[Hs])
    P.op("dve", lambda e: e.memset(Hb[:, :, :], 0.0), w=[Hb])
    bc_r = Ring([P.sb("bc%d" % i, [128, 8, 256], F32) for i in range(2)])
    cbTm = P.sb("cbTm", [128, 2, 256], F32)
    dif_r = Ring([P.sb("dif%d" % i, [128, 256], F32) for i in range(2)])
    MT_r = Ring([P.sb("MT%d" % i, [128, 2, 256], BF16) for i in range(3)])
    yA = P.sb("yA", [128, 512], F32)
    yB = P.sb("yB", [128, 512], F32)
    ynb = P.sb("ynb", [128, 512], BF16)
    BT_v = BTs.t.ap().rearrange("(g n) t -> n g t", n=128)
    CT_v = CTs.t.ap().rearrange("(g n) t -> n g t", n=128)
    for c in range(8):
        t0 = c * 256
        P.load("sp", BTc, BTc[:, :, :], BT_v[:, :, t0:t0 + 256], src=BTs)
        P.load("sp", CTc, CTc[:, :, :], CT_v[:, :, t0:t0 + 256], src=CTs)
        P.load("sp", Btc, Btc[:, :, :], Btok.t.ap()[t0:t0 + 256, :].rearrange("(i p) n -> p i n", p=128), src=Btok)
        P.load("sp", xsc, xsc[:, :, :], xs.t.ap()[t0:t0 + 256, :].rearrange("(i p) n -> p i n", p=128), src=xs)
        P.load("sp", zsc, zsc[:, :, :], zs.t.ap()[t0:t0 + 256, :].rearrange("(i p) n -> p i n", p=128), src=zs)
        for lt in range(2):
            ii = 2 * c + lt
            P.op("dve", lambda e, lt=lt, ii=ii: e.tensor_tensor(
                out=xsd[:, lt, :].rearrange("p (h d) -> p h d", d=64),
                in0=xsc[:, lt, :].rearrange("p (h d) -> p h d", d=64),
                in1=dte[:, ii, :].unsqueeze(2).to_broadcast([128, 64, 64]), op=ALU.mult),
                r=[xsc, dte], w=[xsd])
        for g in range(8):
            bc = bc_r.next()
            P.load("pool", bc, bc[:, :, :],
                   acumT_d.t.ap()[g * 8:(g + 1) * 8, t0:t0 + 256].partition_broadcast(128),
                   src=acumT_d)
            for st in range(2):
                pc = psf.next()
                P.op("pe", lambda e, pc=pc, st=st, g=g: e.matmul(pc[:, :256], lhsT=BTc[:, g, st * 128:(st + 1) * 128],
                                                                 rhs=CTc[:, g, :], start=True, stop=True),
                     r=[BTc, CTc], w=[pc])
                if st == 0:
                    P.op("dve", lambda e, pc=pc: e.tensor_tensor(out=cbTm[:, 0, 0:128], in0=pc[:, 0:128],
                                                                 in1=triu[:, :], op=ALU.mult),
                         r=[pc, triu], w=[cbTm])
                    P.op("act", lambda e, pc=pc: e.copy(out=cbTm[:, 0, 128:256], in_=pc[:, 128:256]),
                         r=[pc], w=[cbTm])
                else:
                    P.op("dve", lambda e, pc=pc: e.tensor_tensor(out=cbTm[:, 1, 128:256], in0=pc[:, 128:256],
                                                                 in1=triu[:, :], op=ALU.mult),
                         r=[pc, triu], w=[cbTm])
            pst = psf.next()
            for lt in range(2):
                P.op("pe", lambda e, lt=lt, g=g, pst=pst: e.matmul(pst[:, :], lhsT=Btc[:, lt, g * 128:(g + 1) * 128],
                                                                   rhs=xsd[:, lt, g * 512:(g + 1) * 512],
                                                                   start=(lt == 0), stop=(lt == 1)),
                     r=[Btc, xsd], w=[pst])
            poff = [psf.next(), psf.next()]
            for lt in range(2):
                P.op("pe", lambda e, lt=lt, g=g, po=poff[lt]: e.matmul(po[:, :], lhsT=CTc[:, g, lt * 128:(lt + 1) * 128],
                                                                       rhs=Hb[:, g, :], start=True, stop=True),
                     r=[CTc, Hb], w=[poff[lt]])
            P.op("dve", lambda e, g=g, c=c: e.tensor_tensor(
                out=Hs[:, g, :].rearrange("p (h d) -> p h d", d=64),
                in0=Hs[:, g, :].rearrange("p (h d) -> p h d", d=64),
                in1=cdec[:, c, g * 8:(g + 1) * 8].unsqueeze(2).to_broadcast([128, 8, 64]), op=ALU.mult),
                r=[Hs, cdec], w=[Hs])
            P.op("dve", lambda e, g=g, pst=pst: e.tensor_tensor(out=Hs[:, g, :], in0=Hs[:, g, :], in1=pst[:, :],
                                                                op=ALU.add), r=[Hs, pst], w=[Hs])
            P.op("act", lambda e, g=g: e.copy(out=Hb[:, g, :], in_=Hs[:, g, :]), r=[Hs, poff[0], poff[1]], w=[Hb])
            yoff = [yA, yB]
            for lt in range(2):
                ii = 2 * c + lt
                P.op("dve", lambda e, lt=lt, ii=ii, g=g, po=poff[lt]: e.tensor_tensor(
                    out=yoff[lt][:, :].rearrange("p (h d) -> p h d", d=64),
                    in0=po[:, :].rearrange("p (h d) -> p h d", d=64),
                    in1=eacum[:, ii, g * 8:(g + 1) * 8].unsqueeze(2).to_broadcast([128, 8, 64]), op=ALU.mult),
                    r=[poff[lt], eacum], w=[yoff[lt]])
            pd = [psf.next(), psf.next()]
            for hh in range(8):
                h = g * 8 + hh
                MT = MT_r.next()
                for st in range(2):
                    ii = 2 * c + st
                    l0 = 0 if st == 0 else 128
                    dif = dif_r.next()
                    P.op("dve", lambda e, dif=dif, bc=bc, hh=hh, ii=ii, h=h, l0=l0: e.tensor_scalar(
                        out=dif[:, l0:256], in0=bc[:, hh, l0:256], scalar1=nacum[:, ii, h:h + 1], scalar2=0.0,
                        op0=ALU.add, op1=ALU.min), r=[bc, nacum], w=[dif])
                    P.op("act", lambda e, dif=dif, l0=l0: e.activation(out=dif[:, l0:256], in_=dif[:, l0:256],
                                                                       func=AF.Exp), r=[dif], w=[dif])
                    P.op("dve", lambda e, dif=dif, MT=MT, st=st, ii=ii, h=h, l0=l0: e.scalar_tensor_tensor(
                        out=MT[:, st, l0:256], in0=dif[:, l0:256], scalar=dt_sb[:, ii, h:h + 1],
                        in1=cbTm[:, st, l0:256], op0=ALU.mult, op1=ALU.mult),
                        r=[dif, dt_sb, cbTm], w=[MT])
                for lt in range(2):
                    for st in range(lt + 1):
                        P.op("pe", lambda e, lt=lt, st=st, MT=MT, hh=hh, h=h, pdl=pd[lt]: e.matmul(
                            pdl[:, hh * 64:(hh + 1) * 64], lhsT=MT[:, st, lt * 128:(lt + 1) * 128],
                            rhs=xsc[:, st, h * 64:(h + 1) * 64], start=(st == 0), stop=(st == lt)),
                            r=[MT, xsc], w=[pd[lt]])
            for lt in range(2):
                ii = 2 * c + lt
                yt = yoff[lt]
                P.op("dve", lambda e, yt=yt, lt=lt, pdl=pd[lt]: e.tensor_tensor(out=yt[:, :], in0=yt[:, :], in1=pdl[:, :],
                                                                    op=ALU.add), r=[yt, pd[lt]], w=[yt])
                P.op("dve", lambda e, lt=lt, g=g: e.tensor_tensor(
                    out=f32a[:, :].rearrange("p (h d) -> p h d", d=64),
                    in0=xsc[:, lt, g * 512:(g + 1) * 512].rearrange("p (h d) -> p h d", d=64),
                    in1=dskb[:, g * 8:(g + 1) * 8].unsqueeze(2).to_broadcast([128, 8, 64]), op=ALU.mult),
                    r=[xsc, dskb], w=[f32a])
                P.op("dve", lambda e, yt=yt: e.tensor_tensor(out=yt[:, :], in0=yt[:, :], in1=f32a[:, :], op=ALU.add),
                     r=[yt, f32a], w=[yt])
                P.op("dve", lambda e, yt=yt, lt=lt, g=g: e.tensor_tensor(out=yt[:, :], in0=yt[:, :],
                                                                         in1=zsc[:, lt, g * 512:(g + 1) * 512],
                                                                         op=ALU.mult), r=[yt, zsc], w=[yt])
                rmsnorm_tile(yt, snw, ynb, width=512, woff=g * 512)
                st_ = stg_r.next()
                transpose_into(ynb, 4, lambda half, nb, st_=st_: st_[:, :].rearrange("p (k t) -> p k t", t=128), st_)
                P.store("sp", yT, yT.t.ap()[g * 512:(g + 1) * 512, ii * 128:(ii + 1) * 128].rearrange(
                    "(k d) t -> d k t", d=128), st_, st_[:, :].rearrange("p (k t) -> p k t", t=128))
    P.pop()
    if stop == "C":
        P.dma("sp", out_t[0:128, 0:512], f32a[:, :], r=[f32a], w=[out_t])
        P.emit()
        return nc, es

    SCALE = float(128 ** -0.5)
    ynT = dten("ynT", [2048, T], BF16)
    if "dbg_sel" in dbg:
        dbg_sel = dten("dbg_sel", [4, 16, 128, 32], BF16)
        dbg_imp = dten("dbg_imp", [4, 16, 128, 32], F32)
    if "ynT" not in ext:
        pek_in = inp("cmp_pe_kT", [128, 32])
        pev_in = inp("cmp_pe_vT", [128, 32])
        w1k_in = inp("cmp_w1_k", [32, 128, 256])
        w1v_in = inp("cmp_w1_v", [32, 128, 256])
        w2k_in = inp("cmp_w2_k", [256, 128])
        w2v_in = inp("cmp_w2_v", [256, 128])
        cmask_in = inp("cmask", [128, T], BF16)
        wmask_in = inp("wmask", [128, 8, 512], BF16)
        E_in = inp("Esel", [32, 16, 128], BF16)
        sel48_in = inp("sel48", [48, 48, 128], BF16)
        ovl_in = inp("ovl", [128, 32])
        keep_in = inp("keepc", [128, 16, 32])
        addc_in = inp("addc", [128, 16, 32])
        P.push()
        kcT_sb = P.sb("kcT_sb", [128, 4, 127], BF16)
        vc_sb = P.sb("vc_sb", [128, 4, 128], BF16)
        P.push()
        raw_sb = P.sb("raw_sb", [128, 4, T], BF16)
        w1_sb = P.sb("w1_sb", [128, 32, 256], BF16)
        w2_sb = P.sb("w2_sb", [128, 2, 128], BF16)
        peT = P.sb("peT", [128, 32], F32)
        blk_all = P.sb("blk_all", [128, 32, 4, 127], BF16)
        hidT = P.sb("hidT", [128, 2, 508], BF16)
        for which in range(2):
            srcT = kTc if which == 0 else vcT
            P.load("sp", raw_sb, raw_sb[:, :, :], srcT.t.ap().rearrange("(g d) t -> d g t", d=128), src=srcT)
            P.load("pool", w1_sb, w1_sb[:, :, :], (w1k_in if which == 0 else w1v_in).t.ap().rearrange("j d f -> d j f"))
            P.load("pool", w2_sb, w2_sb[:, :, :], (w2k_in if which == 0 else w2v_in).t.ap().rearrange("(c f) d -> f c d", f=128))
            P.load("sp", peT, peT[:, :], (pek_in if which == 0 else pev_in)[:, :])
            pk = [psf.tiles[0], psf.tiles[1]]
            rv = raw_sb[:, :, :].rearrange("p g (c s) -> p g c s", s=16)
            for j in range(32):
                c0, jj = j // 16, j % 16
                P.op("dve", lambda e, c0=c0, jj=jj, j=j: e.tensor_scalar(
                    out=blk_all[:, j, :, :], in0=rv[:, :, c0:c0 + 127, jj], scalar1=peT[:, j:j + 1], scalar2=None,
                    op0=ALU.add), r=[raw_sb, peT], w=[blk_all])
            for fc in range(2):
                for g in range(4):
                    for j in range(32):
                        P.op("pe", lambda e, fc=fc, g=g, j=j: e.matmul(
                            pk[fc][:, g * 127:(g + 1) * 127], lhsT=w1_sb[:, j, fc * 128:(fc + 1) * 128],
                            rhs=blk_all[:, j, g, :], start=(j == 0), stop=(j == 31)), r=[w1_sb, blk_all], w=[pk[fc]])
            for fc in range(2):
                P.op("act", lambda e, fc=fc: e.activation(out=hidT[:, fc, :], in_=pk[fc][:, :508], func=AF.Silu),
                     r=[pk[fc]], w=[hidT])
            po_ = psf.tiles[2]
            if which == 0:
                for g in range(4):
                    for fc in range(2):
                        P.op("pe", lambda e, g=g, fc=fc: e.matmul(po_[:, g * 127:(g + 1) * 127], lhsT=w2_sb[:, fc, :],
                                                                  rhs=hidT[:, fc, g * 127:(g + 1) * 127],
                                                                  start=(fc == 0), stop=(fc == 1)),
                             r=[w2_sb, hidT], w=[po_])
                P.op("act", lambda e: e.copy(out=kcT_sb[:, :, :], in_=po_[:, :508].rearrange("p (g c) -> p g c", c=127)),
                     r=[po_], w=[kcT_sb])
            else:
                for g in range(4):
                    for fc in range(2):
                        P.op("pe", lambda e, g=g, fc=fc: e.matmul(po_[:127, g * 128:(g + 1) * 128],
                                                                  lhsT=hidT[:, fc, g * 127:(g + 1) * 127],
                                                                  rhs=w2_sb[:, fc, :], start=(fc == 0), stop=(fc == 1)),
                             r=[w2_sb, hidT], w=[po_])
                P.op("act", lambda e: e.copy(out=vc_sb[:127, :, :], in_=po_[:127, :].rearrange("p (g d) -> p g d", d=128)),
                     r=[po_], w=[vc_sb])
        P.pop()
        kTs_sb = P.sb("kTs_sb", [128, 4, T], BF16)
        kTw_sb = P.sb("kTw_sb", [128, 4, T], BF16)
        vs_sb = P.sb("vs_sb", [128, NT, 512], BF16)
        vw_sb = P.sb("vw_sb", [128, NT, 512], BF16)
        P.load("sp", kTs_sb, kTs_sb[:, :, :], kTs.t.ap().rearrange("(g d) t -> d g t", d=128), src=kTs)
        P.load("sp", kTw_sb, kTw_sb[:, :, :], kTw.t.ap().rearrange("(g d) t -> d g t", d=128), src=kTw)
        P.load("sp", vs_sb, vs_sb[:, :, :], vstok.t.ap().rearrange("(i p) c -> p i c", p=128), src=vstok)
        P.load("sp", vw_sb, vw_sb[:, :, :], vwtok.t.ap().rearrange("(i p) c -> p i c", p=128), src=vwtok)
        cmask = P.sb("cmask", [128, T], BF16)
        wmask = P.sb("wmask", [128, 8, 512], BF16)
        Esel = P.sb("Esel", [32, 16, 128], BF16)
        sel48 = P.sb("sel48", [48, 48, 128], BF16)
        ovl = P.sb("ovl", [128, 32], F32)
        keepc = P.sb("keepc", [128, 16, 32], F32)
        addc = P.sb("addc", [128, 16, 32], F32)
        P.load("sp", cmask, cmask[:, :], cmask_in[:, :])
        P.load("sp", wmask, wmask[:, :, :], wmask_in[:, :, :])
        P.load("sp", Esel, Esel[:, :, :], E_in[:, :, :])
        P.load("sp", sel48, sel48[:, :, :], sel48_in[:, :, :])
        P.load("sp", ovl, ovl[:, :], ovl_in[:, :])
        P.load("sp", keepc, keepc[:, :, :], keep_in[:, :, :])
        P.load("sp", addc, addc[:, :, :], addc_in[:, :, :])
        onesn = P.sb("onesn", [128, 128], BF16)
        P.op("dve", lambda e: e.memset(onesn[:, :], 1.0), w=[onesn])
        qg_r = Ring([P.sb("qg%d" % i, [128, 4, T], BF16) for i in range(2)])
        pT_r = Ring([P.sb("pTn%d" % i, [128, 512], BF16) for i in range(3)])
        yacc = P.sb("yacc", [128, 4, 512], F32)
        rz = P.sb("rz", [128, 512], F32)
        tmpn = P.sb("tmpn", [128, 512], F32)
        Pn = P.sb("Pn", [128, 512], F32)
        PnS = P.sb("PnS", [128, 512], F32)
        impp = P.sb("impp", [128, 32], F32)
        imp2 = P.sb("imp2", [128, 32], F32)
        m8a = P.sb("m8a", [128, 8], F32)
        m8b = P.sb("m8b", [128, 8], F32)
        selb = P.sb("selb", [128, 128], BF16)
        selbT = P.sb("selbT", [32, 512], BF16)
        ybf_r = Ring([P.sb("ybf%d" % i, [128, 512], BF16) for i in range(2)])
        Sb = [psf.tiles[0], psf.tiles[1]]
        n_po, n_pz, n_pg, n_pimp = psf.tiles[2], psf.tiles[3], psf.tiles[4], psf.tiles[5]
        qT_v = qT.t.ap().rearrange("(g r d) t -> g d r t", r=4, d=128)

        def attn_branch(qg, r, tb, units):
            n = len(units)
            qs = qg[:, r, tb * 512:(tb + 1) * 512]

            def issue_S(idx):
                kp, kl, ktl, biases, vl, vtl = units[idx]
                pS = Sb[idx % 2]
                mms = [(kl, qs, [ktl, qg])] + [(b[0], b[2], [b[1], b[3]]) for b in biases]
                for mi, (l_, r_, tls) in enumerate(mms):
                    P.op("pe", lambda e, l_=l_, r_=r_, pS=pS, mi=mi, last=(mi == len(mms) - 1), kp=kp: e.matmul(
                        pS[:kp, :], lhsT=l_, rhs=r_, start=(mi == 0), stop=last), r=tls, w=[pS])

            issue_S(0)
            pT = None
            for idx in range(n):
                if idx + 1 < n:
                    issue_S(idx + 1)
                kp, kl, ktl, biases, vl, vtl = units[idx]
                pS = Sb[idx % 2]
                pT = pT_r.next()
                P.op("act", lambda e, pS=pS, pT=pT, kp=kp: e.activation(out=pT[:kp, :], in_=pS[:kp, :], func=AF.Exp,
                                                                       scale=SCALE), r=[pS], w=[pT])
                P.op("pe", lambda e, pT=pT, vl=vl, kp=kp, idx=idx: e.matmul(n_po[:, :], lhsT=vl, rhs=pT[:kp, :],
                                                                          start=(idx == 0), stop=(idx == n - 1)),
                     r=[vtl, pT], w=[n_po])
                P.op("pe", lambda e, pT=pT, kp=kp, idx=idx: e.matmul(n_pz[:, :], lhsT=onesn[:kp, :], rhs=pT[:kp, :],
                                                                   start=(idx == 0), stop=(idx == n - 1)),
                     r=[onesn, pT], w=[n_pz])
            return pT

        def combine(r, tb, gate_row, first):
            P.op("pe", lambda e: e.matmul(n_pg[:, :], lhsT=sel48[:, gate_row, :], rhs=gT[:, tb * 512:(tb + 1) * 512],
                                          start=True, stop=True), r=[sel48, gT], w=[n_pg])
            P.op("dve", lambda e: e.tensor_scalar(out=rz[:, :], in0=n_pz[:, :], scalar1=1e-20, scalar2=None, op0=ALU.max),
                 r=[n_pz], w=[rz])
            P.op("dve", lambda e: e.reciprocal(out=rz[:, :], in_=rz[:, :]), r=[rz], w=[rz])
            P.op("dve", lambda e: e.tensor_tensor(out=tmpn[:, :], in0=n_pg[:, :], in1=rz[:, :], op=ALU.mult),
                 r=[n_pg, rz], w=[tmpn])
            if first:
                P.op("dve", lambda e: e.tensor_tensor(out=yacc[:, r, :], in0=n_po[:, :], in1=tmpn[:, :], op=ALU.mult),
                     r=[n_po, tmpn], w=[yacc])
            else:
                P.op("dve", lambda e: e.tensor_tensor(out=tmpn[:, :], in0=n_po[:, :], in1=tmpn[:, :], op=ALU.mult),
                     r=[n_po, tmpn], w=[tmpn])
                P.op("dve", lambda e: e.tensor_tensor(out=yacc[:, r, :], in0=yacc[:, r, :], in1=tmpn[:, :], op=ALU.add),
                     r=[yacc, tmpn], w=[yacc])

        def topk_tile(g, tb, a):
            qt = 4 * tb + a
            P.op("dve", lambda e: e.tensor_tensor(out=impp[:, :], in0=n_pimp[:, a * 32:(a + 1) * 32], in1=keepc[:, qt, :],
                                                  op=ALU.mult), r=[n_pimp, keepc], w=[impp])
            P.op("dve", lambda e: e.tensor_tensor(out=impp[:, :], in0=impp[:, :], in1=addc[:, qt, :], op=ALU.add),
                 r=[impp, addc], w=[impp])
            P.op("dve", lambda e: e.max(out=m8a[:, :], in_=impp[:, :]), r=[impp], w=[m8a], selfsync=True)
            P.op("dve", lambda e: e.match_replace(out=imp2[:, :], in_to_replace=m8a[:, :], in_values=impp[:, :],
                                                  imm_value=-1e30), r=[m8a, impp], w=[imp2], selfsync=True)
            P.op("dve", lambda e: e.max(out=m8b[:, :], in_=imp2[:, :]), r=[imp2], w=[m8b], selfsync=True)
            P.op("dve", lambda e: e.tensor_scalar(out=imp2[:, :], in0=impp[:, :], scalar1=m8b[:, 7:8], scalar2=30000.0,
                                                  op0=ALU.is_ge, op1=ALU.mult), r=[impp, m8b], w=[imp2], selfsync=True)
            P.op("dve", lambda e: e.tensor_scalar(out=selb[:, 0:32], in0=imp2[:, :], scalar1=-30000.0, scalar2=None,
                                                  op0=ALU.add), r=[imp2], w=[selb])
            if "dbg_sel" in dbg:
                P.store("sp", dbg_sel, dbg_sel[g, qt, :, :], selb, selb[:, 0:32])
                P.store("sp", dbg_imp, dbg_imp[g, qt, :, :], impp, impp[:, :])
            pb = psb.next()
            P.op("pe", lambda e, pb=pb: e.transpose(out=pb[:32, 0:128], in_=selb[:, 0:32], identity=ident[:, :]),
                 r=[selb, ident], w=[pb])
            P.op("act", lambda e, pb=pb: e.copy(out=selbT[:, a * 128:(a + 1) * 128], in_=pb[:32, 0:128]),
                 r=[pb], w=[selbT])

        for g in range(4):
            qg = qg_r.next()
            P.load("sp", qg, qg[:, :, :], qT_v[g], src=qT)
            for tb in range(4):
                for r in range(4):
                    head = g * 4 + r
                    units = [(127, kcT_sb[:, g, :], kcT_sb,
                              [(ident[:127, :127], ident, cmask[:127, tb * 512:(tb + 1) * 512], cmask)],
                              vc_sb[:127, g, :], vc_sb)]
                    pT = attn_branch(qg, r, tb, units)
                    combine(r, tb, 0 * 16 + head, True)
                    if tb >= 2:
                        if r == 0:
                            P.op("dve", lambda e, pT=pT: e.tensor_tensor(out=PnS[:127, :], in0=pT[:127, :],
                                                                         in1=rz[:127, :], op=ALU.mult),
                                 r=[pT, rz], w=[PnS])
                        else:
                            P.op("dve", lambda e, pT=pT: e.tensor_tensor(out=Pn[:127, :], in0=pT[:127, :],
                                                                         in1=rz[:127, :], op=ALU.mult),
                                 r=[pT, rz], w=[Pn])
                            P.op("dve", lambda e: e.tensor_tensor(out=PnS[:127, :], in0=PnS[:127, :], in1=Pn[:127, :],
                                                                  op=ALU.add), r=[PnS, Pn], w=[PnS])
                if tb >= 2:
                    for a in range(4):
                        P.op("pe", lambda e, a=a: e.matmul(n_pimp[:, a * 32:(a + 1) * 32],
                                                           lhsT=PnS[:127, a * 128:(a + 1) * 128], rhs=ovl[:127, :],
                                                           start=True, stop=True), r=[PnS, ovl], w=[n_pimp])
                if tb >= 2:
                    for a in range(4):
                        topk_tile(g, tb, a)
                for r in range(4):
                    head = g * 4 + r
                    units = []
                    for kt in range(4 * tb + 4):
                        biases = []
                        if tb >= 2:
                            biases.append((Esel[:, kt, :], Esel, selbT[:, :], selbT))
                        if kt >= 4 * tb:
                            biases.append((ident[:, :], ident, wmask[:, 4 + kt - 4 * tb, :], wmask))
                        units.append((128, kTs_sb[:, g, kt * 128:(kt + 1) * 128], kTs_sb, biases,
                                      vs_sb[:, kt, g * 128:(g + 1) * 128], vs_sb))
                    attn_branch(qg, r, tb, units)
                    combine(r, tb, 1 * 16 + head, False)
                    units = []
                    for kt in range(max(0, 4 * tb - 4), 4 * tb + 4):
                        biases = [(ident[:, :], ident, wmask[:, 4 + kt - 4 * tb, :], wmask)]
                        units.append((128, kTw_sb[:, g, kt * 128:(kt + 1) * 128], kTw_sb, biases,
                                      vw_sb[:, kt, g * 128:(g + 1) * 128], vw_sb))
                    attn_branch(qg, r, tb, units)
                    combine(r, tb, 2 * 16 + head, False)
                    ybf = ybf_r.next()
                    P.op("act", lambda e, ybf=ybf, r=r: e.copy(out=ybf[:, :], in_=yacc[:, r, :]), r=[yacc], w=[ybf])
                    P.store("sp", ynT, ynT[head * 128:(head + 1) * 128, tb * 512:(tb + 1) * 512], ybf, ybf[:, :])
        P.pop()
    if stop == "N":
        P.dma("sp", out_t[0:128, 0:512], f32a[:, :], r=[f32a], w=[out_t])
        P.emit()
        return nc, es

    P.push()
    wus_in = inp("w_up_ssd", [4096, D])
    wun_in = inp("w_up_nsa", [2048, D])
    wo_in = inp("w_out", [D, D])
    mT_d = dten("mT_d", [2048, T], BF16)
    h1_d = dten("h1_d", [T, D], F32)
    wus_v = wus_in.t.ap().rearrange("(kc p) n -> p kc n", p=128)
    wun_v = wun_in.t.ap().rearrange("(kc p) n -> p kc n", p=128)
    yT_v = yT.t.ap().rearrange("(kc p) t -> p kc t", p=128)
    ynT_v = ynT.t.ap().rearrange("(kc p) t -> p kc t", p=128)
    wqA = P.sb("wqA", [128, 32, 512], BF16)
    wqB = P.sb("wqB", [128, 16, 512], BF16)
    yTb_r = Ring([P.sb("yTb%d" % i, [128, 32, 512], BF16) for i in range(2)])
    nTb_r = Ring([P.sb("nTb%d" % i, [128, 16, 512], BF16) for i in range(2)])
    gs_r = Ring([P.sb("gsb%d" % i, [128, 512], BF16) for i in range(2)])
    gn_r = Ring([P.sb("gnb%d" % i, [128, 512], BF16) for i in range(2)])
    for qd in range(4):
        for h2_ in range(2):
            P.load("pool", wqA, wqA[:, h2_ * 16:(h2_ + 1) * 16, :], wus_v[:, h2_ * 16:(h2_ + 1) * 16, qd * 512:(qd + 1) * 512])
        P.load("pool", wqB, wqB[:, :, :], wun_v[:, :, qd * 512:(qd + 1) * 512])
        for tb in range(4):
            yTb = yTb_r.next()
            nTb = nTb_r.next()
            P.load("sp", yTb, yTb[:, :, :], yT_v[:, :, tb * 512:(tb + 1) * 512], src=yT)
            P.load("sp", nTb, nTb[:, :, :], ynT_v[:, :, tb * 512:(tb + 1) * 512], src=ynT)
            for f4 in range(4):
                f = qd * 4 + f4
                gsb = gs_r.next()
                gnb = gn_r.next()
                P.load("sp", gsb, gsb[:, :], gsT[f * 128:(f + 1) * 128, tb * 512:(tb + 1) * 512], src=gsT)
                P.load("sp", gnb, gnb[:, :], gnT[f * 128:(f + 1) * 128, tb * 512:(tb + 1) * 512], src=gnT)
                pA = psf.next()
                for kc in range(32):
                    P.op("pe", lambda e, kc=kc, pA=pA, yTb=yTb, f4=f4: e.matmul(
                        pA[:, :], lhsT=wqA[:, kc, f4 * 128:(f4 + 1) * 128], rhs=yTb[:, kc, :],
                        start=(kc == 0), stop=(kc == 31)), r=[wqA, yTb], w=[pA])
                pB = psf.next()
                for kc in range(16):
                    P.op("pe", lambda e, kc=kc, pB=pB, nTb=nTb, f4=f4: e.matmul(
                        pB[:, :], lhsT=wqB[:, kc, f4 * 128:(f4 + 1) * 128], rhs=nTb[:, kc, :],
                        start=(kc == 0), stop=(kc == 15)), r=[wqB, nTb], w=[pB])
                st = stg_r.next()
                P.op("dve", lambda e, pA=pA, gsb=gsb: e.tensor_tensor(out=f32a[:, :], in0=pA[:, :], in1=gsb[:, :], op=ALU.mult),
                     r=[pA, gsb], w=[f32a])
                P.op("dve", lambda e, pB=pB, gnb=gnb: e.tensor_tensor(out=f32b[:, :], in0=pB[:, :], in1=gnb[:, :], op=ALU.mult),
                     r=[pB, gnb], w=[f32b])
                P.op("dve", lambda e, st=st: e.tensor_tensor(out=st[:, :], in0=f32a[:, :], in1=f32b[:, :], op=ALU.add),
                     r=[f32a, f32b], w=[st])
                P.store("sp", mT_d, mT_d[f * 128:(f + 1) * 128, tb * 512:(tb + 1) * 512], st, st[:, :])
    P.pop()
    P.push()
    wo_sb = P.sb("wo_sb", [128, 16, D], BF16)
    wo_v = wo_in.t.ap().rearrange("(kc p) n -> p kc n", p=128)
    for cb in range(4):
        P.load("pool", wo_sb, wo_sb[:, :, cb * 512:(cb + 1) * 512], wo_v[:, :, cb * 512:(cb + 1) * 512])
    mT_v = mT_d.t.ap().rearrange("(kc p) t -> p kc t", p=128)
    mti_r = Ring([P.sb("mti%d" % i, [128, 16, 128], BF16) for i in range(2)])
    xr_r = Ring([P.sb("xr%d" % i, [128, D], F32) for i in range(2)])
    for i in range(NT):
        mti = mti_r.next()
        xr = xr_r.next()
        P.load("sp", mti, mti[:, :, :], mT_v[:, :, i * 128:(i + 1) * 128], src=mT_d)
        P.load("sp", xr, xr[:, :], x_in[i * 128:(i + 1) * 128, :])
        for cb in range(4):
            pt = psf.next()
            for kc in range(16):
                P.op("pe", lambda e, kc=kc, pt=pt, mti=mti, cb=cb: e.matmul(pt[:, :], lhsT=mti[:, kc, :],
                                                                          rhs=wo_sb[:, kc, cb * 512:(cb + 1) * 512],
                                                                          start=(kc == 0), stop=(kc == 15)),
                     r=[mti, wo_sb], w=[pt])
            P.op("dve", lambda e, pt=pt, xr=xr, cb=cb: e.tensor_tensor(out=xr[:, cb * 512:(cb + 1) * 512], in0=pt[:, :],
                                                                     in1=xr[:, cb * 512:(cb + 1) * 512], op=ALU.add),
                 r=[pt, xr], w=[xr])
        P.store("sp", h1_d, h1_d[i * 128:(i + 1) * 128, :], xr, xr[:, :])
    P.pop()
    if stop == "D":
        P.dma("sp", out_t[0:128, 0:512], f32a[:, :], r=[f32a], w=[out_t])
        P.emit()
        return nc, es

    P.push()
    mem_in = inp("mem", [256, D])
    n2_in = inp("norm2_w", [1, D])
    mn_in = inp("mem_norm_w", [1, D])
    xq_in = inp("xq_w", [D, 512])
    xkv_in = inp("xkv_w", [D, 1024])
    xqn_in = inp("x_q_norm_w", [1, 128])
    xkn_in = inp("x_k_norm_w", [1, 128])
    xo_in = inp("xo_w", [512, D])
    h2_d = dten("h2_d", [T, D], F32)
    wb2 = P.sb("wb2", [128, D], F32)
    wbm = P.sb("wbm", [128, D], F32)
    P.load("sp", wb2, wb2[:, :], n2_in[0:1, :].partition_broadcast(128))
    P.load("sp", wbm, wbm[:, :], mn_in[0:1, :].partition_broadcast(128))
    xqnw = P.sb("xqnw", [128, 4, 128], F32)
    xknw = P.sb("xknw", [128, 4, 128], F32)
    for hh in range(4):
        P.load("sp", xqnw, xqnw[:, hh, :], xqn_in[0:1, :].partition_broadcast(128))
        P.load("sp", xknw, xknw[:, hh, :], xkn_in[0:1, :].partition_broadcast(128))
    xq_sb = P.sb("xq_sb", [128, 16, 512], BF16)
    xkv_sb = P.sb("xkv_sb", [128, 16, 1024], BF16)
    xo_sb = P.sb("xo_sb", [128, 4, D], BF16)
    P.load("pool", xq_sb, xq_sb[:, :, :], xq_in.t.ap().rearrange("(kc p) n -> p kc n", p=128))
    P.load("pool", xkv_sb, xkv_sb[:, :, 0:512], xkv_in.t.ap().rearrange("(kc p) n -> p kc n", p=128)[:, :, 0:512])
    P.load("pool", xkv_sb, xkv_sb[:, :, 512:1024], xkv_in.t.ap().rearrange("(kc p) n -> p kc n", p=128)[:, :, 512:1024])
    for cb in range(4):
        P.load("pool", xo_sb, xo_sb[:, :, cb * 512:(cb + 1) * 512],
               xo_in.t.ap().rearrange("(kc p) n -> p kc n", p=128)[:, :, cb * 512:(cb + 1) * 512])
    onesb = P.sb("onesb", [128, 128], BF16)
    P.op("dve", lambda e: e.memset(onesb[:, :], 1.0), w=[onesb])
    rstd4e = P.sb("rstd4e", [128, 4], F32)
    hT_r = Ring([P.sb("hTe%d" % i, [128, 16, 128], BF16) for i in range(2)])
    h1t_r = Ring([P.sb("h1t%d" % i, [128, D], F32) for i in range(2)])
    kxT = P.sb("kxT", [128, 4, 256], BF16)
    vx = P.sb("vx", [128, 2, 512], BF16)
    qxT = P.sb("qxT", [128, 4, 128], BF16)
    pT_r = Ring([P.sb("pTe%d" % i, [128, 2, 128], BF16) for i in range(2)])
    oT = P.sb("oT", [128, 4, 128], BF16)
    rsum = P.sb("rsum", [128, 128], F32)

    def head_norm(pt, nw_tl, dst_hn):
        P.op("dve", lambda e: e.memset(rstd4e[:, :], 0.0), w=[rstd4e])
        for hh in range(4):
            P.op("act", lambda e, hh=hh: e.activation(out=f32a[:, hh * 128:(hh + 1) * 128],
                                                      in_=pt[:, hh * 128:(hh + 1) * 128], func=AF.Square,
                                                      accum_out=rstd4e[:, hh:hh + 1]), r=[pt], w=[f32a, rstd4e])
        P.op("dve", lambda e: e.tensor_scalar(out=rstd4e[:, :], in0=rstd4e[:, :], scalar1=1.0 / 128, scalar2=EPS,
                                              op0=ALU.mult, op1=ALU.add), r=[rstd4e], w=[rstd4e])
        P.op("act", lambda e: e.activation(out=rstd4e[:, :], in_=rstd4e[:, :], func=AF.Sqrt), r=[rstd4e], w=[rstd4e])
        P.op("dve", lambda e: e.reciprocal(out=rstd4e[:, :], in_=rstd4e[:, :]), r=[rstd4e], w=[rstd4e])
        P.op("dve", lambda e: e.tensor_tensor(out=f32a[:, :].rearrange("p (h d) -> p h d", d=128),
                                              in0=pt[:, :].rearrange("p (h d) -> p h d", d=128),
                                              in1=rstd4e[:, :].unsqueeze(2).to_broadcast([128, 4, 128]),
                                              op=ALU.mult), r=[pt, rstd4e], w=[f32a], selfsync=True)
        P.op("dve", lambda e: e.tensor_tensor(out=dst_hn[:, 0:512], in0=f32a[:, :],
                                              in1=nw_tl[:, :, :].rearrange("p h d -> p (h d)"),
                                              op=ALU.mult), r=[f32a, nw_tl], w=[dst_hn])

    for mt in range(2):
        h1t = h1t_r.next()
        hn = hn_r.next()
        hT = hT_r.next()
        P.load("sp", h1t, h1t[:, :], mem_in[mt * 128:(mt + 1) * 128, :])
        rmsnorm_tile(h1t, wbm, hn)
        transpose_into(hn, 16, lambda half, nb, hT=hT: hT[:, half * 8:half * 8 + nb, :], hT)
        for part in range(2):
            pt = psf.next()
            for kc in range(16):
                P.op("pe", lambda e, kc=kc, pt=pt, hT=hT, part=part: e.matmul(
                    pt[:, :], lhsT=hT[:, kc, :], rhs=xkv_sb[:, kc, part * 512:(part + 1) * 512],
                    start=(kc == 0), stop=(kc == 15)), r=[hT, xkv_sb], w=[pt])
            if part == 0:
                hk = hn_r.next()
                head_norm(pt, xknw, hk)
                transpose_into(hk, 4, lambda half, nb, mt=mt: kxT[:, :, mt * 128:(mt + 1) * 128], kxT)
            else:
                P.op("act", lambda e, pt=pt, mt=mt: e.copy(out=vx[:, mt, :], in_=pt[:, :]), r=[pt], w=[vx])
    for i in range(NT):
        h1t = h1t_r.next()
        hn = hn_r.next()
        hT = hT_r.next()
        P.load("sp", h1t, h1t[:, :], h1_d[i * 128:(i + 1) * 128, :], src=h1_d)
        rmsnorm_tile(h1t, wb2, hn)
        transpose_into(hn, 16, lambda half, nb, hT=hT: hT[:, half * 8:half * 8 + nb, :], hT)
        pt = psf.next()
        for kc in range(16):
            P.op("pe", lambda e, kc=kc, pt=pt, hT=hT: e.matmul(pt[:, :], lhsT=hT[:, kc, :], rhs=xq_sb[:, kc, :],
                                                             start=(kc == 0), stop=(kc == 15)),
                 r=[hT, xq_sb], w=[pt])
        hq = hn_r.next()
        head_norm(pt, xqnw, hq)
        transpose_into(hq, 4, lambda half, nb: qxT[:, :, :], qxT)
        for hh in range(4):
            pT = pT_r.next()
            for mt in range(2):
                psc = psf.next()
                P.op("pe", lambda e, psc=psc, hh=hh, mt=mt: e.matmul(psc[:, :128], lhsT=kxT[:, hh, mt * 128:(mt + 1) * 128],
                                                                     rhs=qxT[:, hh, :], start=True, stop=True),
                     r=[kxT, qxT], w=[psc])
                P.op("act", lambda e, psc=psc, pT=pT, mt=mt: e.activation(out=pT[:, mt, :], in_=psc[:, :128], func=AF.Exp,
                                                                         scale=float(128 ** -0.5)), r=[psc], w=[pT])
            po = psf.next()
            pz = psf.next()
            for mt in range(2):
                P.op("pe", lambda e, po=po, pT=pT, hh=hh, mt=mt: e.matmul(po[:, :128], lhsT=vx[:, mt, hh * 128:(hh + 1) * 128],
                                                                         rhs=pT[:, mt, :], start=(mt == 0), stop=(mt == 1)),
                     r=[vx, pT], w=[po])
            for mt in range(2):
                P.op("pe", lambda e, pz=pz, pT=pT, mt=mt: e.matmul(pz[:, :128], lhsT=onesb[:, :], rhs=pT[:, mt, :],
                                                                  start=(mt == 0), stop=(mt == 1)),
                     r=[onesb, pT], w=[pz])
            P.op("dve", lambda e, pz=pz: e.reciprocal(out=rsum[:, :], in_=pz[:, :128]), r=[pz], w=[rsum])
            P.op("dve", lambda e, po=po, hh=hh: e.tensor_tensor(out=oT[:, hh, :], in0=po[:, :128], in1=rsum[:, :],
                                                               op=ALU.mult), r=[po, rsum], w=[oT])
        for cb in range(4):
            pt = psf.next()
            for kc in range(4):
                P.op("pe", lambda e, kc=kc, pt=pt, cb=cb: e.matmul(pt[:, :], lhsT=oT[:, kc, :],
                                                                  rhs=xo_sb[:, kc, cb * 512:(cb + 1) * 512],
                                                                  start=(kc == 0), stop=(kc == 3)),
                     r=[oT, xo_sb], w=[pt])
            P.op("dve", lambda e, pt=pt, h1t=h1t, cb=cb: e.tensor_tensor(out=h1t[:, cb * 512:(cb + 1) * 512], in0=pt[:, :],
                                                                       in1=h1t[:, cb * 512:(cb + 1) * 512], op=ALU.add),
                 r=[pt, h1t], w=[h1t])
        P.store("sp", h2_d, h2_d[i * 128:(i + 1) * 128, :], h1t, h1t[:, :])
    P.pop()
    if stop == "E":
        P.dma("sp", out_t[0:128, 0:512], f32a[:, :], r=[f32a], w=[out_t])
        P.emit()
        return nc, es

    n3_in = inp("norm3_w", [1, D])
    rgw_in = inp("router_g_w", [D, 8])
    rew_in = inp("router_e_w", [D, 64])
    rb_in = inp("router_b", [1, 72])
    wg_in = inp("moe_w_gate", [64, D, EH])
    wu_in = inp("moe_w_up", [64, D, EH])
    wd_in = inp("moe_w_down", [64, EH, D])
    iota_in = inp("iota_row", [128, CG])
    gbase_in = inp("gbase", [128, 8])
    ustr_in = inp("ustrict", [128, 128], BF16)
    XgT_d = dten("XgT_d", [8, 128, 16, CG], BF16)
    Y_d = dten("Y_d", [8 * CG, D], F32)
    P.push()
    posm_all = P.sb("posm_all", [128, NT, 8], F32)
    wdh = P.sb("wdh", [128, NT, 16], BF16)
    ridx = P.sb("ridx", [128, NT], I32)
    wslot = P.sb("wslot", [128, 8, 3, 8], F32)
    onesb2 = P.sb("onesb2", [128, 128], BF16)
    ustr = P.sb("ustr", [128, 128], BF16)
    iota_row = P.sb("iota_row", [128, CG], F32)
    gbase = P.sb("gbase", [128, 8], F32)
    P.op("dve", lambda e: e.memset(onesb2[:, :], 1.0), w=[onesb2])
    P.load("sp", ustr, ustr[:, :], ustr_in[:, :])
    P.load("sp", iota_row, iota_row[:, :], iota_in[:, :])
    P.load("sp", gbase, gbase[:, :], gbase_in[:, :])
    P.push()
    hf_all = P.sb("hf_all", [128, NT, D], BF16)
    wb3 = P.sb("wb3", [128, D], F32)
    P.load("sp", wb3, wb3[:, :], n3_in[0:1, :].partition_broadcast(128))
    identf2 = P.sb("identf2", [128, 128], F32)
    P.load("sp", identf2, identf2[:, :], identf_in[:, :])
    wr_sb = P.sb("wr_sb", [128, 16, 72], F32)
    P.load("sp", wr_sb, wr_sb[:, :, 0:8], rgw_in.t.ap().rearrange("(kc p) n -> p kc n", p=128))
    P.load("sp", wr_sb, wr_sb[:, :, 8:72], rew_in.t.ap().rearrange("(kc p) n -> p kc n", p=128))
    rbias = P.sb("rbias", [128, 72], F32)
    P.load("sp", rbias, rbias[:, :], rb_in[0:1, :].partition_broadcast(128))
    h2t_r = Ring([P.sb("h2t%d" % i, [128, D], F32) for i in range(2)])
    hf32 = P.sb("hf32", [128, D], F32)
    hfT32 = P.sb("hfT32", [128, 16, 128], F32)
    cnt = P.sb("cnt", [128, 8], F32)
    P.op("dve", lambda e: e.memset(cnt[:, :], 0.0), w=[cnt])
    lg = P.sb("lg", [128, 72], F32)
    m8 = P.sb("m8", [128, 8], F32)
    m8e = P.sb("m8e", [128, 8], F32)
    oh = P.sb("oh", [128, 8], F32)
    ohb = P.sb("ohb", [128, 8], BF16)
    sc1 = P.sb("sc1", [128, 8], F32)
    le = P.sb("le", [128, 8], F32)
    e2 = P.sb("e2", [128, 8], F32)
    msk2 = P.sb("msk2", [128, 8], F32)
    wd32 = P.sb("wd32", [128, 8], F32)
    pos = P.sb("pos", [128, 8], F32)
    tmp8 = P.sb("tmp8", [128, 8], F32)
    junk8 = P.sb("junk8", [128, 8], F32)
    rid32 = P.sb("rid32", [128, 1], F32)

    def rms_f32(src_tl, wb_tl, dst_tl):
        ss = ss_r.next()
        P.op("dve", lambda e: e.memset(ss[:, 0:1], 0.0), w=[ss])
        P.op("act", lambda e: e.activation(out=junk[:, :], in_=src_tl[:, :], func=AF.Square, accum_out=ss[:, 0:1]),
             r=[src_tl], w=[junk, ss])
        P.op("dve", lambda e: e.tensor_scalar(out=ss[:, 0:1], in0=ss[:, 0:1], scalar1=1.0 / D, scalar2=EPS,
                                              op0=ALU.mult, op1=ALU.add), r=[ss], w=[ss])
        P.op("act", lambda e: e.activation(out=ss[:, 0:1], in_=ss[:, 0:1], func=AF.Sqrt), r=[ss], w=[ss])
        P.op("dve", lambda e: e.reciprocal(out=ss[:, 0:1], in_=ss[:, 0:1]), r=[ss], w=[ss])
        P.op("dve", lambda e: e.scalar_tensor_tensor(out=dst_tl[:, :], in0=src_tl[:, :], scalar=ss[:, 0:1],
                                                     in1=wb_tl[:, :], op0=ALU.mult, op1=ALU.mult),
             r=[src_tl, ss, wb_tl], w=[dst_tl])

    def route_tile(i):
        h2t = h2t_r.next()
        P.load("sp", h2t, h2t[:, :], h2_d[i * 128:(i + 1) * 128, :], src=h2_d)
        rms_f32(h2t, wb3, hf32)
        P.op("act", lambda e: e.copy(out=hf_all[:, i, :], in_=hf32[:, :]), r=[hf32], w=[hf_all])
        for q4 in range(4):
            pt = psf.next()
            for j in range(4):
                kc = q4 * 4 + j
                P.op("pe", lambda e, pt=pt, j=j, kc=kc: e.transpose(out=pt[:, j * 128:(j + 1) * 128],
                                                                   in_=hf32[:, kc * 128:(kc + 1) * 128],
                                                                   identity=identf2[:, :]),
                     r=[hf32, identf2], w=[pt])
            P.op("act", lambda e, pt=pt, q4=q4: e.copy(out=hfT32[:, q4 * 4:(q4 + 1) * 4, :],
                                                       in_=pt[:, :].rearrange("p (k t) -> p k t", t=128)),
                 r=[pt], w=[hfT32])
        pl = psf.next()
        for kc in range(16):
            P.op("pe", lambda e, kc=kc: e.matmul(pl[:, :72], lhsT=hfT32[:, kc, :], rhs=wr_sb[:, kc, :],
                                                 start=(kc == 0), stop=(kc == 15)), r=[hfT32, wr_sb], w=[pl])
        P.op("dve", lambda e: e.tensor_tensor(out=lg[:, :], in0=pl[:, :72], in1=rbias[:, :], op=ALU.add),
             r=[pl, rbias], w=[lg])
        P.op("dve", lambda e: e.max(out=m8[:, :], in_=lg[:, 0:8]), r=[lg], w=[m8])
        P.op("dve", lambda e: e.tensor_scalar(out=oh[:, :], in0=lg[:, 0:8], scalar1=m8[:, 0:1], scalar2=None,
                                              op0=ALU.is_ge), r=[lg, m8], w=[oh])
        P.op("act", lambda e: e.copy(out=ohb[:, :], in_=oh[:, :]), r=[oh], w=[ohb])
        P.op("dve", lambda e: e.tensor_scalar(out=sc1[:, 0:1], in0=m8[:, 0:1], scalar1=-1.0, scalar2=None,
                                              op0=ALU.mult), r=[m8], w=[sc1])
        P.op("dve", lambda e: e.memset(sc1[:, 1:2], 0.0), r=[sc1], w=[sc1])
        P.op("act", lambda e: e.activation(out=junk8[:, :], in_=lg[:, 0:8], func=AF.Exp, bias=sc1[:, 0:1],
                                           accum_out=sc1[:, 1:2]), r=[lg, sc1], w=[junk8, sc1])
        P.op("dve", lambda e: e.reciprocal(out=sc1[:, 2:3], in_=sc1[:, 1:2]), r=[sc1], w=[sc1])
        for g in range(8):
            if g == 0:
                P.op("dve", lambda e: e.tensor_scalar(out=le[:, :], in0=lg[:, 8:16], scalar1=oh[:, 0:1], scalar2=None,
                                                      op0=ALU.mult), r=[lg, oh], w=[le])
            else:
                P.op("dve", lambda e, g=g: e.scalar_tensor_tensor(out=le[:, :], in0=lg[:, 8 + g * 8:16 + g * 8],
                                                                  scalar=oh[:, g:g + 1], in1=le[:, :],
                                                                  op0=ALU.mult, op1=ALU.add), r=[lg, oh, le], w=[le])
        P.op("dve", lambda e: e.max(out=m8e[:, :], in_=le[:, :]), r=[le], w=[m8e])
        P.op("dve", lambda e: e.tensor_scalar(out=msk2[:, :], in0=le[:, :], scalar1=m8e[:, 1:2], scalar2=None,
                                              op0=ALU.is_ge), r=[le, m8e], w=[msk2])
        P.op("dve", lambda e: e.tensor_scalar(out=sc1[:, 3:4], in0=m8e[:, 0:1], scalar1=-1.0, scalar2=None,
                                              op0=ALU.mult), r=[m8e], w=[sc1])
        P.op("act", lambda e: e.activation(out=e2[:, :], in_=le[:, :], func=AF.Exp, bias=sc1[:, 3:4]),
             r=[le, sc1], w=[e2])
        P.op("act", lambda e: e.activation(out=sc1[:, 4:5], in_=m8e[:, 1:2], func=AF.Exp, bias=sc1[:, 3:4]),
             r=[m8e, sc1], w=[sc1])
        P.op("dve", lambda e: e.tensor_scalar(out=sc1[:, 4:5], in0=sc1[:, 4:5], scalar1=1.0, scalar2=None,
                                              op0=ALU.add), r=[sc1], w=[sc1])
        P.op("dve", lambda e: e.reciprocal(out=sc1[:, 4:5], in_=sc1[:, 4:5]), r=[sc1], w=[sc1])
        P.op("dve", lambda e: e.tensor_tensor(out=sc1[:, 5:6], in0=sc1[:, 4:5], in1=sc1[:, 2:3], op=ALU.mult),
             r=[sc1], w=[sc1])
        P.op("dve", lambda e: e.scalar_tensor_tensor(out=wd32[:, :], in0=e2[:, :], scalar=sc1[:, 5:6], in1=msk2[:, :],
                                                     op0=ALU.mult, op1=ALU.mult), r=[e2, sc1, msk2], w=[wd32])
        P.op("act", lambda e: e.copy(out=wdh[:, i, 0:8], in_=wd32[:, :]), r=[wd32], w=[wdh])
        P.op("dve", lambda e: e.tensor_tensor(out=tmp8[:, :], in0=wd32[:, :], in1=wdh[:, i, 0:8], op=ALU.subtract),
             r=[wd32, wdh], w=[tmp8])
        P.op("act", lambda e: e.copy(out=wdh[:, i, 8:16], in_=tmp8[:, :]), r=[tmp8], w=[wdh])
        pp = psf.next()
        P.op("pe", lambda e: e.matmul(pp[:, 0:8], lhsT=ustr[:, :], rhs=ohb[:, :], start=True, stop=True),
             r=[ustr, ohb], w=[pp])
        P.op("dve", lambda e: e.tensor_tensor(out=pos[:, :], in0=pp[:, 0:8], in1=cnt[:, :], op=ALU.add),
             r=[pp, cnt], w=[pos])
        pc2 = psf.next()
        P.op("pe", lambda e: e.matmul(pc2[:, 0:8], lhsT=onesb2[:, :], rhs=ohb[:, :], start=True, stop=True),
             r=[onesb2, ohb], w=[pc2])
        P.op("dve", lambda e: e.tensor_tensor(out=cnt[:, :], in0=cnt[:, :], in1=pc2[:, 0:8], op=ALU.add),
             r=[cnt, pc2], w=[cnt])
        P.op("dve", lambda e: e.scalar_tensor_tensor(out=tmp8[:, :], in0=pos[:, :], scalar=1.0, in1=oh[:, :],
                                                     op0=ALU.add, op1=ALU.mult), r=[pos, oh], w=[tmp8])
        P.op("dve", lambda e: e.tensor_scalar(out=posm_all[:, i, :], in0=tmp8[:, :], scalar1=-1.0, scalar2=None,
                                              op0=ALU.add), r=[tmp8], w=[posm_all])
        P.op("dve", lambda e: e.tensor_tensor(out=tmp8[:, :], in0=pos[:, :], in1=gbase[:, :], op=ALU.add),
             r=[pos, gbase], w=[tmp8])
        P.op("dve", lambda e: e.tensor_tensor(out=tmp8[:, :], in0=tmp8[:, :], in1=oh[:, :], op=ALU.mult),
             r=[tmp8, oh], w=[tmp8])
        P.op("dve", lambda e: e.memset(rid32[:, :], 0.0), w=[rid32])
        P.op("act", lambda e: e.activation(out=junk8[:, :], in_=tmp8[:, :], func=AF.Identity, accum_out=rid32[:, 0:1]),
             r=[tmp8, rid32], w=[junk8, rid32])
        P.op("dve", lambda e: e.tensor_copy(out=ridx[:, i:i + 1], in_=rid32[:, 0:1]), r=[rid32], w=[ridx])

    for i in range(NT):
        route_tile(i)

    Sg = P.sb("Sg", [128, NT, CG], BF16)
    xstg = P.sb("xstg", [128, 16, CG], BF16)

    def gather_group(g):
        for i in range(NT):
            P.op("dve", lambda e, i=i: e.tensor_scalar(out=Sg[:, i, :], in0=iota_row[:, :],
                                                       scalar1=posm_all[:, i, g:g + 1], scalar2=None,
                                                       op0=ALU.is_equal), r=[iota_row, posm_all], w=[Sg])
        for kc in range(16):
            pt = psf.next()
            for i in range(NT):
                P.op("pe", lambda e, pt=pt, kc=kc, i=i: e.matmul(pt[:, :CG], lhsT=hf_all[:, i, kc * 128:(kc + 1) * 128],
                                                                rhs=Sg[:, i, :], start=(i == 0), stop=(i == NT - 1)),
                     r=[hf_all, Sg], w=[pt])
            P.op("act", lambda e, pt=pt, kc=kc: e.copy(out=xstg[:, kc, :], in_=pt[:, :CG]), r=[pt], w=[xstg])
        P.store("sp", XgT_d, XgT_d[g, :, :, :], xstg, xstg[:, :, :])
        for ct in range(3):
            pw = psf.next()
            n = 0
            for i in range(NT):
                for hl in range(2):
                    P.op("pe", lambda e, pw=pw, i=i, hl=hl, ct=ct, n=n: e.matmul(
                        pw[:, 0:8], lhsT=Sg[:, i, ct * 128:(ct + 1) * 128], rhs=wdh[:, i, hl * 8:(hl + 1) * 8],
                        start=(n == 0), stop=(n == 2 * NT - 1)), r=[Sg, wdh], w=[pw])
                    n += 1
            P.op("act", lambda e, pw=pw, ct=ct: e.copy(out=wslot[:, g, ct, :], in_=pw[:, 0:8]), r=[pw], w=[wslot])

    for g in range(8):
        gather_group(g)
    P.pop()

    P.push()
    xg = P.sb("xg", [128, 16, CG], BF16)
    yg = P.sb("yg", [128, 3, D], F32)
    wgb_r = Ring([P.sb("wgb%d" % i, [128, 16, 512], BF16) for i in range(2)])
    wub_r = Ring([P.sb("wub%d" % i, [128, 16, 512], BF16) for i in range(2)])
    wdb_r = Ring([P.sb("wdb%d" % i, [128, 4, D], BF16) for i in range(2)])
    hact_r = Ring([P.sb("hact%d" % i, [128, 512], BF16) for i in range(2)])
    hactT_r = Ring([P.sb("hactT%d" % i, [128, 4, 128], BF16) for i in range(2)])
    sg32 = P.sb("sg32", [128, 512], F32)
    FB = [(0, 512), (512, 512), (1024, 384)]

    def expert(g, j, first):
        e_id = g * 8 + j
        for (f0, nf) in FB:
            wgb = wgb_r.next()
            wub = wub_r.next()
            wdb = wdb_r.next()
            nch = nf // 128
            P.load("pool", wgb, wgb[:, :, :nf], wg_in.t.ap()[e_id].rearrange("(kc p) n -> p kc n", p=128)[:, :, f0:f0 + nf])
            P.load("pool", wub, wub[:, :, :nf], wu_in.t.ap()[e_id].rearrange("(kc p) n -> p kc n", p=128)[:, :, f0:f0 + nf])
            P.load("pool", wdb, wdb[:, :nch, :], wd_in.t.ap()[e_id, f0:f0 + nf, :].rearrange("(c p) n -> p c n", p=128))
            for ct in range(3):
                pga = psf.next()
                for kc in range(16):
                    P.op("pe", lambda e, pga=pga, kc=kc, ct=ct, wgb=wgb, nf=nf: e.matmul(
                        pga[:, :nf], lhsT=xg[:, kc, ct * 128:(ct + 1) * 128], rhs=wgb[:, kc, :nf],
                        start=(kc == 0), stop=(kc == 15)), r=[xg, wgb], w=[pga])
                pup = psf.next()
                for kc in range(16):
                    P.op("pe", lambda e, pup=pup, kc=kc, ct=ct, wub=wub, nf=nf: e.matmul(
                        pup[:, :nf], lhsT=xg[:, kc, ct * 128:(ct + 1) * 128], rhs=wub[:, kc, :nf],
                        start=(kc == 0), stop=(kc == 15)), r=[xg, wub], w=[pup])
                hact = hact_r.next()
                P.op("act", lambda e, pga=pga, nf=nf: e.activation(out=sg32[:, :nf], in_=pga[:, :nf], func=AF.Silu),
                     r=[pga], w=[sg32])
                P.op("dve", lambda e, pup=pup, hact=hact, nf=nf, ct=ct: e.scalar_tensor_tensor(
                    out=hact[:, :nf], in0=sg32[:, :nf], scalar=wslot[:, g, ct, j:j + 1], in1=pup[:, :nf],
                    op0=ALU.mult, op1=ALU.mult), r=[sg32, wslot, pup], w=[hact])
                hactT = hactT_r.next()
                pb = psb.next()
                for c4 in range(nch):
                    P.op("pe", lambda e, pb=pb, c4=c4, hact=hact: e.transpose(out=pb[:, c4 * 128:(c4 + 1) * 128],
                                                                             in_=hact[:, c4 * 128:(c4 + 1) * 128],
                                                                             identity=ident[:, :]),
                         r=[hact, ident], w=[pb])
                P.op("act", lambda e, pb=pb, hactT=hactT, nch=nch: e.copy(
                    out=hactT[:, :nch, :], in_=pb[:, :nch * 128].rearrange("p (k t) -> p k t", t=128)),
                    r=[pb], w=[hactT])
                for db in range(4):
                    pdn = psf.next()
                    for c4 in range(nch):
                        P.op("pe", lambda e, pdn=pdn, c4=c4, db=db, hactT=hactT, wdb=wdb, nch=nch: e.matmul(
                            pdn[:, :], lhsT=hactT[:, c4, :], rhs=wdb[:, c4, db * 512:(db + 1) * 512],
                            start=(c4 == 0), stop=(c4 == nch - 1)), r=[hactT, wdb], w=[pdn])
                    if first and f0 == 0:
                        P.op("act", lambda e, pdn=pdn, ct=ct, db=db: e.copy(out=yg[:, ct, db * 512:(db + 1) * 512],
                                                                           in_=pdn[:, :]), r=[pdn], w=[yg])
                    else:
                        P.op("dve", lambda e, pdn=pdn, ct=ct, db=db: e.tensor_tensor(
                            out=yg[:, ct, db * 512:(db + 1) * 512], in0=yg[:, ct, db * 512:(db + 1) * 512],
                            in1=pdn[:, :], op=ALU.add), r=[yg, pdn], w=[yg])

    for g in range(8):
        P.load("sp", xg, xg[:, :, :], XgT_d[g, :, :, :], src=XgT_d)
        for j in range(8):
            expert(g, j, j == 0)
        P.store("sp", Y_d, Y_d.t.ap()[g * CG:(g + 1) * CG, :].rearrange("(c p) n -> p c n", p=128), yg, yg[:, :, :])
    P.pop()

    P.push()
    yr_r = Ring([P.sb("yr%d" % i, [128, D], F32) for i in range(2)])
    h2o_r = Ring([P.sb("h2o%d" % i, [128, D], F32) for i in range(2)])
    for i in range(NT):
        yr = yr_r.next()
        h2o = h2o_r.next()
        P.load("sp", h2o, h2o[:, :], h2_d[i * 128:(i + 1) * 128, :], src=h2_d)
        P.dma_fn("pool", lambda e, yr=yr, i=i: e.indirect_dma_start(
            out=yr[:, :], out_offset=None, in_=Y_d[:, :],
            in_offset=bass.IndirectOffsetOnAxis(ap=ridx[:, i:i + 1], axis=0)),
            r=[Y_d, ridx], w=[yr], key=yr.b.name)
        P.op("dve", lambda e, yr=yr, h2o=h2o: e.tensor_tensor(out=h2o[:, :], in0=h2o[:, :], in1=yr[:, :], op=ALU.add),
             r=[h2o, yr], w=[h2o])
        P.store("sp", out_t, out_t[i * 128:(i + 1) * 128, :], h2o, h2o[:, :])
    P.pop()
    P.pop()

    P.emit()
    return nc, es


def rope_tables():
    half = 64
    inv = (10000.0 ** (-np.arange(half, dtype=np.float32) / half)).astype(np.float32)
    pos = np.arange(T, dtype=np.float32)
    ang = pos[:, None] * inv[None, :]
    cos = np.cos(ang).astype(np.float32).reshape(NT, 128, 64).transpose(1, 0, 2)
    sin = np.sin(ang).astype(np.float32).reshape(NT, 128, 64).transpose(1, 0, 2)
    return np.ascontiguousarray(cos), np.ascontiguousarray(sin)


_NSA_CONST = None


def nsa_constants():
    global _NSA_CONST
    if _NSA_CONST is not None:
        return _NSA_CONST
    bf = ml_dtypes.bfloat16
    c = np.arange(128)[:, None]
    q = np.arange(T)[None, :]
    cmask = np.where((16 * c + 31 <= q) & (c < 127), 0.0, NEG).astype(np.float32)
    wmask = np.zeros((128, 8, 512), np.float32)
    k = np.arange(128)[:, None]
    qq = np.arange(512)[None, :]
    for idx in range(8):
        rel = idx - 4
        diff = qq - (rel * 128 + k)
        wmask[:, idx, :] = np.where((diff >= 0) & (diff < 512), 0.0, NEG)
    E = np.zeros((32, 16, 128), np.float32)
    for kt in range(16):
        for kk in range(128):
            E[2 * kt + kk // 64, kt, kk] = 1.0
    sel48 = np.zeros((48, 48, 128), np.float32)
    for j in range(48):
        sel48[j, j, :] = 1.0
    ci = np.arange(128)[:, None]
    sj = np.arange(32)[None, :]
    ovl = ((ci * 16 < (sj + 1) * 64) & (ci * 16 + 32 > sj * 64) & (ci < 127)).astype(np.float32)
    keep = np.zeros((128, 16, 32), np.float32)
    addc = np.zeros((128, 16, 32), np.float32)
    for qt in range(16):
        tq = qt * 128 + np.arange(128)[:, None]
        blk_t = tq // 64
        sb_ = np.arange(32)[None, :]
        valid = sb_ <= blk_t
        forced = (sb_ == 0) | (sb_ == blk_t) | (sb_ == blk_t - 1)
        keep[:, qt, :] = (valid & ~forced).astype(np.float32)
        addc[:, qt, :] = np.where(~valid, -1e30, np.where(forced, 1e4 + sb_, 0.0))
    _NSA_CONST = {"cmask": cmask.astype(bf), "wmask": wmask.astype(bf), "Esel": E.astype(bf), "sel48": sel48.astype(bf),
                  "ovl": ovl, "keepc": keep, "addc": addc}
    return _NSA_CONST


def make_in_map(inputs, b):
    f = np.float32
    cos, sin = rope_tables()
    m = {}
    m["x"] = np.ascontiguousarray(inputs["x"][b], dtype=f)
    m["w_in"] = np.ascontiguousarray(inputs["w_in"][0], dtype=f)
    m["norm1_w"] = np.ascontiguousarray(inputs["norm1_w"][0].reshape(1, D), dtype=f)
    m["conv_w"] = np.ascontiguousarray(inputs["ssd_conv_w"][0].reshape(4, 48, 128).transpose(2, 1, 0), dtype=f)
    m["conv_b"] = np.ascontiguousarray(inputs["ssd_conv_b"][0].reshape(48, 128).T, dtype=f)
    m["qnw"] = np.ascontiguousarray(inputs["nsa_q_norm_w"][0].reshape(1, 128), dtype=f)
    m["knw"] = np.ascontiguousarray(inputs["nsa_k_norm_w"][0].reshape(3, 128), dtype=f)
    m["rope_cos"] = cos
    m["rope_sin"] = sin
    m["ident"] = np.eye(128, dtype=np.float32).astype(ml_dtypes.bfloat16)
    m["identf"] = np.eye(128, dtype=np.float32)
    m["triu"] = np.triu(np.ones((128, 128), dtype=np.float32))
    m["dt_bias"] = np.ascontiguousarray(inputs["ssd_dt_bias"][0].reshape(1, 64), dtype=f)
    m["a_log"] = np.ascontiguousarray(inputs["ssd_a_log"][0].reshape(1, 64), dtype=f)
    m["d_skip"] = np.ascontiguousarray(inputs["ssd_d"][0].reshape(1, 64), dtype=f)
    m["ssd_norm_w"] = np.ascontiguousarray(inputs["ssd_norm_w"][0].reshape(1, 4096), dtype=f)
    m.update(nsa_constants())
    m["cmp_pe_kT"] = np.ascontiguousarray(inputs["cmp_pe_k"][0].T, dtype=f)
    m["cmp_pe_vT"] = np.ascontiguousarray(inputs["cmp_pe_v"][0].T, dtype=f)
    m["cmp_w1_k"] = np.ascontiguousarray(inputs["cmp_w1_k"][0], dtype=f)
    m["cmp_w1_v"] = np.ascontiguousarray(inputs["cmp_w1_v"][0], dtype=f)
    m["cmp_w2_k"] = np.ascontiguousarray(inputs["cmp_w2_k"][0], dtype=f)
    m["cmp_w2_v"] = np.ascontiguousarray(inputs["cmp_w2_v"][0], dtype=f)
    m["w_up_ssd"] = np.ascontiguousarray(inputs["w_up_ssd"][0], dtype=f)
    m["w_up_nsa"] = np.ascontiguousarray(inputs["w_up_nsa"][0], dtype=f)
    m["w_out"] = np.ascontiguousarray(inputs["w_out"][0], dtype=f)
    m["mem"] = np.ascontiguousarray(inputs["mem"][b], dtype=f)
    m["norm2_w"] = np.ascontiguousarray(inputs["norm2_w"][0].reshape(1, D), dtype=f)
    m["mem_norm_w"] = np.ascontiguousarray(inputs["mem_norm_w"][0].reshape(1, D), dtype=f)
    m["xq_w"] = np.ascontiguousarray(inputs["xq_w"][0], dtype=f)
    m["xkv_w"] = np.ascontiguousarray(inputs["xkv_w"][0], dtype=f)
    m["x_q_norm_w"] = np.ascontiguousarray(inputs["x_q_norm_w"][0].reshape(1, 128), dtype=f)
    m["x_k_norm_w"] = np.ascontiguousarray(inputs["x_k_norm_w"][0].reshape(1, 128), dtype=f)
    m["xo_w"] = np.ascontiguousarray(inputs["xo_w"][0], dtype=f)
    if "moe_w_gate" in inputs:
        m["norm3_w"] = np.ascontiguousarray(inputs["norm3_w"][0].reshape(1, D), dtype=f)
        m["router_g_w"] = np.ascontiguousarray(inputs["router_g_w"][0], dtype=f)
        m["router_e_w"] = np.ascontiguousarray(inputs["router_e_w"][0], dtype=f)
        m["router_b"] = np.ascontiguousarray(
            np.concatenate([inputs["router_g_b"][0].reshape(-1), inputs["router_e_b"][0].reshape(-1)]).reshape(1, 72), dtype=f)
        m["moe_w_gate"] = np.ascontiguousarray(inputs["moe_w_gate"][0], dtype=f)
        m["moe_w_up"] = np.ascontiguousarray(inputs["moe_w_up"][0], dtype=f)
        m["moe_w_down"] = np.ascontiguousarray(inputs["moe_w_down"][0], dtype=f)
        m["iota_row"] = np.ascontiguousarray(np.broadcast_to(np.arange(CG, dtype=f)[None, :], (128, CG)))
        m["gbase"] = np.ascontiguousarray(np.broadcast_to((np.arange(8, dtype=f) * CG)[None, :], (128, 8)))
        m["ustrict"] = np.triu(np.ones((128, 128), dtype=f), k=1).astype(ml_dtypes.bfloat16)
    return m


def kernel(**inputs):
    nc, es = build()
    in_maps = [make_in_map(inputs, b) for b in range(8)]
    res = run_bass_kernel_spmd(nc, in_maps, core_ids=list(range(8)))
    out = np.stack([np.asarray(r["out"], dtype=np.float32) for r in res.results], axis=0)
    return out
```

```python
import numpy as np
import ml_dtypes
from contextlib import ExitStack
import concourse.bass as bass
import concourse.mybir as mybir
from concourse.bass_utils import run_bass_kernel_spmd

F32 = mybir.dt.float32
BF16 = mybir.dt.bfloat16
I32 = mybir.dt.int32
ALU = mybir.AluOpType
AF = mybir.ActivationFunctionType
AX = mybir.AxisListType

T = 2048
D = 2048
NT = 16
EPS = 1e-6
NEG = -30000.0
IN_SPLITS = (4096, 6144, 64, 2048, 512, 512, 512, 512, 512, 512, 48, 2048, 2048)
IN_COLS = sum(IN_SPLITS)
SEG_NAMES = ("z", "xbc", "dt", "q", "kc", "vc", "ks", "vs", "kw", "vw", "nsag", "gssd", "gnsa")
SEG_OFF = dict(zip(SEG_NAMES, np.cumsum((0,) + IN_SPLITS[:-1]).tolist()))
SEG_LEN = dict(zip(SEG_NAMES, IN_SPLITS))
CG = 384
NGRP = 8
EH = 1408

ENGS = ("pe", "dve", "act", "pool", "sp")


class Buf:
    __slots__ = ("name", "w", "rs")

    def __init__(self, name):
        self.name = name
        self.w = None
        self.rs = []


class Tl:
    def __init__(self, t, name):
        self.t = t
        self.b = Buf(name)

    def __getitem__(self, k):
        return self.t[k]


class Op:
    __slots__ = ("eng", "fn", "deps", "marked", "idx", "is_dma", "dsem", "dval")

    def __init__(self, eng, fn):
        self.eng = eng
        self.fn = fn
        self.deps = []
        self.marked = False
        self.idx = -1
        self.is_dma = False
        self.dsem = None
        self.dval = 0


class Prog:
    def __init__(self, nc, es):
        self.nc = nc
        self.es = es
        self.ops = {e: [] for e in ENGS}
        self.sems = {e: es.enter_context(nc.semaphore("s_" + e)) for e in ENGS}
        self.waited = {e: {f: -1 for f in ENGS} for e in ENGS}
        self.waited_d = {e: {} for e in ENGS}
        self.keymap = {}
        self.sem_pool = []
        self.sem_count = []
        self.sem_free = []
        self.last_dma = {}
        self.scopes = [es]

    def sb(self, name, shape, dtype):
        t = self.scopes[-1].enter_context(self.nc.sbuf_tensor("sb_" + name, list(shape), dtype))
        return Tl(t, name)

    def push(self):
        self.scopes.append(ExitStack())

    def pop(self):
        self.barrier()
        self.scopes.pop().close()

    def ps(self, name, shape, dtype):
        t = self.es.enter_context(self.nc.psum_tensor("ps_" + name, list(shape), dtype))
        return Tl(t, name)

    def dram(self, name, shape, dtype, kind="Internal"):
        t = self.nc.dram_tensor(name, list(shape), dtype, kind=kind)
        return Tl(t, name)

    def _dsem(self, key):
        if key not in self.keymap:
            if self.sem_free:
                idx = self.sem_free.pop()
            else:
                idx = len(self.sem_pool)
                self.sem_pool.append(self.es.enter_context(self.nc.semaphore("d_%d" % idx)))
                self.sem_count.append(0)
            self.keymap[key] = idx
        return self.keymap[key]

    def op(self, eng, fn, r=(), w=(), selfsync=False):
        o = Op(eng, fn)
        o.idx = len(self.ops[eng])
        raw = []
        war = []
        for t in r:
            if t.b.w is not None:
                raw.append(t.b.w)
        for t in w:
            if t.b.w is not None:
                raw.append(t.b.w)
            war.extend(t.b.rs)
        for d, is_raw in [(d, True) for d in raw] + [(d, False) for d in war]:
            if d is o:
                continue
            if d.is_dma:
                cur = self.waited_d[eng].get(d.dsem, 0)
                if d.dval > cur:
                    self.waited_d[eng][d.dsem] = d.dval
                    o.deps.append(d)
            else:
                if d.eng == eng:
                    if eng != "pe" and is_raw and d.fn is not None and d.idx > self.waited[eng][eng]:
                        self.waited[eng][eng] = d.idx
                        d.marked = True
                        o.deps.append(d)
                    continue
                if d.idx > self.waited[eng][d.eng]:
                    self.waited[eng][d.eng] = d.idx
                    d.marked = True
                    o.deps.append(d)
        for t in r:
            t.b.rs.append(o)
        for t in w:
            t.b.w = o
            t.b.rs = []
        self.ops[eng].append(o)
        return o

    def dma(self, q, out, in_, r=(), w=(), key=None, **kw):
        if key is None:
            key = r[0].b.name if r else w[0].b.name

        def fn(e):
            return e.dma_start(out=out, in_=in_, **kw)

        o = Op(q, fn)
        o.is_dma = True
        o.idx = len(self.ops[q])
        deps = []
        for t in r:
            if t.b.w is not None:
                deps.append(t.b.w)
        for t in w:
            if t.b.w is not None:
                deps.append(t.b.w)
            deps.extend(t.b.rs)
        for d in deps:
            if d.is_dma:
                cur = self.waited_d[q].get(d.dsem, 0)
                if d.dval > cur:
                    self.waited_d[q][d.dsem] = d.dval
                    o.deps.append(d)
            else:
                if d.eng == q:
                    d.marked = True
                    o.deps.append(d)
                elif d.idx > self.waited[q][d.eng]:
                    self.waited[q][d.eng] = d.idx
                    d.marked = True
                    o.deps.append(d)
        sidx = self._dsem(key)
        o.dsem = self.sem_pool[sidx]
        self.sem_count[sidx] += 16
        o.dval = self.sem_count[sidx]
        self.last_dma[key] = o
        for t in r:
            t.b.rs.append(o)
        for t in w:
            t.b.w = o
            t.b.rs = []
        self.ops[q].append(o)
        return o

    def dma_fn(self, q, fn, r=(), w=(), key=None):
        o = self.dma(q, None, None, r=r, w=w, key=key)
        o.fn = fn
        return o

    def store(self, q, dram_tl, out, src_tl, in_, **kw):
        return self.dma(q, out, in_, r=[src_tl], w=[dram_tl], key=src_tl.b.name, **kw)

    def load(self, q, dst_tl, out, in_, src=None, **kw):
        return self.dma(q, out, in_, r=([src] if src is not None else []), w=[dst_tl], key=dst_tl.b.name, **kw)

    def barrier(self):
        lasts = {}
        for f in ENGS:
            for o in reversed(self.ops[f]):
                if o.fn is not None and not o.is_dma:
                    lasts[f] = o
                    break
        for e in ENGS:
            o = Op(e, None)
            o.idx = len(self.ops[e])
            for f, lo in lasts.items():
                if f == e:
                    continue
                if lo.idx > self.waited[e][f]:
                    self.waited[e][f] = lo.idx
                    lo.marked = True
                    o.deps.append(lo)
            for key, d in self.last_dma.items():
                cur = self.waited_d[e].get(d.dsem, 0)
                if d.dval > cur:
                    self.waited_d[e][d.dsem] = d.dval
                    o.deps.append(d)
            self.ops[e].append(o)
        self.last_dma = {}
        self.keymap = {}
        self.sem_free = list(range(len(self.sem_pool)))

    def emit(self):
        nc = self.nc
        val = {}
        for e in ENGS:
            c = 0
            for o in self.ops[e]:
                if o.marked and not o.is_dma:
                    c += 1
                    val[id(o)] = c
        final_waits = [(self.sem_pool[i], self.sem_count[i]) for i in range(len(self.sem_pool))]

        def run(ename, eng):
            for o in self.ops[ename]:
                for d in o.deps:
                    if d.is_dma:
                        eng.wait_ge(d.dsem, d.dval)
                    else:
                        eng.wait_ge(self.sems[d.eng], val[id(d)])
                if o.fn is None:
                    continue
                ins = o.fn(eng)
                if o.is_dma:
                    ins.then_inc(o.dsem, 16)
                elif o.marked:
                    ins.then_inc(self.sems[ename], 1)
            if ename == "sp":
                for s, v in final_waits:
                    if v > 0:
                        eng.wait_ge(s, v)

        with nc.Block() as block:
            @block.tensor
            def _(pe):
                run("pe", pe)

            @block.vector
            def _(dve):
                run("dve", dve)

            @block.scalar
            def _(act):
                run("act", act)

            @block.gpsimd
            def _(pool):
                run("pool", pool)

            @block.sync
            def _(sp):
                run("sp", sp)


class Ring:
    def __init__(self, tiles):
        self.tiles = tiles
        self.i = 0

    def next(self):
        t = self.tiles[self.i % len(self.tiles)]
        self.i += 1
        return t


def seg_blocks():
    out = []
    for s in SEG_NAMES:
        off, ln = SEG_OFF[s], SEG_LEN[s]
        c = 0
        while c < ln:
            n = min(512, ln - c)
            out.append((s, off + c, n, c))
            c += n
    return out


def build(dbg=(), stop=None, ext=()):
    nc = bass.Bass("TRN2", target_bir_lowering=False)
    es = ExitStack()
    P = Prog(nc, es)
    dbg = set(dbg)

    def dten(name, shape, dtype):
        kind = "ExternalOutput" if name in dbg else ("ExternalInput" if name in ext else "Internal")
        return P.dram(name, shape, dtype, kind=kind)

    def inp(name, shape, dtype=F32):
        return P.dram(name, shape, dtype, kind="ExternalInput")

    x_in = inp("x", [T, D])
    w_in = inp("w_in", [D, IN_COLS])
    norm1_bc = inp("norm1_w", [1, D])
    convw = inp("conv_w", [128, 48, 4])
    convb = inp("conv_b", [128, 48])
    qnw = inp("qnw", [1, 128])
    knw = inp("knw", [3, 128])
    ropec = inp("rope_cos", [128, NT, 64])
    ropes = inp("rope_sin", [128, NT, 64])
    ident_in = inp("ident", [128, 128], BF16)
    out_t = P.dram("out", [T, D], F32, kind="ExternalOutput")

    zs = dten("zs", [T, 4096], BF16)
    xs = dten("xs", [T, 4096], BF16)
    BTs = dten("BTs", [1024, T], BF16)
    CTs = dten("CTs", [1024, T], BF16)
    Btok = dten("Btok", [T, 1024], BF16)
    qT = dten("qT", [2048, T], BF16)
    kTc = dten("kTc", [512, T], BF16)
    kTs = dten("kTs", [512, T], BF16)
    kTw = dten("kTw", [512, T], BF16)
    vcT = dten("vcT", [512, T], BF16)
    vstok = dten("vstok", [T, 512], BF16)
    vwtok = dten("vwtok", [T, 512], BF16)
    gsT = dten("gsT", [2048, T], BF16)
    gnT = dten("gnT", [2048, T], BF16)
    dtraw_d = dten("dtraw", [T, 64], F32)

    ident = P.sb("ident", [128, 128], BF16)
    P.dma("sp", ident[:, :], ident_in[:, :], w=[ident])
    gT = P.sb("gT", [48, T], BF16)
    junk = P.sb("junk", [128, D], BF16)
    ss_r = Ring([P.sb("ss%d" % i, [128, 1], F32) for i in range(2)])
    hn_r = Ring([P.sb("hn%d" % i, [128, D], BF16) for i in range(2)])
    stg_r = Ring([P.sb("stg%d" % i, [128, 512], BF16) for i in range(3)])
    f32a = P.sb("f32a", [128, 512], F32)
    f32b = P.sb("f32b", [128, 512], F32)
    P.push()
    wb1 = P.sb("wb1", [128, D], F32)
    P.dma("sp", wb1[:, :], norm1_bc[0:1, :].partition_broadcast(128), w=[wb1])
    hnT = P.sb("hnT", [128, 16, T], BF16)
    cw = P.sb("cw", [128, 48, 4], F32)
    P.dma("sp", cw[:, :, :], convw[:, :, :], w=[cw])
    cbias = P.sb("cb", [128, 48], F32)
    P.dma("sp", cbias[:, :], convb[:, :], w=[cbias])
    rc = P.sb("rc", [128, NT, 64], F32)
    rs_ = P.sb("rs", [128, NT, 64], F32)
    P.dma("sp", rc[:, :, :], ropec[:, :, :], w=[rc])
    P.dma("sp", rs_[:, :, :], ropes[:, :, :], w=[rs_])
    nwq = P.sb("nwq", [128, 4, 128], F32)
    nwk = [P.sb("nwk%d" % i, [128, 4, 128], F32) for i in range(3)]
    for hh in range(4):
        P.dma("sp", nwq[:, hh, :], qnw[0:1, :].partition_broadcast(128), w=[nwq])
        for i in range(3):
            P.dma("sp", nwk[i][:, hh, :], knw[i:i + 1, :].partition_broadcast(128), w=[nwk[i]])

    psf = Ring([P.ps("psf%d" % i, [128, 512], F32) for i in range(6)])
    psb = Ring([P.ps("psb%d" % i, [128, 1024], BF16) for i in range(2)])

    xt_r = Ring([P.sb("xt%d" % i, [128, D], F32) for i in range(2)])

    def rmsnorm_tile(src_tl, wb_tl, dst_tl, width=D, woff=0):
        ss = ss_r.next()
        P.op("dve", lambda e, ss=ss: e.memset(ss[:, 0:1], 0.0), w=[ss])
        P.op("act", lambda e, ss=ss: e.activation(out=junk[:, :width], in_=src_tl[:, :width], func=AF.Square,
                                                   accum_out=ss[:, 0:1]), r=[src_tl], w=[junk, ss])
        P.op("dve", lambda e, ss=ss: e.tensor_scalar(out=ss[:, 0:1], in0=ss[:, 0:1], scalar1=1.0 / width,
                                                      scalar2=EPS, op0=ALU.mult, op1=ALU.add), r=[ss], w=[ss])
        P.op("act", lambda e, ss=ss: e.activation(out=ss[:, 0:1], in_=ss[:, 0:1], func=AF.Sqrt), r=[ss], w=[ss])
        P.op("dve", lambda e, ss=ss: e.reciprocal(out=ss[:, 0:1], in_=ss[:, 0:1]), r=[ss], w=[ss])
        P.op("dve", lambda e, ss=ss: e.scalar_tensor_tensor(out=dst_tl[:, :width], in0=src_tl[:, :width],
                                                             scalar=ss[:, 0:1], in1=wb_tl[:, woff:woff + width],
                                                             op0=ALU.mult, op1=ALU.mult),
             r=[src_tl, ss, wb_tl], w=[dst_tl], selfsync=True)

    def transpose_into(src_tl, nblk, dst_fn, dst_tl):
        for half in range((nblk + 7) // 8):
            pb = psb.next()
            nb = min(8, nblk - half * 8)
            for j in range(nb):
                kc = half * 8 + j
                P.op("pe", lambda e, pb=pb, j=j, kc=kc: e.transpose(out=pb[:, j * 128:(j + 1) * 128],
                                                                     in_=src_tl[:, kc * 128:(kc + 1) * 128],
                                                                     identity=ident[:, :]),
                     r=[src_tl, ident], w=[pb])
            P.op("act", lambda e, pb=pb, half=half, nb=nb: e.copy(
                out=dst_fn(half, nb), in_=pb[:, :nb * 128].rearrange("p (k t) -> p k t", t=128)),
                r=[pb], w=[dst_tl])

    for i in range(NT):
        xt = xt_r.next()
        hn = hn_r.next()
        P.dma("sp", xt[:, :], x_in[i * 128:(i + 1) * 128, :], w=[xt])
        rmsnorm_tile(xt, wb1, hn)
        transpose_into(hn, 16, lambda half, nb, i=i: hnT[:, half * 8:half * 8 + nb, i * 128:(i + 1) * 128], hnT)

    wt_r = Ring([P.sb("wt%d" % i, [128, 16, 512], BF16) for i in range(3)])
    rstd4 = P.sb("rstd4", [128, 4], F32)
    xpad = P.sb("xpad", [128, 3 + T], F32)
    acc = P.sb("acc", [128, T], F32)
    xc = P.sb("xc", [128, T], BF16)
    tstg = P.sb("tstg", [128, 16, 128], BF16)
    P.op("dve", lambda e: e.memset(xpad[:, 0:3], 0.0), w=[xpad])
    w_view = w_in.t.ap().rearrange("(kc p) n -> p kc n", p=128)

    def mm_tok(wt, n, i, pt):
        for kc in range(16):
            P.op("pe", lambda e, kc=kc: e.matmul(pt[:, :n], lhsT=hnT[:, kc, i * 128:(i + 1) * 128],
                                                 rhs=wt[:, kc, :n], start=(kc == 0), stop=(kc == 15)),
                 r=[hnT, wt], w=[pt])

    def mm_feat(wt, j, m, tb, pt):
        for kc in range(16):
            P.op("pe", lambda e, kc=kc: e.matmul(pt[:m, :], lhsT=wt[:, kc, j * 128:j * 128 + m],
                                                 rhs=hnT[:, kc, tb * 512:(tb + 1) * 512],
                                                 start=(kc == 0), stop=(kc == 15)),
                 r=[hnT, wt], w=[pt])

    def qk_epilogue(pt, i, nw_tl, dstT, c0r):
        P.op("dve", lambda e: e.memset(rstd4[:, :], 0.0), w=[rstd4])
        for hh in range(4):
            P.op("act", lambda e, hh=hh: e.activation(out=f32a[:, hh * 128:(hh + 1) * 128],
                                                      in_=pt[:, hh * 128:(hh + 1) * 128], func=AF.Square,
                                                      accum_out=rstd4[:, hh:hh + 1]), r=[pt], w=[f32a, rstd4])
        P.op("dve", lambda e: e.tensor_scalar(out=rstd4[:, :], in0=rstd4[:, :], scalar1=1.0 / 128, scalar2=EPS,
                                              op0=ALU.mult, op1=ALU.add), r=[rstd4], w=[rstd4])
        P.op("act", lambda e: e.activation(out=rstd4[:, :], in_=rstd4[:, :], func=AF.Sqrt), r=[rstd4], w=[rstd4])
        P.op("dve", lambda e: e.reciprocal(out=rstd4[:, :], in_=rstd4[:, :]), r=[rstd4], w=[rstd4])
        P.op("dve", lambda e: e.tensor_tensor(out=f32a[:, :].rearrange("p (h d) -> p h d", d=128),
                                              in0=pt[:, :].rearrange("p (h d) -> p h d", d=128),
                                              in1=rstd4[:, :].unsqueeze(2).to_broadcast([128, 4, 128]),
                                              op=ALU.mult), r=[pt, rstd4], w=[f32a], selfsync=True)
        P.op("dve", lambda e: e.tensor_tensor(out=f32a[:, :], in0=f32a[:, :],
                                              in1=nw_tl[:, :, :].rearrange("p h d -> p (h d)"),
                                              op=ALU.mult), r=[f32a, nw_tl], w=[f32a])
        v = f32a[:, :].rearrange("p (h t d) -> p h t d", h=4, t=2)
        o = f32b[:, :].rearrange("p (h t d) -> p h t d", h=4, t=2)
        cosb = rc[:, i, :].unsqueeze(1).to_broadcast([128, 4, 64])
        sinb = rs_[:, i, :].unsqueeze(1).to_broadcast([128, 4, 64])
        hnq = hn_r.next()
        hv = hnq[:, 0:512].rearrange("p (h t d) -> p h t d", h=4, t=2)
        P.op("dve", lambda e: e.tensor_tensor(out=o[:, :, 0, :], in0=v[:, :, 0, :], in1=cosb, op=ALU.mult),
             r=[f32a, rc], w=[f32b])
        P.op("dve", lambda e: e.tensor_tensor(out=o[:, :, 1, :], in0=v[:, :, 1, :], in1=cosb, op=ALU.mult),
             r=[f32a, rc], w=[f32b])
        P.op("dve", lambda e: e.tensor_tensor(out=v[:, :, 0, :], in0=v[:, :, 0, :], in1=sinb, op=ALU.mult),
             r=[f32a, rs_, f32b], w=[f32a])
        P.op("dve", lambda e: e.tensor_tensor(out=v[:, :, 1, :], in0=v[:, :, 1, :], in1=sinb, op=ALU.mult),
             r=[f32a, rs_], w=[f32a])
        P.op("dve", lambda e: e.tensor_tensor(out=hv[:, :, 0, :], in0=o[:, :, 0, :], in1=v[:, :, 1, :],
                                              op=ALU.subtract), r=[f32a, f32b], w=[hnq])
        P.op("dve", lambda e: e.tensor_tensor(out=hv[:, :, 1, :], in0=o[:, :, 1, :], in1=v[:, :, 0, :],
                                              op=ALU.add), r=[f32a, f32b], w=[hnq])
        st = stg_r.next()
        transpose_into(hnq, 4, lambda half, nb: st[:, :].rearrange("p (k t) -> p k t", t=128), st)
        P.dma("sp", dstT.t.ap()[c0r:c0r + 512, i * 128:(i + 1) * 128].rearrange("(h d) t -> d h t", d=128),
              st[:, :].rearrange("p (k t) -> p k t", t=128), r=[st], w=[dstT])

    blocks = seg_blocks()
    for (seg, c0, n, c0r) in blocks:
        wt = wt_r.next()
        P.dma("pool", wt[:, :, :n], w_view[:, :, c0:c0 + n], w=[wt])
        if seg in ("z", "vs", "vw", "dt", "q", "kc", "ks", "kw"):
            for i in range(NT):
                pt = psf.next()
                mm_tok(wt, n, i, pt)
                if seg == "z":
                    st = stg_r.next()
                    P.op("act", lambda e, pt=pt, st=st: e.activation(out=st[:, :], in_=pt[:, :], func=AF.Silu),
                         r=[pt], w=[st])
                    P.dma("sp", zs[i * 128:(i + 1) * 128, c0r:c0r + 512], st[:, :], r=[st], w=[zs])
                elif seg in ("vs", "vw"):
                    st = stg_r.next()
                    dst = vstok if seg == "vs" else vwtok
                    P.op("act", lambda e, pt=pt, st=st: e.copy(out=st[:, :], in_=pt[:, :]), r=[pt], w=[st])
                    P.dma("sp", dst[i * 128:(i + 1) * 128, :], st[:, :], r=[st], w=[dst])
                elif seg == "dt":
                    P.op("act", lambda e, pt=pt: e.copy(out=f32a[:, :64], in_=pt[:, :64]), r=[pt], w=[f32a])
                    P.dma("sp", dtraw_d[i * 128:(i + 1) * 128, :], f32a[:, :64], r=[f32a], w=[dtraw_d])
                elif seg == "q":
                    qk_epilogue(pt, i, nwq, qT, c0r)
                else:
                    bi = ("kc", "ks", "kw").index(seg)
                    qk_epilogue(pt, i, nwk[bi], (kTc, kTs, kTw)[bi], 0)
        else:
            nj = (n + 127) // 128
            for j in range(nj):
                m = min(128, n - j * 128)
                if seg == "xbc":
                    ct = (c0r + j * 128) // 128
                    for tb in range(4):
                        pt = psf.next()
                        mm_feat(wt, j, m, tb, pt)
                        P.op("act", lambda e, pt=pt, tb=tb: e.copy(out=xpad[:, 3 + tb * 512:3 + (tb + 1) * 512],
                                                                   in_=pt[:, :]), r=[pt], w=[xpad])
                    P.op("dve", lambda e, ct=ct: e.tensor_scalar(out=acc[:, :], in0=xpad[:, 0:T],
                                                                 scalar1=cw[:, ct, 0:1], scalar2=None,
                                                                 op0=ALU.mult), r=[xpad, cw], w=[acc])
                    for k in range(1, 4):
                        P.op("dve", lambda e, ct=ct, k=k: e.scalar_tensor_tensor(
                            out=acc[:, :], in0=xpad[:, k:k + T], scalar=cw[:, ct, k:k + 1], in1=acc[:, :],
                            op0=ALU.mult, op1=ALU.add), r=[xpad, cw, acc], w=[acc])
                    P.op("act", lambda e, ct=ct: e.activation(out=xc[:, :], in_=acc[:, :], func=AF.Silu,
                                                              bias=cbias[:, ct:ct + 1]), r=[acc, cbias], w=[xc])
                    if ct < 32 or ct < 40:
                        transpose_into(xc, 16, lambda half, nb: tstg[:, half * 8:half * 8 + nb, :], tstg)
                        if ct < 32:
                            dst_ap = xs.t.ap()[:, ct * 128:(ct + 1) * 128].rearrange("(i p) c -> p i c", p=128)
                            P.dma("sp", dst_ap, tstg[:, :, :], r=[tstg], w=[xs])
                        else:
                            cc = ct - 32
                            dst_ap = Btok.t.ap()[:, cc * 128:(cc + 1) * 128].rearrange("(i p) c -> p i c", p=128)
                            P.dma("sp", dst_ap, tstg[:, :, :], r=[tstg], w=[Btok])
                    if 32 <= ct < 40:
                        P.dma("sp", BTs[(ct - 32) * 128:(ct - 31) * 128, :], xc[:, :], r=[xc], w=[BTs])
                    elif ct >= 40:
                        P.dma("sp", CTs[(ct - 40) * 128:(ct - 39) * 128, :], xc[:, :], r=[xc], w=[CTs])
                else:
                    for tb in range(4):
                        pt = psf.next()
                        mm_feat(wt, j, m, tb, pt)
                        if seg == "nsag":
                            P.op("act", lambda e, pt=pt, tb=tb, m=m: e.activation(
                                out=gT[:m, tb * 512:(tb + 1) * 512], in_=pt[:m, :], func=AF.Sigmoid),
                                r=[pt], w=[gT])
                        else:
                            st = stg_r.next()
                            if seg == "vc":
                                P.op("act", lambda e, pt=pt, st=st: e.copy(out=st[:, :], in_=pt[:, :]),
                                     r=[pt], w=[st])
                                dst = vcT
                            else:
                                P.op("act", lambda e, pt=pt, st=st: e.activation(out=st[:, :], in_=pt[:, :],
                                                                                 func=AF.Sigmoid),
                                     r=[pt], w=[st])
                                dst = gsT if seg == "gssd" else gnT
                            r0 = c0r + j * 128
                            P.dma("sp", dst[r0:r0 + 128, tb * 512:(tb + 1) * 512], st[:, :], r=[st], w=[dst])

    if stop == "B":
        P.dma("sp", out_t[0:128, 0:512], f32a[:, :], r=[f32a], w=[out_t])
        P.emit()
        return nc, es

    P.pop()
    P.push()
    identf_in = inp("identf", [128, 128])
    triu_in = inp("triu", [128, 128])
    dtb_in = inp("dt_bias", [1, 64])
    alog_in = inp("a_log", [1, 64])
    dsk_in = inp("d_skip", [1, 64])
    snw_in = inp("ssd_norm_w", [1, 4096])
    acumT_d = dten("acumT_d", [64, T], F32)
    yT = dten("yT", [4096, T], BF16)

    identf = P.sb("identf", [128, 128], F32)
    triu = P.sb("triu", [128, 128], F32)
    onesf = P.sb("onesf", [128, 128], F32)
    P.load("sp", identf, identf[:, :], identf_in[:, :])
    P.load("sp", triu, triu[:, :], triu_in[:, :])
    P.op("dve", lambda e: e.memset(onesf[:, :], 1.0), w=[onesf])
    dtb = P.sb("dtb", [128, 64], F32)
    aneg = P.sb("aneg", [128, 64], F32)
    dskb = P.sb("dskb", [128, 64], F32)
    snw = P.sb("snw", [128, 4096], F32)
    P.load("sp", dtb, dtb[:, :], dtb_in[0:1, :].partition_broadcast(128))
    P.load("sp", aneg, aneg[:, :], alog_in[0:1, :].partition_broadcast(128))
    P.load("sp", dskb, dskb[:, :], dsk_in[0:1, :].partition_broadcast(128))
    P.load("sp", snw, snw[:, :], snw_in[0:1, :].partition_broadcast(128))
    P.op("act", lambda e: e.activation(out=aneg[:, :], in_=aneg[:, :], func=AF.Exp), r=[aneg], w=[aneg])
    P.op("dve", lambda e: e.tensor_scalar(out=aneg[:, :], in0=aneg[:, :], scalar1=-1.0, scalar2=None,
                                          op0=ALU.mult), r=[aneg], w=[aneg])
    dt_sb = P.sb("dt_sb", [128, NT, 64], F32)
    da_sb = P.sb("da_sb", [128, NT, 64], F32)
    acum = P.sb("acum", [128, NT, 64], F32)
    nacum = P.sb("nacum", [128, NT, 64], F32)
    eacum = P.sb("eacum", [128, NT, 64], F32)
    dte = P.sb("dte", [128, NT, 64], F32)
    cdec = P.sb("cdec", [128, 8, 64], F32)
    P.load("sp", dt_sb, dt_sb[:, :, :], dtraw_d.t.ap().rearrange("(i p) h -> p i h", p=128), src=dtraw_d)
    P.op("dve", lambda e: e.tensor_tensor(out=dt_sb[:, :, :], in0=dt_sb[:, :, :],
                                          in1=dtb[:, :].unsqueeze(1).to_broadcast([128, NT, 64]), op=ALU.add),
         r=[dt_sb, dtb], w=[dt_sb])
    P.op("act", lambda e: e.activation(out=dt_sb[:, :, :], in_=dt_sb[:, :, :], func=AF.Exp), r=[dt_sb], w=[dt_sb])
    P.op("dve", lambda e: e.tensor_scalar(out=dt_sb[:, :, :], in0=dt_sb[:, :, :], scalar1=1.0, scalar2=None,
                                          op0=ALU.add), r=[dt_sb], w=[dt_sb])
    P.op("act", lambda e: e.activation(out=dt_sb[:, :, :], in_=dt_sb[:, :, :], func=AF.Ln), r=[dt_sb], w=[dt_sb])
    P.op("dve", lambda e: e.tensor_tensor(out=da_sb[:, :, :], in0=dt_sb[:, :, :],
                                          in1=aneg[:, :].unsqueeze(1).to_broadcast([128, NT, 64]), op=ALU.mult),
         r=[dt_sb, aneg], w=[da_sb])
    for c in range(8):
        i0, i1 = 2 * c, 2 * c + 1
        p0 = psf.next()
        P.op("pe", lambda e, p0=p0, i0=i0: e.matmul(p0[:, :64], lhsT=triu[:, :], rhs=da_sb[:, i0, :],
                                                    start=True, stop=True), r=[triu, da_sb], w=[p0])
        P.op("act", lambda e, p0=p0, i0=i0: e.copy(out=acum[:, i0, :], in_=p0[:, :64]), r=[p0], w=[acum])
        p1 = psf.next()
        P.op("pe", lambda e, p1=p1, i0=i0: e.matmul(p1[:, :64], lhsT=onesf[:, :], rhs=da_sb[:, i0, :],
                                                    start=True, stop=False), r=[onesf, da_sb], w=[p1])
        P.op("pe", lambda e, p1=p1, i1=i1: e.matmul(p1[:, :64], lhsT=triu[:, :], rhs=da_sb[:, i1, :],
                                                    start=False, stop=True), r=[triu, da_sb], w=[p1])
        P.op("act", lambda e, p1=p1, i1=i1: e.copy(out=acum[:, i1, :], in_=p1[:, :64]), r=[p1], w=[acum])
        p2 = psf.next()
        P.op("pe", lambda e, p2=p2, i0=i0: e.matmul(p2[:, :64], lhsT=onesf[:, :], rhs=da_sb[:, i0, :],
                                                    start=True, stop=False), r=[onesf, da_sb], w=[p2])
        P.op("pe", lambda e, p2=p2, i1=i1: e.matmul(p2[:, :64], lhsT=onesf[:, :], rhs=da_sb[:, i1, :],
                                                    start=False, stop=True), r=[onesf, da_sb], w=[p2])
        for ii in (i0, i1):
            P.op("dve", lambda e, p2=p2, ii=ii: e.tensor_tensor(out=dte[:, ii, :], in0=p2[:, :64],
                                                                in1=acum[:, ii, :], op=ALU.subtract),
                 r=[p2, acum], w=[dte])
        P.op("act", lambda e, p2=p2, c=c: e.activation(out=cdec[:, c, :], in_=p2[:, :64], func=AF.Exp),
             r=[p2], w=[cdec])
    P.op("act", lambda e: e.activation(out=dte[:, :, :], in_=dte[:, :, :], func=AF.Exp), r=[dte], w=[dte])
    P.op("dve", lambda e: e.tensor_tensor(out=dte[:, :, :], in0=dte[:, :, :], in1=dt_sb[:, :, :], op=ALU.mult),
         r=[dte, dt_sb], w=[dte])
    P.op("act", lambda e: e.activation(out=eacum[:, :, :], in_=acum[:, :, :], func=AF.Exp), r=[acum], w=[eacum])
    P.op("dve", lambda e: e.tensor_scalar(out=nacum[:, :, :], in0=acum[:, :, :], scalar1=-1.0, scalar2=None,
                                          op0=ALU.mult), r=[acum], w=[nacum])
    acT = P.sb("acT", [64, T], F32)
    for i in range(NT):
        pt = psf.next()
        P.op("pe", lambda e, pt=pt, i=i: e.transpose(out=pt[:64, :128], in_=acum[:, i, :], identity=identf[:, :]),
             r=[acum, identf], w=[pt])
        P.op("act", lambda e, pt=pt, i=i: e.copy(out=acT[:, i * 128:(i + 1) * 128], in_=pt[:64, :128]),
             r=[pt], w=[acT])
    P.store("sp", acumT_d, acumT_d[:, :], acT, acT[:, :])
    P.barrier()

    BTc = P.sb("BTc", [128, 8, 256], BF16)
    CTc = P.sb("CTc", [128, 8, 256], BF16)
    Btc = P.sb("Btc", [128, 2, 1024], BF16)
    xsc = P.sb("xsc", [128, 2, 4096], BF16)
    zsc = P.sb("zsc", [128, 2, 4096], BF16)
    xsd = P.sb("xsd", [128, 2, 4096], BF16)
    Hs = P.sb("Hs", [128, 8, 512], F32)
    Hb = P.sb("Hb", [128, 8, 512], BF16)
    P.op("dve", lambda e: e.memset(Hs[:, :, :], 0.0), w=[Hs])
    P.op("dve", lambda e: e.memset(Hb[:, :, :], 0.0), w=[Hb])
    bc_r = Ring([P.sb("bc%d" % i, [128, 8, 256], F32) for i in range(2)])
    cbTm = P.sb("cbTm", [128, 2, 256], F32)
    dif_r = Ring([P.sb("dif%d" % i, [128, 256], F32) for i in range(2)])
    MT_r = Ring([P.sb("MT%d" % i, [128, 2, 256], BF16) for i in range(3)])
    yA = P.sb("yA", [128, 512], F32)
    yB = P.sb("yB", [128, 512], F32)
    ynb = P.sb("ynb", [128, 512], BF16)
    BT_v = BTs.t.ap().rearrange("(g n) t -> n g t", n=128)
    CT_v = CTs.t.ap().rearrange("(g n) t -> n g t", n=128)
    for c in range(8):
        t0 = c * 256
        P.load("sp", BTc, BTc[:, :, :], BT_v[:, :, t0:t0 + 256], src=BTs)
        P.load("sp", CTc, CTc[:, :, :], CT_v[:, :, t0:t0 + 256], src=CTs)
        P.load("sp", Btc, Btc[:, :, :], Btok.t.ap()[t0:t0 + 256, :].rearrange("(i p) n -> p i n", p=128), src=Btok)
        P.load("sp", xsc, xsc[:, :, :], xs.t.ap()[t0:t0 + 256, :].rearrange("(i p) n -> p i n", p=128), src=xs)
        P.load("sp", zsc, zsc[:, :, :], zs.t.ap()[t0:t0 + 256, :].rearrange("(i p) n -> p i n", p=128), src=zs)
        for lt in range(2):
            ii = 2 * c + lt
            P.op("dve", lambda e, lt=lt, ii=ii: e.tensor_tensor(
                out=xsd[:, lt, :].rearrange("p (h d) -> p h d", d=64),
                in0=xsc[:, lt, :].rearrange("p (h d) -> p h d", d=64),
                in1=dte[:, ii, :].unsqueeze(2).to_broadcast([128, 64, 64]), op=ALU.mult),
                r=[xsc, dte], w=[xsd])
        for g in range(8):
            bc = bc_r.next()
            P.load("pool", bc, bc[:, :, :],
                   acumT_d.t.ap()[g * 8:(g + 1) * 8, t0:t0 + 256].partition_broadcast(128),
                   src=acumT_d)
            for st in range(2):
                pc = psf.next()
                P.op("pe", lambda e, pc=pc, st=st, g=g: e.matmul(pc[:, :256], lhsT=BTc[:, g, st * 128:(st + 1) * 128],
                                                                 rhs=CTc[:, g, :], start=True, stop=True),
                     r=[BTc, CTc], w=[pc])
                if st == 0:
                    P.op("dve", lambda e, pc=pc: e.tensor_tensor(out=cbTm[:, 0, 0:128], in0=pc[:, 0:128],
                                                                 in1=triu[:, :], op=ALU.mult),
                         r=[pc, triu], w=[cbTm])
                    P.op("act", lambda e, pc=pc: e.copy(out=cbTm[:, 0, 128:256], in_=pc[:, 128:256]),
                         r=[pc], w=[cbTm])
                else:
                    P.op("dve", lambda e, pc=pc: e.tensor_tensor(out=cbTm[:, 1, 128:256], in0=pc[:, 128:256],
                                                                 in1=triu[:, :], op=ALU.mult),
                         r=[pc, triu], w=[cbTm])
            pst = psf.next()
            for lt in range(2):
                P.op("pe", lambda e, lt=lt, g=g, pst=pst: e.matmul(pst[:, :], lhsT=Btc[:, lt, g * 128:(g + 1) * 128],
                                                                   rhs=xsd[:, lt, g * 512:(g + 1) * 512],
                                                                   start=(lt == 0), stop=(lt == 1)),
                     r=[Btc, xsd], w=[pst])
            poff = [psf.next(), psf.next()]
            for lt in range(2):
                P.op("pe", lambda e, lt=lt, g=g, po=poff[lt]: e.matmul(po[:, :], lhsT=CTc[:, g, lt * 128:(lt + 1) * 128],
                                                                       rhs=Hb[:, g, :], start=True, stop=True),
                     r=[CTc, Hb], w=[poff[lt]])
            P.op("dve", lambda e, g=g, c=c: e.tensor_tensor(
                out=Hs[:, g, :].rearrange("p (h d) -> p h d", d=64),
                in0=Hs[:, g, :].rearrange("p (h d) -> p h d", d=64),
                in1=cdec[:, c, g * 8:(g + 1) * 8].unsqueeze(2).to_broadcast([128, 8, 64]), op=ALU.mult),
                r=[Hs, cdec], w=[Hs])
            P.op("dve", lambda e, g=g, pst=pst: e.tensor_tensor(out=Hs[:, g, :], in0=Hs[:, g, :], in1=pst[:, :],
                                                                op=ALU.add), r=[Hs, pst], w=[Hs])
            P.op("act", lambda e, g=g: e.copy(out=Hb[:, g, :], in_=Hs[:, g, :]), r=[Hs, poff[0], poff[1]], w=[Hb])
            yoff = [yA, yB]
            for lt in range(2):
                ii = 2 * c + lt
                P.op("dve", lambda e, lt=lt, ii=ii, g=g, po=poff[lt]: e.tensor_tensor(
                    out=yoff[lt][:, :].rearrange("p (h d) -> p h d", d=64),
                    in0=po[:, :].rearrange("p (h d) -> p h d", d=64),
                    in1=eacum[:, ii, g * 8:(g + 1) * 8].unsqueeze(2).to_broadcast([128, 8, 64]), op=ALU.mult),
                    r=[poff[lt], eacum], w=[yoff[lt]])
            pd = [psf.next(), psf.next()]
            for hh in range(8):
                h = g * 8 + hh
                MT = MT_r.next()
                for st in range(2):
                    ii = 2 * c + st
                    l0 = 0 if st == 0 else 128
                    dif = dif_r.next()
                    P.op("dve", lambda e, dif=dif, bc=bc, hh=hh, ii=ii, h=h, l0=l0: e.tensor_scalar(
                        out=dif[:, l0:256], in0=bc[:, hh, l0:256], scalar1=nacum[:, ii, h:h + 1], scalar2=0.0,
                        op0=ALU.add, op1=ALU.min), r=[bc, nacum], w=[dif])
                    P.op("act", lambda e, dif=dif, l0=l0: e.activation(out=dif[:, l0:256], in_=dif[:, l0:256],
                                                                       func=AF.Exp), r=[dif], w=[dif])
                    P.op("dve", lambda e, dif=dif, MT=MT, st=st, ii=ii, h=h, l0=l0: e.scalar_tensor_tensor(
                        out=MT[:, st, l0:256], in0=dif[:, l0:256], scalar=dt_sb[:, ii, h:h + 1],
                        in1=cbTm[:, st, l0:256], op0=ALU.mult, op1=ALU.mult),
                        r=[dif, dt_sb, cbTm], w=[MT])
                for lt in range(2):
                    for st in range(lt + 1):
                        P.op("pe", lambda e, lt=lt, st=st, MT=MT, hh=hh, h=h, pdl=pd[lt]: e.matmul(
                            pdl[:, hh * 64:(hh + 1) * 64], lhsT=MT[:, st, lt * 128:(lt + 1) * 128],
                            rhs=xsc[:, st, h * 64:(h + 1) * 64], start=(st == 0), stop=(st == lt)),
                            r=[MT, xsc], w=[pd[lt]])
            for lt in range(2):
                ii = 2 * c + lt
                yt = yoff[lt]
                P.op("dve", lambda e, yt=yt, lt=lt, pdl=pd[lt]: e.tensor_tensor(out=yt[:, :], in0=yt[:, :], in1=pdl[:, :],
                                                                    op=ALU.add), r=[yt, pd[lt]], w=[yt])
                P.op("dve", lambda e, lt=lt, g=g: e.tensor_tensor(
                    out=f32a[:, :].rearrange("p (h d) -> p h d", d=64),
                    in0=xsc[:, lt, g * 512:(g + 1) * 512].rearrange("p (h d) -> p h d", d=64),
                    in1=dskb[:, g * 8:(g + 1) * 8].unsqueeze(2).to_broadcast([128, 8, 64]), op=ALU.mult),
                    r=[xsc, dskb], w=[f32a])
                P.op("dve", lambda e, yt=yt: e.tensor_tensor(out=yt[:, :], in0=yt[:, :], in1=f32a[:, :], op=ALU.add),
                     r=[yt, f32a], w=[yt])
                P.op("dve", lambda e, yt=yt, lt=lt, g=g: e.tensor_tensor(out=yt[:, :], in0=yt[:, :],
                                                                         in1=zsc[:, lt, g * 512:(g + 1) * 512],
                                                                         op=ALU.mult), r=[yt, zsc], w=[yt])
                rmsnorm_tile(yt, snw, ynb, width=512, woff=g * 512)
                st_ = stg_r.next()
                transpose_into(ynb, 4, lambda half, nb, st_=st_: st_[:, :].rearrange("p (k t) -> p k t", t=128), st_)
                P.store("sp", yT, yT.t.ap()[g * 512:(g + 1) * 512, ii * 128:(ii + 1) * 128].rearrange(
                    "(k d) t -> d k t", d=128), st_, st_[:, :].rearrange("p (k t) -> p k t", t=128))
    P.pop()
    if stop == "C":
        P.dma("sp", out_t[0:128, 0:512], f32a[:, :], r=[f32a], w=[out_t])
        P.emit()
        return nc, es

    SCALE = float(128 ** -0.5)
    ynT = dten("ynT", [2048, T], BF16)
    if "dbg_sel" in dbg:
        dbg_sel = dten("dbg_sel", [4, 16, 128, 32], BF16)
        dbg_imp = dten("dbg_imp", [4, 16, 128, 32], F32)
    if "ynT" not in ext:
        pek_in = inp("cmp_pe_kT", [128, 32])
        pev_in = inp("cmp_pe_vT", [128, 32])
        w1k_in = inp("cmp_w1_k", [32, 128, 256])
        w1v_in = inp("cmp_w1_v", [32, 128, 256])
        w2k_in = inp("cmp_w2_k", [256, 128])
        w2v_in = inp("cmp_w2_v", [256, 128])
        cmask_in = inp("cmask", [128, T], BF16)
        wmask_in = inp("wmask", [128, 8, 512], BF16)
        E_in = inp("Esel", [32, 16, 128], BF16)
        sel48_in = inp("sel48", [48, 48, 128], BF16)
        ovl_in = inp("ovl", [128, 32])
        keep_in = inp("keepc", [128, 16, 32])
        addc_in = inp("addc", [128, 16, 32])
        P.push()
        kcT_sb = P.sb("kcT_sb", [128, 4, 127], BF16)
        vc_sb = P.sb("vc_sb", [128, 4, 128], BF16)
        P.push()
        raw_sb = P.sb("raw_sb", [128, 4, T], BF16)
        w1_sb = P.sb("w1_sb", [128, 32, 256], BF16)
        w2_sb = P.sb("w2_sb", [128, 2, 128], BF16)
        peT = P.sb("peT", [128, 32], F32)
        blk_all = P.sb("blk_all", [128, 32, 4, 127], BF16)
        hidT = P.sb("hidT", [128, 2, 508], BF16)
        for which in range(2):
            srcT = kTc if which == 0 else vcT
            P.load("sp", raw_sb, raw_sb[:, :, :], srcT.t.ap().rearrange("(g d) t -> d g t", d=128), src=srcT)
            P.load("pool", w1_sb, w1_sb[:, :, :], (w1k_in if which == 0 else w1v_in).t.ap().rearrange("j d f -> d j f"))
            P.load("pool", w2_sb, w2_sb[:, :, :], (w2k_in if which == 0 else w2v_in).t.ap().rearrange("(c f) d -> f c d", f=128))
            P.load("sp", peT, peT[:, :], (pek_in if which == 0 else pev_in)[:, :])
            pk = [psf.tiles[0], psf.tiles[1]]
            rv = raw_sb[:, :, :].rearrange("p g (c s) -> p g c s", s=16)
            for j in range(32):
                c0, jj = j // 16, j % 16
                P.op("dve", lambda e, c0=c0, jj=jj, j=j: e.tensor_scalar(
                    out=blk_all[:, j, :, :], in0=rv[:, :, c0:c0 + 127, jj], scalar1=peT[:, j:j + 1], scalar2=None,
                    op0=ALU.add), r=[raw_sb, peT], w=[blk_all])
            for fc in range(2):
                for g in range(4):
                    for j in range(32):
                        P.op("pe", lambda e, fc=fc, g=g, j=j: e.matmul(
                            pk[fc][:, g * 127:(g + 1) * 127], lhsT=w1_sb[:, j, fc * 128:(fc + 1) * 128],
                            rhs=blk_all[:, j, g, :], start=(j == 0), stop=(j == 31)), r=[w1_sb, blk_all], w=[pk[fc]])
            for fc in range(2):
                P.op("act", lambda e, fc=fc: e.activation(out=hidT[:, fc, :], in_=pk[fc][:, :508], func=AF.Silu),
                     r=[pk[fc]], w=[hidT])
            po_ = psf.tiles[2]
            if which == 0:
                for g in range(4):
                    for fc in range(2):
                        P.op("pe", lambda e, g=g, fc=fc: e.matmul(po_[:, g * 127:(g + 1) * 127], lhsT=w2_sb[:, fc, :],
                                                                  rhs=hidT[:, fc, g * 127:(g + 1) * 127],
                                                                  start=(fc == 0), stop=(fc == 1)),
                             r=[w2_sb, hidT], w=[po_])
                P.op("act", lambda e: e.copy(out=kcT_sb[:, :, :], in_=po_[:, :508].rearrange("p (g c) -> p g c", c=127)),
                     r=[po_], w=[kcT_sb])
            else:
                for g in range(4):
                    for fc in range(2):
                        P.op("pe", lambda e, g=g, fc=fc: e.matmul(po_[:127, g * 128:(g + 1) * 128],
                                                                  lhsT=hidT[:, fc, g * 127:(g + 1) * 127],
                                                                  rhs=w2_sb[:, fc, :], start=(fc == 0), stop=(fc == 1)),
                             r=[w2_sb, hidT], w=[po_])
                P.op("act", lambda e: e.copy(out=vc_sb[:127, :, :], in_=po_[:127, :].rearrange("p (g d) -> p g d", d=128)),
                     r=[po_], w=[vc_sb])
        P.pop()
        kTs_sb = P.sb("kTs_sb", [128, 4, T], BF16)
        kTw_sb = P.sb("kTw_sb", [128, 4, T], BF16)
        vs_sb = P.sb("vs_sb", [128, NT, 512], BF16)
        vw_sb = P.sb("vw_sb", [128, NT, 512], BF16)
        P.load("sp", kTs_sb, kTs_sb[:, :, :], kTs.t.ap().rearrange("(g d) t -> d g t", d=128), src=kTs)
        P.load("sp", kTw_sb, kTw_sb[:, :, :], kTw.t.ap().rearrange("(g d) t -> d g t", d=128), src=kTw)
        P.load("sp", vs_sb, vs_sb[:, :, :], vstok.t.ap().rearrange("(i p) c -> p i c", p=128), src=vstok)
        P.load("sp", vw_sb, vw_sb[:, :, :], vwtok.t.ap().rearrange("(i p) c -> p i c", p=128), src=vwtok)
        cmask = P.sb("cmask", [128, T], BF16)
        wmask = P.sb("wmask", [128, 8, 512], BF16)
        Esel = P.sb("Esel", [32, 16, 128], BF16)
        sel48 = P.sb("sel48", [48, 48, 128], BF16)
        ovl = P.sb("ovl", [128, 32], F32)
        keepc = P.sb("keepc", [128, 16, 32], F32)
        addc = P.sb("addc", [128, 16, 32], F32)
        P.load("sp", cmask, cmask[:, :], cmask_in[:, :])
        P.load("sp", wmask, wmask[:, :, :], wmask_in[:, :, :])
        P.load("sp", Esel, Esel[:, :, :], E_in[:, :, :])
        P.load("sp", sel48, sel48[:, :, :], sel48_in[:, :, :])
        P.load("sp", ovl, ovl[:, :], ovl_in[:, :])
        P.load("sp", keepc, keepc[:, :, :], keep_in[:, :, :])
        P.load("sp", addc, addc[:, :, :], addc_in[:, :, :])
        onesn = P.sb("onesn", [128, 128], BF16)
        P.op("dve", lambda e: e.memset(onesn[:, :], 1.0), w=[onesn])
        qg_r = Ring([P.sb("qg%d" % i, [128, 4, T], BF16) for i in range(2)])
        pT_r = Ring([P.sb("pTn%d" % i, [128, 512], BF16) for i in range(3)])
        yacc = P.sb("yacc", [128, 4, 512], F32)
        rz = P.sb("rz", [128, 512], F32)
        tmpn = P.sb("tmpn", [128, 512], F32)
        Pn = P.sb("Pn", [128, 512], F32)
        PnS = P.sb("PnS", [128, 512], F32)
        impp = P.sb("impp", [128, 32], F32)
        imp2 = P.sb("imp2", [128, 32], F32)
        m8a = P.sb("m8a", [128, 8], F32)
        m8b = P.sb("m8b", [128, 8], F32)
        selb = P.sb("selb", [128, 128], BF16)
        selbT = P.sb("selbT", [32, 512], BF16)
        ybf_r = Ring([P.sb("ybf%d" % i, [128, 512], BF16) for i in range(2)])
        Sb = [psf.tiles[0], psf.tiles[1]]
        n_po, n_pz, n_pg, n_pimp = psf.tiles[2], psf.tiles[3], psf.tiles[4], psf.tiles[5]
        qT_v = qT.t.ap().rearrange("(g r d) t -> g d r t", r=4, d=128)

        def attn_branch(qg, r, tb, units):
            n = len(units)
            qs = qg[:, r, tb * 512:(tb + 1) * 512]

            def issue_S(idx):
                kp, kl, ktl, biases, vl, vtl = units[idx]
                pS = Sb[idx % 2]
                mms = [(kl, qs, [ktl, qg])] + [(b[0], b[2], [b[1], b[3]]) for b in biases]
                for mi, (l_, r_, tls) in enumerate(mms):
                    P.op("pe", lambda e, l_=l_, r_=r_, pS=pS, mi=mi, last=(mi == len(mms) - 1), kp=kp: e.matmul(
                        pS[:kp, :], lhsT=l_, rhs=r_, start=(mi == 0), stop=last), r=tls, w=[pS])

            issue_S(0)
            pT = None
            for idx in range(n):
                if idx + 1 < n:
                    issue_S(idx + 1)
                kp, kl, ktl, biases, vl, vtl = units[idx]
                pS = Sb[idx % 2]
                pT = pT_r.next()
                P.op("act", lambda e, pS=pS, pT=pT, kp=kp: e.activation(out=pT[:kp, :], in_=pS[:kp, :], func=AF.Exp,
                                                                       scale=SCALE), r=[pS], w=[pT])
                P.op("pe", lambda e, pT=pT, vl=vl, kp=kp, idx=idx: e.matmul(n_po[:, :], lhsT=vl, rhs=pT[:kp, :],
                                                                          start=(idx == 0), stop=(idx == n - 1)),
                     r=[vtl, pT], w=[n_po])
                P.op("pe", lambda e, pT=pT, kp=kp, idx=idx: e.matmul(n_pz[:, :], lhsT=onesn[:kp, :], rhs=pT[:kp, :],
                                                                   start=(idx == 0), stop=(idx == n - 1)),
                     r=[onesn, pT], w=[n_pz])
            return pT

        def combine(r, tb, gate_row, first):
            P.op("pe", lambda e: e.matmul(n_pg[:, :], lhsT=sel48[:, gate_row, :], rhs=gT[:, tb * 512:(tb + 1) * 512],
                                          start=True, stop=True), r=[sel48, gT], w=[n_pg])
            P.op("dve", lambda e: e.tensor_scalar(out=rz[:, :], in0=n_pz[:, :], scalar1=1e-20, scalar2=None, op0=ALU.max),
                 r=[n_pz], w=[rz])
            P.op("dve", lambda e: e.reciprocal(out=rz[:, :], in_=rz[:, :]), r=[rz], w=[rz])
            P.op("dve", lambda e: e.tensor_tensor(out=tmpn[:, :], in0=n_pg[:, :], in1=rz[:, :], op=ALU.mult),
                 r=[n_pg, rz], w=[tmpn])
            if first:
                P.op("dve", lambda e: e.tensor_tensor(out=yacc[:, r, :], in0=n_po[:, :], in1=tmpn[:, :], op=ALU.mult),
                     r=[n_po, tmpn], w=[yacc])
            else:
                P.op("dve", lambda e: e.tensor_tensor(out=tmpn[:, :], in0=n_po[:, :], in1=tmpn[:, :], op=ALU.mult),
                     r=[n_po, tmpn], w=[tmpn])
                P.op("dve", lambda e: e.tensor_tensor(out=yacc[:, r, :], in0=yacc[:, r, :], in1=tmpn[:, :], op=ALU.add),
                     r=[yacc, tmpn], w=[yacc])

        def topk_tile(g, tb, a):
            qt = 4 * tb + a
            P.op("dve", lambda e: e.tensor_tensor(out=impp[:, :], in0=n_pimp[:, a * 32:(a + 1) * 32], in1=keepc[:, qt, :],
                                                  op=ALU.mult), r=[n_pimp, keepc], w=[impp])
            P.op("dve", lambda e: e.tensor_tensor(out=impp[:, :], in0=impp[:, :], in1=addc[:, qt, :], op=ALU.add),
                 r=[impp, addc], w=[impp])
            P.op("dve", lambda e: e.max(out=m8a[:, :], in_=impp[:, :]), r=[impp], w=[m8a], selfsync=True)
            P.op("dve", lambda e: e.match_replace(out=imp2[:, :], in_to_replace=m8a[:, :], in_values=impp[:, :],
                                                  imm_value=-1e30), r=[m8a, impp], w=[imp2], selfsync=True)
            P.op("dve", lambda e: e.max(out=m8b[:, :], in_=imp2[:, :]), r=[imp2], w=[m8b], selfsync=True)
            P.op("dve", lambda e: e.tensor_scalar(out=imp2[:, :], in0=impp[:, :], scalar1=m8b[:, 7:8], scalar2=30000.0,
                                                  op0=ALU.is_ge, op1=ALU.mult), r=[impp, m8b], w=[imp2], selfsync=True)
            P.op("dve", lambda e: e.tensor_scalar(out=selb[:, 0:32], in0=imp2[:, :], scalar1=-30000.0, scalar2=None,
                                                  op0=ALU.add), r=[imp2], w=[selb])
            if "dbg_sel" in dbg:
                P.store("sp", dbg_sel, dbg_sel[g, qt, :, :], selb, selb[:, 0:32])
                P.store("sp", dbg_imp, dbg_imp[g, qt, :, :], impp, impp[:, :])
            pb = psb.next()
            P.op("pe", lambda e, pb=pb: e.transpose(out=pb[:32, 0:128], in_=selb[:, 0:32], identity=ident[:, :]),
                 r=[selb, ident], w=[pb])
            P.op("act", lambda e, pb=pb: e.copy(out=selbT[:, a * 128:(a + 1) * 128], in_=pb[:32, 0:128]),
                 r=[pb], w=[selbT])

        for g in range(4):
            qg = qg_r.next()
            P.load("sp", qg, qg[:, :, :], qT_v[g], src=qT)
            for tb in range(4):
                for r in range(4):
                    head = g * 4 + r
                    units = [(127, kcT_sb[:, g, :], kcT_sb,
                              [(ident[:127, :127], ident, cmask[:127, tb * 512:(tb + 1) * 512], cmask)],
                              vc_sb[:127, g, :], vc_sb)]
                    pT = attn_branch(qg, r, tb, units)
                    combine(r, tb, 0 * 16 + head, True)
                    if tb >= 2:
                        if r == 0:
                            P.op("dve", lambda e, pT=pT: e.tensor_tensor(out=PnS[:127, :], in0=pT[:127, :],
                                                                         in1=rz[:127, :], op=ALU.mult),
                                 r=[pT, rz], w=[PnS])
                        else:
                            P.op("dve", lambda e, pT=pT: e.tensor_tensor(out=Pn[:127, :], in0=pT[:127, :],
                                                                         in1=rz[:127, :], op=ALU.mult),
                                 r=[pT, rz], w=[Pn])
                            P.op("dve", lambda e: e.tensor_tensor(out=PnS[:127, :], in0=PnS[:127, :], in1=Pn[:127, :],
                                                                  op=ALU.add), r=[PnS, Pn], w=[PnS])
                if tb >= 2:
                    for a in range(4):
                        P.op("pe", lambda e, a=a: e.matmul(n_pimp[:, a * 32:(a + 1) * 32],
                                                           lhsT=PnS[:127, a * 128:(a + 1) * 128], rhs=ovl[:127, :],
                                                           start=True, stop=True), r=[PnS, ovl], w=[n_pimp])
                if tb >= 2:
                    for a in range(4):
                        topk_tile(g, tb, a)
                for r in range(4):
                    head = g * 4 + r
                    units = []
                    for kt in range(4 * tb + 4):
                        biases = []
                        if tb >= 2:
                            biases.append((Esel[:, kt, :], Esel, selbT[:, :], selbT))
                        if kt >= 4 * tb:
                            biases.append((ident[:, :], ident, wmask[:, 4 + kt - 4 * tb, :], wmask))
                        units.append((128, kTs_sb[:, g, kt * 128:(kt + 1) * 128], kTs_sb, biases,
                                      vs_sb[:, kt, g * 128:(g + 1) * 128], vs_sb))
                    attn_branch(qg, r, tb, units)
                    combine(r, tb, 1 * 16 + head, False)
                    units = []
                    for kt in range(max(0, 4 * tb - 4), 4 * tb + 4):
                        biases = [(ident[:, :], ident, wmask[:, 4 + kt - 4 * tb, :], wmask)]
                        units.append((128, kTw_sb[:, g, kt * 128:(kt + 1) * 128], kTw_sb, biases,
                                      vw_sb[:, kt, g * 128:(g + 1) * 128], vw_sb))
                    attn_branch(qg, r, tb, units)
                    combine(r, tb, 2 * 16 + head, False)
                    ybf = ybf_r.next()
                    P.op("act", lambda e, ybf=ybf, r=r: e.copy(out=ybf[:, :], in_=yacc[:, r, :]), r=[yacc], w=[ybf])
                    P.store("sp", ynT, ynT[head * 128:(head + 1) * 128, tb * 512:(tb + 1) * 512], ybf, ybf[:, :])
        P.pop()
    if stop == "N":
        P.dma("sp", out_t[0:128, 0:512], f32a[:, :], r=[f32a], w=[out_t])
        P.emit()
        return nc, es

    P.push()
    wus_in = inp("w_up_ssd", [4096, D])
    wun_in = inp("w_up_nsa", [2048, D])
    wo_in = inp("w_out", [D, D])
    mT_d = dten("mT_d", [2048, T], BF16)
    h1_d = dten("h1_d", [T, D], F32)
    wus_v = wus_in.t.ap().rearrange("(kc p) n -> p kc n", p=128)
    wun_v = wun_in.t.ap().rearrange("(kc p) n -> p kc n", p=128)
    yT_v = yT.t.ap().rearrange("(kc p) t -> p kc t", p=128)
    ynT_v = ynT.t.ap().rearrange("(kc p) t -> p kc t", p=128)
    wqA = P.sb("wqA", [128, 32, 512], BF16)
    wqB = P.sb("wqB", [128, 16, 512], BF16)
    yTb_r = Ring([P.sb("yTb%d" % i, [128, 32, 512], BF16) for i in range(2)])
    nTb_r = Ring([P.sb("nTb%d" % i, [128, 16, 512], BF16) for i in range(2)])
    gs_r = Ring([P.sb("gsb%d" % i, [128, 512], BF16) for i in range(2)])
    gn_r = Ring([P.sb("gnb%d" % i, [128, 512], BF16) for i in range(2)])
    for qd in range(4):
        for h2_ in range(2):
            P.load("pool", wqA, wqA[:, h2_ * 16:(h2_ + 1) * 16, :], wus_v[:, h2_ * 16:(h2_ + 1) * 16, qd * 512:(qd + 1) * 512])
        P.load("pool", wqB, wqB[:, :, :], wun_v[:, :, qd * 512:(qd + 1) * 512])
        for tb in range(4):
            yTb = yTb_r.next()
            nTb = nTb_r.next()
            P.load("sp", yTb, yTb[:, :, :], yT_v[:, :, tb * 512:(tb + 1) * 512], src=yT)
            P.load("sp", nTb, nTb[:, :, :], ynT_v[:, :, tb * 512:(tb + 1) * 512], src=ynT)
            for f4 in range(4):
                f = qd * 4 + f4
                gsb = gs_r.next()
                gnb = gn_r.next()
                P.load("sp", gsb, gsb[:, :], gsT[f * 128:(f + 1) * 128, tb * 512:(tb + 1) * 512], src=gsT)
                P.load("sp", gnb, gnb[:, :], gnT[f * 128:(f + 1) * 128, tb * 512:(tb + 1) * 512], src=gnT)
                pA = psf.next()
                for kc in range(32):
                    P.op("pe", lambda e, kc=kc, pA=pA, yTb=yTb, f4=f4: e.matmul(
                        pA[:, :], lhsT=wqA[:, kc, f4 * 128:(f4 + 1) * 128], rhs=yTb[:, kc, :],
                        start=(kc == 0), stop=(kc == 31)), r=[wqA, yTb], w=[pA])
                pB = psf.next()
                for kc in range(16):
                    P.op("pe", lambda e, kc=kc, pB=pB, nTb=nTb, f4=f4: e.matmul(
                        pB[:, :], lhsT=wqB[:, kc, f4 * 128:(f4 + 1) * 128], rhs=nTb[:, kc, :],
                        start=(kc == 0), stop=(kc == 15)), r=[wqB, nTb], w=[pB])
                st = stg_r.next()
                P.op("dve", lambda e, pA=pA, gsb=gsb: e.tensor_tensor(out=f32a[:, :], in0=pA[:, :], in1=gsb[:, :], op=ALU.mult),
                     r=[pA, gsb], w=[f32a])
                P.op("dve", lambda e, pB=pB, gnb=gnb: e.tensor_tensor(out=f32b[:, :], in0=pB[:, :], in1=gnb[:, :], op=ALU.mult),
                     r=[pB, gnb], w=[f32b])
                P.op("dve", lambda e, st=st: e.tensor_tensor(out=st[:, :], in0=f32a[:, :], in1=f32b[:, :], op=ALU.add),
                     r=[f32a, f32b], w=[st])
                P.store("sp", mT_d, mT_d[f * 128:(f + 1) * 128, tb * 512:(tb + 1) * 512], st, st[:, :])
    P.pop()
    P.push()
    wo_sb = P.sb("wo_sb", [128, 16, D], BF16)
    wo_v = wo_in.t.ap().rearrange("(kc p) n -> p kc n", p=128)
    for cb in range(4):
        P.load("pool", wo_sb, wo_sb[:, :, cb * 512:(cb + 1) * 512], wo_v[:, :, cb * 512:(cb + 1) * 512])
    mT_v = mT_d.t.ap().rearrange("(kc p) t -> p kc t", p=128)
    mti_r = Ring([P.sb("mti%d" % i, [128, 16, 128], BF16) for i in range(2)])
    xr_r = Ring([P.sb("xr%d" % i, [128, D], F32) for i in range(2)])
    for i in range(NT):
        mti = mti_r.next()
        xr = xr_r.next()
        P.load("sp", mti, mti[:, :, :], mT_v[:, :, i * 128:(i + 1) * 128], src=mT_d)
        P.load("sp", xr, xr[:, :], x_in[i * 128:(i + 1) * 128, :])
        for cb in range(4):
            pt = psf.next()
            for kc in range(16):
                P.op("pe", lambda e, kc=kc, pt=pt, mti=mti, cb=cb: e.matmul(pt[:, :], lhsT=mti[:, kc, :],
                                                                          rhs=wo_sb[:, kc, cb * 512:(cb + 1) * 512],
                                                                          start=(kc == 0), stop=(kc == 15)),
                     r=[mti, wo_sb], w=[pt])
            P.op("dve", lambda e, pt=pt, xr=xr, cb=cb: e.tensor_tensor(out=xr[:, cb * 512:(cb + 1) * 512], in0=pt[:, :],
                                                                     in1=xr[:, cb * 512:(cb + 1) * 512], op=ALU.add),
                 r=[pt, xr], w=[xr])
        P.store("sp", h1_d, h1_d[i * 128:(i + 1) * 128, :], xr, xr[:, :])
    P.pop()
    if stop == "D":
        P.dma("sp", out_t[0:128, 0:512], f32a[:, :], r=[f32a], w=[out_t])
        P.emit()
        return nc, es

    P.push()
    mem_in = inp("mem", [256, D])
    n2_in = inp("norm2_w", [1, D])
    mn_in = inp("mem_norm_w", [1, D])
    xq_in = inp("xq_w", [D, 512])
    xkv_in = inp("xkv_w", [D, 1024])
    xqn_in = inp("x_q_norm_w", [1, 128])
    xkn_in = inp("x_k_norm_w", [1, 128])
    xo_in = inp("xo_w", [512, D])
    h2_d = dten("h2_d", [T, D], F32)
    wb2 = P.sb("wb2", [128, D], F32)
    wbm = P.sb("wbm", [128, D], F32)
    P.load("sp", wb2, wb2[:, :], n2_in[0:1, :].partition_broadcast(128))
    P.load("sp", wbm, wbm[:, :], mn_in[0:1, :].partition_broadcast(128))
    xqnw = P.sb("xqnw", [128, 4, 128], F32)
    xknw = P.sb("xknw", [128, 4, 128], F32)
    for hh in range(4):
        P.load("sp", xqnw, xqnw[:, hh, :], xqn_in[0:1, :].partition_broadcast(128))
        P.load("sp", xknw, xknw[:, hh, :], xkn_in[0:1, :].partition_broadcast(128))
    xq_sb = P.sb("xq_sb", [128, 16, 512], BF16)
    xkv_sb = P.sb("xkv_sb", [128, 16, 1024], BF16)
    xo_sb = P.sb("xo_sb", [128, 4, D], BF16)
    P.load("pool", xq_sb, xq_sb[:, :, :], xq_in.t.ap().rearrange("(kc p) n -> p kc n", p=128))
    P.load("pool", xkv_sb, xkv_sb[:, :, 0:512], xkv_in.t.ap().rearrange("(kc p) n -> p kc n", p=128)[:, :, 0:512])
    P.load("pool", xkv_sb, xkv_sb[:, :, 512:1024], xkv_in.t.ap().rearrange("(kc p) n -> p kc n", p=128)[:, :, 512:1024])
    for cb in range(4):
        P.load("pool", xo_sb, xo_sb[:, :, cb * 512:(cb + 1) * 512],
               xo_in.t.ap().rearrange("(kc p) n -> p kc n", p=128)[:, :, cb * 512:(cb + 1) * 512])
    onesb = P.sb("onesb", [128, 128], BF16)
    P.op("dve", lambda e: e.memset(onesb[:, :], 1.0), w=[onesb])
    rstd4e = P.sb("rstd4e", [128, 4], F32)
    hT_r = Ring([P.sb("hTe%d" % i, [128, 16, 128], BF16) for i in range(2)])
    h1t_r = Ring([P.sb("h1t%d" % i, [128, D], F32) for i in range(2)])
    kxT = P.sb("kxT", [128, 4, 256], BF16)
    vx = P.sb("vx", [128, 2, 512], BF16)
    qxT = P.sb("qxT", [128, 4, 128], BF16)
    pT_r = Ring([P.sb("pTe%d" % i, [128, 2, 128], BF16) for i in range(2)])
    oT = P.sb("oT", [128, 4, 128], BF16)
    rsum = P.sb("rsum", [128, 128], F32)

    def head_norm(pt, nw_tl, dst_hn):
        P.op("dve", lambda e: e.memset(rstd4e[:, :], 0.0), w=[rstd4e])
        for hh in range(4):
            P.op("act", lambda e, hh=hh: e.activation(out=f32a[:, hh * 128:(hh + 1) * 128],
                                                      in_=pt[:, hh * 128:(hh + 1) * 128], func=AF.Square,
                                                      accum_out=rstd4e[:, hh:hh + 1]), r=[pt], w=[f32a, rstd4e])
        P.op("dve", lambda e: e.tensor_scalar(out=rstd4e[:, :], in0=rstd4e[:, :], scalar1=1.0 / 128, scalar2=EPS,
                                              op0=ALU.mult, op1=ALU.add), r=[rstd4e], w=[rstd4e])
        P.op("act", lambda e: e.activation(out=rstd4e[:, :], in_=rstd4e[:, :], func=AF.Sqrt), r=[rstd4e], w=[rstd4e])
        P.op("dve", lambda e: e.reciprocal(out=rstd4e[:, :], in_=rstd4e[:, :]), r=[rstd4e], w=[rstd4e])
        P.op("dve", lambda e: e.tensor_tensor(out=f32a[:, :].rearrange("p (h d) -> p h d", d=128),
                                              in0=pt[:, :].rearrange("p (h d) -> p h d", d=128),
                                              in1=rstd4e[:, :].unsqueeze(2).to_broadcast([128, 4, 128]),
                                              op=ALU.mult), r=[pt, rstd4e], w=[f32a], selfsync=True)
        P.op("dve", lambda e: e.tensor_tensor(out=dst_hn[:, 0:512], in0=f32a[:, :],
                                              in1=nw_tl[:, :, :].rearrange("p h d -> p (h d)"),
                                              op=ALU.mult), r=[f32a, nw_tl], w=[dst_hn])

    for mt in range(2):
        h1t = h1t_r.next()
        hn = hn_r.next()
        hT = hT_r.next()
        P.load("sp", h1t, h1t[:, :], mem_in[mt * 128:(mt + 1) * 128, :])
        rmsnorm_tile(h1t, wbm, hn)
        transpose_into(hn, 16, lambda half, nb, hT=hT: hT[:, half * 8:half * 8 + nb, :], hT)
        for part in range(2):
            pt = psf.next()
            for kc in range(16):
                P.op("pe", lambda e, kc=kc, pt=pt, hT=hT, part=part: e.matmul(
                    pt[:, :], lhsT=hT[:, kc, :], rhs=xkv_sb[:, kc, part * 512:(part + 1) * 512],
                    start=(kc == 0), stop=(kc == 15)), r=[hT, xkv_sb], w=[pt])
            if part == 0:
                hk = hn_r.next()
                head_norm(pt, xknw, hk)
                transpose_into(hk, 4, lambda half, nb, mt=mt: kxT[:, :, mt * 128:(mt + 1) * 128], kxT)
            else:
                P.op("act", lambda e, pt=pt, mt=mt: e.copy(out=vx[:, mt, :], in_=pt[:, :]), r=[pt], w=[vx])
    for i in range(NT):
        h1t = h1t_r.next()
        hn = hn_r.next()
        hT = hT_r.next()
        P.load("sp", h1t, h1t[:, :], h1_d[i * 128:(i + 1) * 128, :], src=h1_d)
        rmsnorm_tile(h1t, wb2, hn)
        transpose_into(hn, 16, lambda half, nb, hT=hT: hT[:, half * 8:half * 8 + nb, :], hT)
        pt = psf.next()
        for kc in range(16):
            P.op("pe", lambda e, kc=kc, pt=pt, hT=hT: e.matmul(pt[:, :], lhsT=hT[:, kc, :], rhs=xq_sb[:, kc, :],
                                                             start=(kc == 0), stop=(kc == 15)),
                 r=[hT, xq_sb], w=[pt])
        hq = hn_r.next()
        head_norm(pt, xqnw, hq)
        transpose_into(hq, 4, lambda half, nb: qxT[:, :, :], qxT)
        for hh in range(4):
            pT = pT_r.next()
            for mt in range(2):
                psc = psf.next()
                P.op("pe", lambda e, psc=psc, hh=hh, mt=mt: e.matmul(psc[:, :128], lhsT=kxT[:, hh, mt * 128:(mt + 1) * 128],
                                                                     rhs=qxT[:, hh, :], start=True, stop=True),
                     r=[kxT, qxT], w=[psc])
                P.op("act", lambda e, psc=psc, pT=pT, mt=mt: e.activation(out=pT[:, mt, :], in_=psc[:, :128], func=AF.Exp,
                                                                         scale=float(128 ** -0.5)), r=[psc], w=[pT])
            po = psf.next()
            pz = psf.next()
            for mt in range(2):
                P.op("pe", lambda e, po=po, pT=pT, hh=hh, mt=mt: e.matmul(po[:, :128], lhsT=vx[:, mt, hh * 128:(hh + 1) * 128],
                                                                         rhs=pT[:, mt, :], start=(mt == 0), stop=(mt == 1)),
                     r=[vx, pT], w=[po])
            for mt in range(2):
                P.op("pe", lambda e, pz=pz, pT=pT, mt=mt: e.matmul(pz[:, :128], lhsT=onesb[:, :], rhs=pT[:, mt, :],
                                                                  start=(mt == 0), stop=(mt == 1)),
                     r=[onesb, pT], w=[pz])
            P.op("dve", lambda e, pz=pz: e.reciprocal(out=rsum[:, :], in_=pz[:, :128]), r=[pz], w=[rsum])
            P.op("dve", lambda e, po=po, hh=hh: e.tensor_tensor(out=oT[:, hh, :], in0=po[:, :128], in1=rsum[:, :],
                                                               op=ALU.mult), r=[po, rsum], w=[oT])
        for cb in range(4):
            pt = psf.next()
            for kc in range(4):
                P.op("pe", lambda e, kc=kc, pt=pt, cb=cb: e.matmul(pt[:, :], lhsT=oT[:, kc, :],
                                                                  rhs=xo_sb[:, kc, cb * 512:(cb + 1) * 512],
                                                                  start=(kc == 0), stop=(kc == 3)),
                     r=[oT, xo_sb], w=[pt])
            P.op("dve", lambda e, pt=pt, h1t=h1t, cb=cb: e.tensor_tensor(out=h1t[:, cb * 512:(cb + 1) * 512], in0=pt[:, :],
                                                                       in1=h1t[:, cb * 512:(cb + 1) * 512], op=ALU.add),
                 r=[pt, h1t], w=[h1t])
        P.store("sp", h2_d, h2_d[i * 128:(i + 1) * 128, :], h1t, h1t[:, :])
    P.pop()
    if stop == "E":
        P.dma("sp", out_t[0:128, 0:512], f32a[:, :], r=[f32a], w=[out_t])
        P.emit()
        return nc, es

    n3_in = inp("norm3_w", [1, D])
    rgw_in = inp("router_g_w", [D, 8])
    rew_in = inp("router_e_w", [D, 64])
    rb_in = inp("router_b", [1, 72])
    wg_in = inp("moe_w_gate", [64, D, EH])
    wu_in = inp("moe_w_up", [64, D, EH])
    wd_in = inp("moe_w_down", [64, EH, D])
    iota_in = inp("iota_row", [128, CG])
    gbase_in = inp("gbase", [128, 8])
    ustr_in = inp("ustrict", [128, 128], BF16)
    XgT_d = dten("XgT_d", [8, 128, 16, CG], BF16)
    Y_d = dten("Y_d", [8 * CG, D], F32)
    P.push()
    posm_all = P.sb("posm_all", [128, NT, 8], F32)
    wdh = P.sb("wdh", [128, NT, 16], BF16)
    ridx = P.sb("ridx", [128, NT], I32)
    wslot = P.sb("wslot", [128, 8, 3, 8], F32)
    onesb2 = P.sb("onesb2", [128, 128], BF16)
    ustr = P.sb("ustr", [128, 128], BF16)
    iota_row = P.sb("iota_row", [128, CG], F32)
    gbase = P.sb("gbase", [128, 8], F32)
    P.op("dve", lambda e: e.memset(onesb2[:, :], 1.0), w=[onesb2])
    P.load("sp", ustr, ustr[:, :], ustr_in[:, :])
    P.load("sp", iota_row, iota_row[:, :], iota_in[:, :])
    P.load("sp", gbase, gbase[:, :], gbase_in[:, :])
    P.push()
    hf_all = P.sb("hf_all", [128, NT, D], BF16)
    wb3 = P.sb("wb3", [128, D], F32)
    P.load("sp", wb3, wb3[:, :], n3_in[0:1, :].partition_broadcast(128))
    identf2 = P.sb("identf2", [128, 128], F32)
    P.load("sp", identf2, identf2[:, :], identf_in[:, :])
    wr_sb = P.sb("wr_sb", [128, 16, 72], F32)
    P.load("sp", wr_sb, wr_sb[:, :, 0:8], rgw_in.t.ap().rearrange("(kc p) n -> p kc n", p=128))
    P.load("sp", wr_sb, wr_sb[:, :, 8:72], rew_in.t.ap().rearrange("(kc p) n -> p kc n", p=128))
    rbias = P.sb("rbias", [128, 72], F32)
    P.load("sp", rbias, rbias[:, :], rb_in[0:1, :].partition_broadcast(128))
    h2t_r = Ring([P.sb("h2t%d" % i, [128, D], F32) for i in range(2)])
    hf32 = P.sb("hf32", [128, D], F32)
    hfT32 = P.sb("hfT32", [128, 16, 128], F32)
    cnt = P.sb("cnt", [128, 8], F32)
    P.op("dve", lambda e: e.memset(cnt[:, :], 0.0), w=[cnt])
    lg = P.sb("lg", [128, 72], F32)
    m8 = P.sb("m8", [128, 8], F32)
    m8e = P.sb("m8e", [128, 8], F32)
    oh = P.sb("oh", [128, 8], F32)
    ohb = P.sb("ohb", [128, 8], BF16)
    sc1 = P.sb("sc1", [128, 8], F32)
    le = P.sb("le", [128, 8], F32)
    e2 = P.sb("e2", [128, 8], F32)
    msk2 = P.sb("msk2", [128, 8], F32)
    wd32 = P.sb("wd32", [128, 8], F32)
    pos = P.sb("pos", [128, 8], F32)
    tmp8 = P.sb("tmp8", [128, 8], F32)
    junk8 = P.sb("junk8", [128, 8], F32)
    rid32 = P.sb("rid32", [128, 1], F32)

    def rms_f32(src_tl, wb_tl, dst_tl):
        ss = ss_r.next()
        P.op("dve", lambda e: e.memset(ss[:, 0:1], 0.0), w=[ss])
        P.op("act", lambda e: e.activation(out=junk[:, :], in_=src_tl[:, :], func=AF.Square, accum_out=ss[:, 0:1]),
             r=[src_tl], w=[junk, ss])
        P.op("dve", lambda e: e.tensor_scalar(out=ss[:, 0:1], in0=ss[:, 0:1], scalar1=1.0 / D, scalar2=EPS,
                                              op0=ALU.mult, op1=ALU.add), r=[ss], w=[ss])
        P.op("act", lambda e: e.activation(out=ss[:, 0:1], in_=ss[:, 0:1], func=AF.Sqrt), r=[ss], w=[ss])
        P.op("dve", lambda e: e.reciprocal(out=ss[:, 0:1], in_=ss[:, 0:1]), r=[ss], w=[ss])
        P.op("dve", lambda e: e.scalar_tensor_tensor(out=dst_tl[:, :], in0=src_tl[:, :], scalar=ss[:, 0:1],
                                                     in1=wb_tl[:, :], op0=ALU.mult, op1=ALU.mult),
             r=[src_tl, ss, wb_tl], w=[dst_tl])

    def route_tile(i):
        h2t = h2t_r.next()
        P.load("sp", h2t, h2t[:, :], h2_d[i * 128:(i + 1) * 128, :], src=h2_d)
        rms_f32(h2t, wb3, hf32)
        P.op("act", lambda e: e.copy(out=hf_all[:, i, :], in_=hf32[:, :]), r=[hf32], w=[hf_all])
        for q4 in range(4):
            pt = psf.next()
            for j in range(4):
                kc = q4 * 4 + j
                P.op("pe", lambda e, pt=pt, j=j, kc=kc: e.transpose(out=pt[:, j * 128:(j + 1) * 128],
                                                                   in_=hf32[:, kc * 128:(kc + 1) * 128],
                                                                   identity=identf2[:, :]),
                     r=[hf32, identf2], w=[pt])
            P.op("act", lambda e, pt=pt, q4=q4: e.copy(out=hfT32[:, q4 * 4:(q4 + 1) * 4, :],
                                                       in_=pt[:, :].rearrange("p (k t) -> p k t", t=128)),
                 r=[pt], w=[hfT32])
        pl = psf.next()
        for kc in range(16):
            P.op("pe", lambda e, kc=kc: e.matmul(pl[:, :72], lhsT=hfT32[:, kc, :], rhs=wr_sb[:, kc, :],
                                                 start=(kc == 0), stop=(kc == 15)), r=[hfT32, wr_sb], w=[pl])
        P.op("dve", lambda e: e.tensor_tensor(out=lg[:, :], in0=pl[:, :72], in1=rbias[:, :], op=ALU.add),
             r=[pl, rbias], w=[lg])
        P.op("dve", lambda e: e.max(out=m8[:, :], in_=lg[:, 0:8]), r=[lg], w=[m8])
        P.op("dve", lambda e: e.tensor_scalar(out=oh[:, :], in0=lg[:, 0:8], scalar1=m8[:, 0:1], scalar2=None,
                                              op0=ALU.is_ge), r=[lg, m8], w=[oh])
        P.op("act", lambda e: e.copy(out=ohb[:, :], in_=oh[:, :]), r=[oh], w=[ohb])
        P.op("dve", lambda e: e.tensor_scalar(out=sc1[:, 0:1], in0=m8[:, 0:1], scalar1=-1.0, scalar2=None,
                                              op0=ALU.mult), r=[m8], w=[sc1])
        P.op("dve", lambda e: e.memset(sc1[:, 1:2], 0.0), r=[sc1], w=[sc1])
        P.op("act", lambda e: e.activation(out=junk8[:, :], in_=lg[:, 0:8], func=AF.Exp, bias=sc1[:, 0:1],
                                           accum_out=sc1[:, 1:2]), r=[lg, sc1], w=[junk8, sc1])
        P.op("dve", lambda e: e.reciprocal(out=sc1[:, 2:3], in_=sc1[:, 1:2]), r=[sc1], w=[sc1])
        for g in range(8):
            if g == 0:
                P.op("dve", lambda e: e.tensor_scalar(out=le[:, :], in0=lg[:, 8:16], scalar1=oh[:, 0:1], scalar2=None,
                                                      op0=ALU.mult), r=[lg, oh], w=[le])
            else:
                P.op("dve", lambda e, g=g: e.scalar_tensor_tensor(out=le[:, :], in0=lg[:, 8 + g * 8:16 + g * 8],
                                                                  scalar=oh[:, g:g + 1], in1=le[:, :],
                                                                  op0=ALU.mult, op1=ALU.add), r=[lg, oh, le], w=[le])
        P.op("dve", lambda e: e.max(out=m8e[:, :], in_=le[:, :]), r=[le], w=[m8e])
        P.op("dve", lambda e: e.tensor_scalar(out=msk2[:, :], in0=le[:, :], scalar1=m8e[:, 1:2], scalar2=None,
                                              op0=ALU.is_ge), r=[le, m8e], w=[msk2])
        P.op("dve", lambda e: e.tensor_scalar(out=sc1[:, 3:4], in0=m8e[:, 0:1], scalar1=-1.0, scalar2=None,
                                              op0=ALU.mult), r=[m8e], w=[sc1])
        P.op("act", lambda e: e.activation(out=e2[:, :], in_=le[:, :], func=AF.Exp, bias=sc1[:, 3:4]),
             r=[le, sc1], w=[e2])
        P.op("act", lambda e: e.activation(out=sc1[:, 4:5], in_=m8e[:, 1:2], func=AF.Exp, bias=sc1[:, 3:4]),
             r=[m8e, sc1], w=[sc1])
        P.op("dve", lambda e: e.tensor_scalar(out=sc1[:, 4:5], in0=sc1[:, 4:5], scalar1=1.0, scalar2=None,
                                              op0=ALU.add), r=[sc1], w=[sc1])
        P.op("dve", lambda e: e.reciprocal(out=sc1[:, 4:5], in_=sc1[:, 4:5]), r=[sc1], w=[sc1])
        P.op("dve", lambda e: e.tensor_tensor(out=sc1[:, 5:6], in0=sc1[:, 4:5], in1=sc1[:, 2:3], op=ALU.mult),
             r=[sc1], w=[sc1])
        P.op("dve", lambda e: e.scalar_tensor_tensor(out=wd32[:, :], in0=e2[:, :], scalar=sc1[:, 5:6], in1=msk2[:, :],
                                                     op0=ALU.mult, op1=ALU.mult), r=[e2, sc1, msk2], w=[wd32])
        P.op("act", lambda e: e.copy(out=wdh[:, i, 0:8], in_=wd32[:, :]), r=[wd32], w=[wdh])
        P.op("dve", lambda e: e.tensor_tensor(out=tmp8[:, :], in0=wd32[:, :], in1=wdh[:, i, 0:8], op=ALU.subtract),
             r=[wd32, wdh], w=[tmp8])
        P.op("act", lambda e: e.copy(out=wdh[:, i, 8:16], in_=tmp8[:, :]), r=[tmp8], w=[wdh])
        pp = psf.next()
        P.op("pe", lambda e: e.matmul(pp[:, 0:8], lhsT=ustr[:, :], rhs=ohb[:, :], start=True, stop=True),
             r=[ustr, ohb], w=[pp])
        P.op("dve", lambda e: e.tensor_tensor(out=pos[:, :], in0=pp[:, 0:8], in1=cnt[:, :], op=ALU.add),
             r=[pp, cnt], w=[pos])
        pc2 = psf.next()
        P.op("pe", lambda e: e.matmul(pc2[:, 0:8], lhsT=onesb2[:, :], rhs=ohb[:, :], start=True, stop=True),
             r=[onesb2, ohb], w=[pc2])
        P.op("dve", lambda e: e.tensor_tensor(out=cnt[:, :], in0=cnt[:, :], in1=pc2[:, 0:8], op=ALU.add),
             r=[cnt, pc2], w=[cnt])
        P.op("dve", lambda e: e.scalar_tensor_tensor(out=tmp8[:, :], in0=pos[:, :], scalar=1.0, in1=oh[:, :],
                                                     op0=ALU.add, op1=ALU.mult), r=[pos, oh], w=[tmp8])
        P.op("dve", lambda e: e.tensor_scalar(out=posm_all[:, i, :], in0=tmp8[:, :], scalar1=-1.0, scalar2=None,
                                              op0=ALU.add), r=[tmp8], w=[posm_all])
        P.op("dve", lambda e: e.tensor_tensor(out=tmp8[:, :], in0=pos[:, :], in1=gbase[:, :], op=ALU.add),
             r=[pos, gbase], w=[tmp8])
        P.op("dve", lambda e: e.tensor_tensor(out=tmp8[:, :], in0=tmp8[:, :], in1=oh[:, :], op=ALU.mult),
             r=[tmp8, oh], w=[tmp8])
        P.op("dve", lambda e: e.memset(rid32[:, :], 0.0), w=[rid32])
        P.op("act", lambda e: e.activation(out=junk8[:, :], in_=tmp8[:, :], func=AF.Identity, accum_out=rid32[:, 0:1]),
             r=[tmp8, rid32], w=[junk8, rid32])
        P.op("dve", lambda e: e.tensor_copy(out=ridx[:, i:i + 1], in_=rid32[:, 0:1]), r=[rid32], w=[ridx])

    for i in range(NT):
        route_tile(i)

    Sg = P.sb("Sg", [128, NT, CG], BF16)
    xstg = P.sb("xstg", [128, 16, CG], BF16)

    def gather_group(g):
        for i in range(NT):
            P.op("dve", lambda e, i=i: e.tensor_scalar(out=Sg[:, i, :], in0=iota_row[:, :],
                                                       scalar1=posm_all[:, i, g:g + 1], scalar2=None,
                                                       op0=ALU.is_equal), r=[iota_row, posm_all], w=[Sg])
        for kc in range(16):
            pt = psf.next()
            for i in range(NT):
                P.op("pe", lambda e, pt=pt, kc=kc, i=i: e.matmul(pt[:, :CG], lhsT=hf_all[:, i, kc * 128:(kc + 1) * 128],
                                                                rhs=Sg[:, i, :], start=(i == 0), stop=(i == NT - 1)),
                     r=[hf_all, Sg], w=[pt])
            P.op("act", lambda e, pt=pt, kc=kc: e.copy(out=xstg[:, kc, :], in_=pt[:, :CG]), r=[pt], w=[xstg])
        P.store("sp", XgT_d, XgT_d[g, :, :, :], xstg, xstg[:, :, :])
        for ct in range(3):
            pw = psf.next()
            n = 0
            for i in range(NT):
                for hl in range(2):
                    P.op("pe", lambda e, pw=pw, i=i, hl=hl, ct=ct, n=n: e.matmul(
                        pw[:, 0:8], lhsT=Sg[:, i, ct * 128:(ct + 1) * 128], rhs=wdh[:, i, hl * 8:(hl + 1) * 8],
                        start=(n == 0), stop=(n == 2 * NT - 1)), r=[Sg, wdh], w=[pw])
                    n += 1
            P.op("act", lambda e, pw=pw, ct=ct: e.copy(out=wslot[:, g, ct, :], in_=pw[:, 0:8]), r=[pw], w=[wslot])

    for g in range(8):
        gather_group(g)
    P.pop()

    P.push()
    xg = P.sb("xg", [128, 16, CG], BF16)
    yg = P.sb("yg", [128, 3, D], F32)
    wgb_r = Ring([P.sb("wgb%d" % i, [128, 16, 512], BF16) for i in range(3)])
    wub_r = Ring([P.sb("wub%d" % i, [128, 16, 512], BF16) for i in range(3)])
    wdb_r = Ring([P.sb("wdb%d" % i, [128, 4, D], BF16) for i in range(2)])
    hact_r = Ring([P.sb("hact%d" % i, [128, 512], BF16) for i in range(2)])
    hactT_r = Ring([P.sb("hactT%d" % i, [128, 4, 128], BF16) for i in range(2)])
    sg32 = P.sb("sg32", [128, 512], F32)
    FB = [(0, 512), (512, 512), (1024, 384)]

    def expert(g, j, first):
        e_id = g * 8 + j
        for (f0, nf) in FB:
            wgb = wgb_r.next()
            wub = wub_r.next()
            wdb = wdb_r.next()
            nch = nf // 128
            P.load("pool", wgb, wgb[:, :, :nf], wg_in.t.ap()[e_id].rearrange("(kc p) n -> p kc n", p=128)[:, :, f0:f0 + nf])
            P.load("pool", wub, wub[:, :, :nf], wu_in.t.ap()[e_id].rearrange("(kc p) n -> p kc n", p=128)[:, :, f0:f0 + nf])
            P.load("pool", wdb, wdb[:, :nch, :], wd_in.t.ap()[e_id, f0:f0 + nf, :].rearrange("(c p) n -> p c n", p=128))
            for ct in range(3):
                pga = psf.next()
                for kc in range(16):
                    P.op("pe", lambda e, pga=pga, kc=kc, ct=ct, wgb=wgb, nf=nf: e.matmul(
                        pga[:, :nf], lhsT=xg[:, kc, ct * 128:(ct + 1) * 128], rhs=wgb[:, kc, :nf],
                        start=(kc == 0), stop=(kc == 15)), r=[xg, wgb], w=[pga])
                pup = psf.next()
                for kc in range(16):
                    P.op("pe", lambda e, pup=pup, kc=kc, ct=ct, wub=wub, nf=nf: e.matmul(
                        pup[:, :nf], lhsT=xg[:, kc, ct * 128:(ct + 1) * 128], rhs=wub[:, kc, :nf],
                        start=(kc == 0), stop=(kc == 15)), r=[xg, wub], w=[pup])
                hact = hact_r.next()
                P.op("act", lambda e, pga=pga, nf=nf: e.activation(out=sg32[:, :nf], in_=pga[:, :nf], func=AF.Silu),
                     r=[pga], w=[sg32])
                P.op("dve", lambda e, pup=pup, hact=hact, nf=nf, ct=ct: e.scalar_tensor_tensor(
                    out=hact[:, :nf], in0=sg32[:, :nf], scalar=wslot[:, g, ct, j:j + 1], in1=pup[:, :nf],
                    op0=ALU.mult, op1=ALU.mult), r=[sg32, wslot, pup], w=[hact])
                hactT = hactT_r.next()
                pb = psb.next()
                for c4 in range(nch):
                    P.op("pe", lambda e, pb=pb, c4=c4, hact=hact: e.transpose(out=pb[:, c4 * 128:(c4 + 1) * 128],
                                                                             in_=hact[:, c4 * 128:(c4 + 1) * 128],
                                                                             identity=ident[:, :]),
                         r=[hact, ident], w=[pb])
                P.op("act", lambda e, pb=pb, hactT=hactT, nch=nch: e.copy(
                    out=hactT[:, :nch, :], in_=pb[:, :nch * 128].rearrange("p (k t) -> p k t", t=128)),
                    r=[pb], w=[hactT])
                for db in range(4):
                    pdn = psf.next()
                    for c4 in range(nch):
                        P.op("pe", lambda e, pdn=pdn, c4=c4, db=db, hactT=hactT, wdb=wdb, nch=nch: e.matmul(
                            pdn[:, :], lhsT=hactT[:, c4, :], rhs=wdb[:, c4, db * 512:(db + 1) * 512],
                            start=(c4 == 0), stop=(c4 == nch - 1)), r=[hactT, wdb], w=[pdn])
                    if first and f0 == 0:
                        P.op("act", lambda e, pdn=pdn, ct=ct, db=db: e.copy(out=yg[:, ct, db * 512:(db + 1) * 512],
                                                                           in_=pdn[:, :]), r=[pdn], w=[yg])
                    else:
                        P.op("dve", lambda e, pdn=pdn, ct=ct, db=db: e.tensor_tensor(
                            out=yg[:, ct, db * 512:(db + 1) * 512], in0=yg[:, ct, db * 512:(db + 1) * 512],
                            in1=pdn[:, :], op=ALU.add), r=[yg, pdn], w=[yg])

    for g in range(8):
        P.load("sp", xg, xg[:, :, :], XgT_d[g, :, :, :], src=XgT_d)
        for j in range(8):
            expert(g, j, j == 0)
        P.store("sp", Y_d, Y_d.t.ap()[g * CG:(g + 1) * CG, :].rearrange("(c p) n -> p c n", p=128), yg, yg[:, :, :])
    P.pop()

    P.push()
    yr_r = Ring([P.sb("yr%d" % i, [128, D], F32) for i in range(2)])
    h2o_r = Ring([P.sb("h2o%d" % i, [128, D], F32) for i in range(2)])
    for i in range(NT):
        yr = yr_r.next()
        h2o = h2o_r.next()
        P.load("sp", h2o, h2o[:, :], h2_d[i * 128:(i + 1) * 128, :], src=h2_d)
        P.dma_fn("pool", lambda e, yr=yr, i=i: e.indirect_dma_start(
            out=yr[:, :], out_offset=None, in_=Y_d[:, :],
            in_offset=bass.IndirectOffsetOnAxis(ap=ridx[:, i:i + 1], axis=0)),
            r=[Y_d, ridx], w=[yr], key=yr.b.name)
        P.op("dve", lambda e, yr=yr, h2o=h2o: e.tensor_tensor(out=h2o[:, :], in0=h2o[:, :], in1=yr[:, :], op=ALU.add),
             r=[h2o, yr], w=[h2o])
        P.store("sp", out_t, out_t[i * 128:(i + 1) * 128, :], h2o, h2o[:, :])
    P.pop()
    P.pop()

    P.emit()
    return nc, es


def rope_tables():
    half = 64
    inv = (10000.0 ** (-np.arange(half, dtype=np.float32) / half)).astype(np.float32)
    pos = np.arange(T, dtype=np.float32)
    ang = pos[:, None] * inv[None, :]
    cos = np.cos(ang).astype(np.float32).reshape(NT, 128, 64).transpose(1, 0, 2)
    sin = np.sin(ang).astype(np.float32).reshape(NT, 128, 64).transpose(1, 0, 2)
    return np.ascontiguousarray(cos), np.ascontiguousarray(sin)


_NSA_CONST = None


def nsa_constants():
    global _NSA_CONST
    if _NSA_CONST is not None:
        return _NSA_CONST
    bf = ml_dtypes.bfloat16
    c = np.arange(128)[:, None]
    q = np.arange(T)[None, :]
    cmask = np.where((16 * c + 31 <= q) & (c < 127), 0.0, NEG).astype(np.float32)
    wmask = np.zeros((128, 8, 512), np.float32)
    k = np.arange(128)[:, None]
    qq = np.arange(512)[None, :]
    for idx in range(8):
        rel = idx - 4
        diff = qq - (rel * 128 + k)
        wmask[:, idx, :] = np.where((diff >= 0) & (diff < 512), 0.0, NEG)
    E = np.zeros((32, 16, 128), np.float32)
    for kt in range(16):
        for kk in range(128):
            E[2 * kt + kk // 64, kt, kk] = 1.0
    sel48 = np.zeros((48, 48, 128), np.float32)
    for j in range(48):
        sel48[j, j, :] = 1.0
    ci = np.arange(128)[:, None]
    sj = np.arange(32)[None, :]
    ovl = ((ci * 16 < (sj + 1) * 64) & (ci * 16 + 32 > sj * 64) & (ci < 127)).astype(np.float32)
    keep = np.zeros((128, 16, 32), np.float32)
    addc = np.zeros((128, 16, 32), np.float32)
    for qt in range(16):
        tq = qt * 128 + np.arange(128)[:, None]
        blk_t = tq // 64
        sb_ = np.arange(32)[None, :]
        valid = sb_ <= blk_t
        forced = (sb_ == 0) | (sb_ == blk_t) | (sb_ == blk_t - 1)
        keep[:, qt, :] = (valid & ~forced).astype(np.float32)
        addc[:, qt, :] = np.where(~valid, -1e30, np.where(forced, 1e4 + sb_, 0.0))
    _NSA_CONST = {"cmask": cmask.astype(bf), "wmask": wmask.astype(bf), "Esel": E.astype(bf), "sel48": sel48.astype(bf),
                  "ovl": ovl, "keepc": keep, "addc": addc}
    return _NSA_CONST


def make_in_map(inputs, b):
    f = np.float32
    cos, sin = rope_tables()
    m = {}
    m["x"] = np.ascontiguousarray(inputs["x"][b], dtype=f)
    m["w_in"] = np.ascontiguousarray(inputs["w_in"][0], dtype=f)
    m["norm1_w"] = np.ascontiguousarray(inputs["norm1_w"][0].reshape(1, D), dtype=f)
    m["conv_w"] = np.ascontiguousarray(inputs["ssd_conv_w"][0].reshape(4, 48, 128).transpose(2, 1, 0), dtype=f)
    m["conv_b"] = np.ascontiguousarray(inputs["ssd_conv_b"][0].reshape(48, 128).T, dtype=f)
    m["qnw"] = np.ascontiguousarray(inputs["nsa_q_norm_w"][0].reshape(1, 128), dtype=f)
    m["knw"] = np.ascontiguousarray(inputs["nsa_k_norm_w"][0].reshape(3, 128), dtype=f)
    m["rope_cos"] = cos
    m["rope_sin"] = sin
    m["ident"] = np.eye(128, dtype=np.float32).astype(ml_dtypes.bfloat16)
    m["identf"] = np.eye(128, dtype=np.float32)
    m["triu"] = np.triu(np.ones((128, 128), dtype=np.float32))
    m["dt_bias"] = np.ascontiguousarray(inputs["ssd_dt_bias"][0].reshape(1, 64), dtype=f)
    m["a_log"] = np.ascontiguousarray(inputs["ssd_a_log"][0].reshape(1, 64), dtype=f)
    m["d_skip"] = np.ascontiguousarray(inputs["ssd_d"][0].reshape(1, 64), dtype=f)
    m["ssd_norm_w"] = np.ascontiguousarray(inputs["ssd_norm_w"][0].reshape(1, 4096), dtype=f)
    m.update(nsa_constants())
    m["cmp_pe_kT"] = np.ascontiguousarray(inputs["cmp_pe_k"][0].T, dtype=f)
    m["cmp_pe_vT"] = np.ascontiguousarray(inputs["cmp_pe_v"][0].T, dtype=f)
    m["cmp_w1_k"] = np.ascontiguousarray(inputs["cmp_w1_k"][0], dtype=f)
    m["cmp_w1_v"] = np.ascontiguousarray(inputs["cmp_w1_v"][0], dtype=f)
    m["cmp_w2_k"] = np.ascontiguousarray(inputs["cmp_w2_k"][0], dtype=f)
    m["cmp_w2_v"] = np.ascontiguousarray(inputs["cmp_w2_v"][0], dtype=f)
    m["w_up_ssd"] = np.ascontiguousarray(inputs["w_up_ssd"][0], dtype=f)
    m["w_up_nsa"] = np.ascontiguousarray(inputs["w_up_nsa"][0], dtype=f)
    m["w_out"] = np.ascontiguousarray(inputs["w_out"][0], dtype=f)
    m["mem"] = np.ascontiguousarray(inputs["mem"][b], dtype=f)
    m["norm2_w"] = np.ascontiguousarray(inputs["norm2_w"][0].reshape(1, D), dtype=f)
    m["mem_norm_w"] = np.ascontiguousarray(inputs["mem_norm_w"][0].reshape(1, D), dtype=f)
    m["xq_w"] = np.ascontiguousarray(inputs["xq_w"][0], dtype=f)
    m["xkv_w"] = np.ascontiguousarray(inputs["xkv_w"][0], dtype=f)
    m["x_q_norm_w"] = np.ascontiguousarray(inputs["x_q_norm_w"][0].reshape(1, 128), dtype=f)
    m["x_k_norm_w"] = np.ascontiguousarray(inputs["x_k_norm_w"][0].reshape(1, 128), dtype=f)
    m["xo_w"] = np.ascontiguousarray(inputs["xo_w"][0], dtype=f)
    if "moe_w_gate" in inputs:
        m["norm3_w"] = np.ascontiguousarray(inputs["norm3_w"][0].reshape(1, D), dtype=f)
        m["router_g_w"] = np.ascontiguousarray(inputs["router_g_w"][0], dtype=f)
        m["router_e_w"] = np.ascontiguousarray(inputs["router_e_w"][0], dtype=f)
        m["router_b"] = np.ascontiguousarray(
            np.concatenate([inputs["router_g_b"][0].reshape(-1), inputs["router_e_b"][0].reshape(-1)]).reshape(1, 72), dtype=f)
        m["moe_w_gate"] = np.ascontiguousarray(inputs["moe_w_gate"][0], dtype=f)
        m["moe_w_up"] = np.ascontiguousarray(inputs["moe_w_up"][0], dtype=f)
        m["moe_w_down"] = np.ascontiguousarray(inputs["moe_w_down"][0], dtype=f)
        m["iota_row"] = np.ascontiguousarray(np.broadcast_to(np.arange(CG, dtype=f)[None, :], (128, CG)))
        m["gbase"] = np.ascontiguousarray(np.broadcast_to((np.arange(8, dtype=f) * CG)[None, :], (128, 8)))
        m["ustrict"] = np.triu(np.ones((128, 128), dtype=f), k=1).astype(ml_dtypes.bfloat16)
    return m


def kernel(**inputs):
    nc, es = build()
    in_maps = [make_in_map(inputs, b) for b in range(8)]
    res = run_bass_kernel_spmd(nc, in_maps, core_ids=list(range(8)))
    out = np.stack([np.asarray(r["out"], dtype=np.float32) for r in res.results], axis=0)
    return out
```
